# Optimizing a Trainium2 kernel written in Bass

```python
import jax, jax.numpy as jnp
from jax import lax
import numpy as np

D_MODEL = 2048
BATCH = 4
SEQ = 4096
DEPTH = 1

GRID_W = 64
CTX_LEN = 256
HEAD_DIM = 128
ATT_HEADS = 8
ATT_KV_HEADS = 2
ATT_GROUP = ATT_HEADS // ATT_KV_HEADS
RET_HEADS = 4
RET_QK_DIM = 128
RET_V_DIM = 256
ROPE_THETA = 10000.0
Q_BLOCK = 128
RET_CHUNK = 128
N_EXPERTS = 16
EXPERT_FF = 2048
EC_FACTOR = 2
EPS = 1e-6

ATT_W = ATT_HEADS * HEAD_DIM
ATT_KV_W = ATT_KV_HEADS * HEAD_DIM
RET_QK_W = RET_HEADS * RET_QK_DIM
RET_V_W = RET_HEADS * RET_V_DIM
Q_SIZES = (ATT_W, RET_QK_W, RET_V_W, D_MODEL, D_MODEL)
KV_SIZES = (ATT_KV_W, ATT_KV_W, RET_QK_W, RET_V_W)
Q_WIDTH = ATT_W + RET_QK_W + RET_V_W + 2 * D_MODEL
KV_WIDTH = 2 * ATT_KV_W + RET_QK_W + RET_V_W
IN_WIDTH = Q_WIDTH + KV_WIDTH

kernel_name = 'hybrid_gqa_retention_ec_moe_prefix_dit'


def _points(sizes):
    return [int(p) for p in np.cumsum(sizes)[:-1]]


def rms_norm(x, g):
    xf = x.astype(jnp.float32)
    y = xf * lax.rsqrt(jnp.mean(xf * xf, axis=-1, keepdims=True) + EPS)
    return (y * g.astype(jnp.float32)).astype(x.dtype)


def modulate(x, shift, scale):
    return x * (1.0 + scale) + shift


def axial_rope_tables(n_tokens):
    rows = n_tokens // GRID_W
    r, cl = jnp.meshgrid(jnp.arange(rows), jnp.arange(GRID_W), indexing='ij')
    r = r.reshape(-1).astype(jnp.float32)
    cl = cl.reshape(-1).astype(jnp.float32)
    quarter = HEAD_DIM // 4
    inv = ROPE_THETA ** (-jnp.arange(quarter, dtype=jnp.float32) / quarter)
    ang_r = r[:, None] * inv
    ang_c = cl[:, None] * inv
    ang = jnp.concatenate([ang_r, ang_r, ang_c, ang_c], axis=-1)
    return jnp.cos(ang), jnp.sin(ang)


def apply_rope(x, cos, sin):
    x4 = x.reshape(x.shape[:-1] + (2, 2, HEAD_DIM // 4))
    rot = jnp.stack([-x4[..., 1, :], x4[..., 0, :]], axis=-2).reshape(x.shape)
    out = x.astype(jnp.float32) * cos[:, None, :] + rot.astype(jnp.float32) * sin[:, None, :]
    return out.astype(x.dtype)


def q_heads(z_q, q_norm_g, cos, sin):
    B, T, _ = z_q.shape
    q_a, q_r, g_r, gate_a, gate_r = jnp.split(z_q, _points(Q_SIZES), axis=-1)
    q_a = rms_norm(q_a.reshape(B, T, ATT_HEADS, HEAD_DIM), q_norm_g)
    q_r = q_r.reshape(B, T, RET_HEADS, RET_QK_DIM)
    if cos is not None:
        q_a = apply_rope(q_a, cos, sin)
        q_r = apply_rope(q_r, cos, sin)
    q_r = q_r.transpose(0, 2, 1, 3).astype(jnp.float32)
    return q_a, q_r, g_r, gate_a, gate_r


def kv_heads(z_kv, k_norm_g, cos, sin):
    B, T, _ = z_kv.shape
    k_a, v_a, k_r, v_r = jnp.split(z_kv, _points(KV_SIZES), axis=-1)
    k_a = rms_norm(k_a.reshape(B, T, ATT_KV_HEADS, HEAD_DIM), k_norm_g)
    v_a = v_a.reshape(B, T, ATT_KV_HEADS, HEAD_DIM)
    k_r = k_r.reshape(B, T, RET_HEADS, RET_QK_DIM) * (RET_QK_DIM ** -0.5)
    if cos is not None:
        k_a = apply_rope(k_a, cos, sin)
        k_r = apply_rope(k_r, cos, sin)
    k_r = k_r.transpose(0, 2, 1, 3).astype(jnp.float32)
    v_r = v_r.reshape(B, T, RET_HEADS, RET_V_DIM).transpose(0, 2, 1, 3).astype(jnp.float32)
    return k_a, v_a, k_r, v_r


def latent_attention(q, k, v, k_ctx, v_ctx):
    B, T, _, _ = q.shape
    nb = T // Q_BLOCK
    keys = jnp.concatenate([k_ctx, k], axis=1).transpose(0, 2, 1, 3)
    vals = jnp.concatenate([v_ctx, v], axis=1).transpose(0, 2, 1, 3)
    qb = q.reshape(B, nb, Q_BLOCK, ATT_KV_HEADS, ATT_GROUP, HEAD_DIM).transpose(1, 0, 3, 4, 2, 5)
    scale = HEAD_DIM ** -0.5

    def block(qi):
        s = jnp.einsum('bhgqd,bhkd->bhgqk', qi, keys).astype(jnp.float32) * scale
        p = jax.nn.softmax(s, axis=-1).astype(vals.dtype)
        return jnp.einsum('bhgqk,bhkd->bhgqd', p, vals)

    o = lax.map(block, qb)
    return o.transpose(1, 0, 4, 2, 3, 5).reshape(B, T, ATT_W)


def context_attention(q, k, v):
    B, L, _, _ = q.shape
    qg = q.reshape(B, L, ATT_KV_HEADS, ATT_GROUP, HEAD_DIM)
    s = jnp.einsum('blhgd,bmhd->bhglm', qg, k).astype(jnp.float32) * (HEAD_DIM ** -0.5)
    p = jax.nn.softmax(s, axis=-1).astype(v.dtype)
    return jnp.einsum('bhglm,bmhd->blhgd', p, v).reshape(B, L, ATT_W)


def retention_chunked(q, k, v, log_g, s0):
    B, H, T, dk = q.shape
    dv = v.shape[-1]
    C = RET_CHUNK
    n = T // C
    i = jnp.arange(C, dtype=jnp.float32)
    diff = i[:, None] - i[None, :]
    mask = jnp.where(diff >= 0, jnp.exp(jnp.maximum(diff, 0.0)[None] * log_g[:, None, None]), 0.0)
    qc = q.reshape(B, H, n, C, dk)
    kc = k.reshape(B, H, n, C, dk)
    vc = v.reshape(B, H, n, C, dv)
    s = jnp.einsum('bhnid,bhnjd->bhnij', qc, kc) * mask[None, :, None]
    o_inner = jnp.einsum('bhnij,bhnjv->bhniv', s, vc)
    w_k = jnp.exp((C - 1 - i)[None] * log_g[:, None])
    u = jnp.einsum('bhnjd,bhnjv->nbhdv', kc * w_k[None, :, None, :, None], vc)
    g_chunk = jnp.exp(C * log_g)[None, :, None, None]

    def step(state, u_n):
        return g_chunk * state + u_n, state

    s_final, s_prev = lax.scan(step, s0, u)
    w_q = jnp.exp((i + 1.0)[None] * log_g[:, None])
    o_cross = jnp.einsum('bhnid,nbhdv->bhniv', qc * w_q[None, :, None, :, None], s_prev)
    return (o_inner + o_cross).reshape(B, H, T, dv), s_final


def retention_final_state(k, v, log_g):
    L = k.shape[2]
    age = jnp.arange(L - 1, -1, -1, dtype=jnp.float32)
    w = jnp.exp(age[None] * log_g[:, None])
    return jnp.einsum('bhld,bhlv->bhdv', k * w[None, :, :, None], v)


def bidir_retention(q, k, v, log_f, log_b, s_f, s_b):
    o_f, _ = retention_chunked(q, k, v, log_f, s_f)
    o_b, _ = retention_chunked(jnp.flip(q, 2), jnp.flip(k, 2), jnp.flip(v, 2), log_b, s_b)
    return o_f + jnp.flip(o_b, 2)


def retention_output(o, g_r, gn_g):
    B, H, T, dv = o.shape
    mu = jnp.mean(o, axis=-1, keepdims=True)
    var = jnp.mean(jnp.square(o - mu), axis=-1, keepdims=True)
    y = ((o - mu) * lax.rsqrt(var + EPS)).transpose(0, 2, 1, 3).reshape(B, T, H * dv)
    y = y * gn_g.astype(jnp.float32) * jax.nn.silu(g_r.astype(jnp.float32))
    return y.astype(g_r.dtype)


def merge_branches(o_att, o_ret, gate_a, gate_r, w_o_att, w_o_ret, w_out):
    y = jax.nn.sigmoid(gate_a) * (o_att @ w_o_att) + jax.nn.sigmoid(gate_r) * (o_ret @ w_o_ret)
    return y @ w_out


def expert_choice_ffn(h, w_router, w_gate, w_up, w_down):
    B, T, D = h.shape
    cap = EC_FACTOR * T // N_EXPERTS
    aff = jax.nn.softmax((h @ w_router).astype(jnp.float32), axis=-1)
    top_w, top_i = lax.top_k(aff.transpose(0, 2, 1), cap)
    xg = jax.vmap(lambda hb, ib: hb[ib])(h, top_i)
    a = jnp.einsum('becd,edf->becf', xg, w_gate)
    b = jnp.einsum('becd,edf->becf', xg, w_up)
    y = jnp.einsum('becf,efd->becd', jax.nn.silu(a) * b, w_down) * top_w[..., None].astype(h.dtype)
    return jax.vmap(lambda yb, ib: jnp.zeros((T, D), h.dtype).at[ib.reshape(-1)].add(yb.reshape(-1, D)))(y, top_i)


def setup_inputs(seed: int = 0) -> dict:
    key = jax.random.key(seed)
    ks = jax.random.split(key, 24)
    f32 = jnp.float32

    def nrm(k, shape, s):
        return jax.random.normal(k, shape, f32) * s

    def gain(k, shape):
        return 1.0 + 0.01 * jax.random.normal(k, shape, f32)

    gamma0 = 1.0 - 2.0 ** (-5.0 - np.arange(RET_HEADS))
    p0 = np.log(np.expm1(-np.log(gamma0))).astype(np.float32)
    return {
        'x': nrm(ks[0], (BATCH, SEQ, D_MODEL), 1.0),
        'c': nrm(ks[1], (BATCH, D_MODEL), 1.0),
        'ctx': nrm(ks[2], (BATCH, CTX_LEN, D_MODEL), 1.0),
        'c_ctx': nrm(ks[3], (D_MODEL,), 1.0),
        'w_mod': nrm(ks[4], (DEPTH, D_MODEL, 6 * D_MODEL), 0.5 * D_MODEL ** -0.5),
        'b_mod': nrm(ks[5], (DEPTH, 6 * D_MODEL), 0.01),
        'pre_norm1': gain(ks[6], (DEPTH, D_MODEL)),
        'post_norm1': gain(ks[7], (DEPTH, D_MODEL)),
        'pre_norm2': gain(ks[8], (DEPTH, D_MODEL)),
        'post_norm2': gain(ks[9], (DEPTH, D_MODEL)),
        'w_in': nrm(ks[10], (DEPTH, D_MODEL, IN_WIDTH), D_MODEL ** -0.5),
        'q_norm': gain(ks[11], (DEPTH, HEAD_DIM)),
        'k_norm': gain(ks[12], (DEPTH, HEAD_DIM)),
        'ret_decay': jnp.asarray(p0)[None, None, :] + nrm(ks[13], (DEPTH, 2, RET_HEADS), 0.05),
        'ret_gn': gain(ks[14], (DEPTH, RET_V_W)),
        'w_o_att': nrm(ks[15], (DEPTH, ATT_W, D_MODEL), ATT_W ** -0.5),
        'w_o_ret': nrm(ks[16], (DEPTH, RET_V_W, D_MODEL), RET_V_W ** -0.5),
        'w_out': nrm(ks[17], (DEPTH, D_MODEL, D_MODEL), D_MODEL ** -0.5),
        'w_router': nrm(ks[18], (DEPTH, D_MODEL, N_EXPERTS), D_MODEL ** -0.5),
        'w_gate': nrm(ks[19], (DEPTH, N_EXPERTS, D_MODEL, EXPERT_FF), D_MODEL ** -0.5),
        'w_up': nrm(ks[20], (DEPTH, N_EXPERTS, D_MODEL, EXPERT_FF), D_MODEL ** -0.5),
        'w_down': nrm(ks[21], (DEPTH, N_EXPERTS, EXPERT_FF, D_MODEL), EXPERT_FF ** -0.5),
    }


def reference(x, c, ctx, c_ctx, w_mod, b_mod, pre_norm1, post_norm1, pre_norm2, post_norm2,
              w_in, q_norm, k_norm, ret_decay, ret_gn, w_o_att, w_o_ret, w_out,
              w_router, w_gate, w_up, w_down):
    B, T, _ = x.shape
    cos, sin = axial_rope_tables(T)
    xc = ctx
    for layer in range(DEPTH):
        last = layer == DEPTH - 1
        mod = jax.nn.silu(c) @ w_mod[layer] + b_mod[layer]
        sh1, sc1, gt1, sh2, sc2, gt2 = [m[:, None, :] for m in jnp.split(mod, 6, axis=-1)]
        mod_c = jax.nn.silu(c_ctx) @ w_mod[layer] + b_mod[layer]
        csh1, csc1, cgt1, csh2, csc2, cgt2 = jnp.split(mod_c, 6, axis=-1)
        log_f = -jax.nn.softplus(ret_decay[layer, 0].astype(jnp.float32))
        log_b = -jax.nn.softplus(ret_decay[layer, 1].astype(jnp.float32))

        h = modulate(rms_norm(x, pre_norm1[layer]), sh1, sc1)
        hc = modulate(rms_norm(xc, pre_norm1[layer]), csh1, csc1)
        kc_a, vc_a, kc_r, vc_r = kv_heads(hc @ w_in[layer][:, Q_WIDTH:], k_norm[layer], None, None)
        sc_f = retention_final_state(kc_r, vc_r, log_f)
        sc_b = retention_final_state(jnp.flip(kc_r, 2), jnp.flip(vc_r, 2), log_b)
        z = h @ w_in[layer]
        q_a, q_r, g_r, gate_a, gate_r = q_heads(z[..., :Q_WIDTH], q_norm[layer], cos, sin)
        k_a, v_a, k_r, v_r = kv_heads(z[..., Q_WIDTH:], k_norm[layer], cos, sin)
        o_att = latent_attention(q_a, k_a, v_a, kc_a, vc_a)
        o_ret = retention_output(bidir_retention(q_r, k_r, v_r, log_f, log_b, sc_f, sc_b), g_r, ret_gn[layer])
        y = merge_branches(o_att, o_ret, gate_a, gate_r, w_o_att[layer], w_o_ret[layer], w_out[layer])
        x = x + gt1 * rms_norm(y, post_norm1[layer])
        if not last:
            qc_a, qc_r, gc_r, gatec_a, gatec_r = q_heads(hc @ w_in[layer][:, :Q_WIDTH], q_norm[layer], None, None)
            oc_att = context_attention(qc_a, kc_a, vc_a)
            zero_state = jnp.zeros_like(sc_f)
            oc_ret = retention_output(bidir_retention(qc_r, kc_r, vc_r, log_f, log_b, zero_state, zero_state),
                                      gc_r, ret_gn[layer])
            yc = merge_branches(oc_att, oc_ret, gatec_a, gatec_r, w_o_att[layer], w_o_ret[layer], w_out[layer])
            xc = xc + cgt1 * rms_norm(yc, post_norm1[layer])

        h2 = modulate(rms_norm(x, pre_norm2[layer]), sh2, sc2)
        y2 = expert_choice_ffn(h2, w_router[layer], w_gate[layer], w_up[layer], w_down[layer])
        x = x + gt2 * rms_norm(y2, post_norm2[layer])
        if not last:
            hc2 = modulate(rms_norm(xc, pre_norm2[layer]), csh2, csc2)
            yc2 = expert_choice_ffn(hc2, w_router[layer], w_gate[layer], w_up[layer], w_down[layer])
            xc = xc + cgt2 * rms_norm(yc2, post_norm2[layer])
    return x
```

```python
import numpy as np
import contextlib
import concourse.bass as bass
import concourse.mybir as mybir
from concourse.bass_utils import run_bass_kernel_spmd

F32 = mybir.dt.float32
BF16 = mybir.dt.bfloat16
I32 = mybir.dt.int32
U32 = mybir.dt.uint32
AF = mybir.ActivationFunctionType
ALU = mybir.AluOpType
AX = mybir.AxisListType

D = 2048
T = 4096
LC = 256
TK = T + LC
NCH = 16
QW = 6656
INW = 8704
EPS = 1e-6


class Buf:
    __slots__ = ("w", "r", "name")

    def __init__(self, name=""):
        self.w = None
        self.r = {}
        self.name = name


class KB:
    def __init__(self, nc):
        self.nc = nc
        self.es = contextlib.ExitStack()
        self.E = dict(pe=nc.tensor, act=nc.scalar, dve=nc.vector, pool=nc.gpsimd, sp=nc.sync)
        self.clk = {}
        self.seen = {e: {} for e in self.E}
        self.nsem = 0
        for e in ("pe", "act", "dve", "pool"):
            self._new_clk(e)
        self.dpool = {}
        for q, n in (("sp", 20), ("pool", 12), ("act", 4)):
            self.dpool[q] = [[self._sem(f"d{q}{i}"), 0] for i in range(n)]
        self.dnext = {q: 0 for q in self.dpool}

    def _sem(self, name):
        self.nsem += 1
        sm = self.es.enter_context(self.nc.semaphore(name + f"_{self.nsem}"))
        if not hasattr(self, "_keep"):
            self._keep = []
        self._keep.append(sm)
        return sm

    def _new_clk(self, e):
        self.clk[e] = [self._sem("clk" + e), 0]

    def wait(self, eng, tok):
        sem, val = tok
        k = id(sem)
        if eng == "pe" and sem is self.clk["pe"][0]:
            return
        if self.seen[eng].get(k, 0) >= val:
            return
        self.E[eng].wait_ge(sem, val)
        self.seen[eng][k] = val

    def _deps(self, eng, reads, writes):
        for b in reads:
            if b.w is not None:
                self.wait(eng, b.w)
        for b in writes:
            if b.w is not None:
                self.wait(eng, b.w)
            for k, (sem, val) in b.r.items():
                self.wait(eng, (sem, val))

    def _commit(self, tok, reads, writes):
        sem, val = tok
        k = id(sem)
        for b in reads:
            if k not in b.r or b.r[k][1] < val:
                b.r[k] = (sem, val)
        for b in writes:
            b.w = tok
            b.r = {}

    def op(self, eng, fn, reads=(), writes=()):
        self._deps(eng, reads, writes)
        ins = fn()
        c = self.clk[eng]
        c[1] += 1
        ins.then_inc(c[0], 1)
        tok = (c[0], c[1])
        self._commit(tok, reads, writes)
        return tok

    def dma(self, q, fn, reads=(), writes=()):
        self._deps(q, reads, writes)
        pool = self.dpool[q]
        i = self.dnext[q]
        self.dnext[q] = (i + 1) % len(pool)
        ent = pool[i]
        if ent[1] > 0:
            self.wait(q, (ent[0], ent[1]))
        ins = fn()
        ent[1] += 16
        ins.then_inc(ent[0], 16)
        tok = (ent[0], ent[1])
        self._commit(tok, reads, writes)
        return tok

    def _alltoks(self):
        toks = []
        for e, c in self.clk.items():
            if c[1] > 0:
                toks.append((c[0], c[1]))
        for q, pool in self.dpool.items():
            for ent in pool:
                if ent[1] > 0:
                    toks.append((ent[0], ent[1]))
        return toks

    def barrier(self):
        toks = self._alltoks()
        for e in self.E:
            for t in toks:
                self.wait(e, t)

    def new_phase(self):
        self.barrier()
        for e in ("pe", "act", "dve", "pool"):
            if self.clk[e][1] > 0:
                self._new_clk(e)

    def finish(self, eng="sp"):
        for t in self._alltoks():
            self.wait(eng, t)


class Phase:
    def __init__(self, kb):
        self.kb = kb
        self.es = contextlib.ExitStack()

    def __enter__(self):
        return self

    def __exit__(self, *a):
        self.kb.new_phase()
        self.es.close()
        return False

    _n = [0]

    def sb(self, name, shape, dt):
        Phase._n[0] += 1
        t = self.es.enter_context(self.kb.nc.sbuf_tensor(f"{name}_s{Phase._n[0]}", list(shape), dt))
        return t

    def ps(self, name, shape, dt):
        Phase._n[0] += 1
        return self.es.enter_context(self.kb.nc.psum_tensor(f"{name}_p{Phase._n[0]}", list(shape), dt))


def rope_consts():
    Rm = np.zeros((128, 128), np.float32)
    for a in range(2):
        for j in range(32):
            Rm[a * 64 + 32 + j, a * 64 + j] = -1.0
            Rm[a * 64 + j, a * 64 + 32 + j] = 1.0
    t = np.arange(T)
    r = (t // 64).astype(np.float32)
    cl = (t % 64).astype(np.float32)
    inv = (10000.0 ** (-np.arange(32, dtype=np.float32) / 32)).astype(np.float32)
    ang_r = r[:, None] * inv
    ang_c = cl[:, None] * inv
    ang = np.concatenate([ang_r, ang_r, ang_c, ang_c], axis=-1)
    cosT = np.ascontiguousarray(np.cos(ang).T.astype(np.float32))
    sinT = np.ascontiguousarray(np.sin(ang).T.astype(np.float32))
    return Rm, cosT, sinT


P4STOP = 0
P4VAR = 2
SKIP3 = 0
NEXP = 16
NBISECT = 26


def build(dbg=(), stop_after=99):
    nc = bass.Bass("TRN2", target_bir_lowering=False)
    kb = KB(nc)
    dbg = set(dbg)

    def din(name, shape, dt=F32):
        return nc.dram_tensor(name, list(shape), dt, kind="ExternalInput").ap()

    def dscr(name, shape, dt):
        kind = "ExternalOutput" if name in dbg else "Internal"
        return nc.dram_tensor(name, list(shape), dt, kind=kind).ap()

    xk = din("xk", [TK, D])
    c2 = din("c2", [2, D])
    w_mod = din("w_mod", [D, 6 * D])
    b_mod = din("b_mod", [1, 6 * D])
    pre1 = din("pre1", [1, D]); post1 = din("post1", [1, D]); pre2 = din("pre2", [1, D]); post2 = din("post2", [1, D])
    w_in = din("w_in", [D, INW])
    ident_in = din("ident", [128, 128])
    out = nc.dram_tensor("out", [T, D], F32, kind="ExternalOutput").ap()

    mod_d = dscr("mod_d", [2, 6 * D], F32)
    hT_d = dscr("hT_d", [128, NCH, TK], BF16)

    G = Phase(kb)
    ident_f = G.sb("ident_f", [128, 128], F32)
    ident_b = G.sb("ident_b", [128, 128], BF16)
    eps_t = G.sb("eps_t", [128, 1], F32)
    B_const = Buf("const")
    kb.dma("sp", lambda: nc.sync.dma_start(out=ident_f[:], in_=ident_in[:, :]), writes=[B_const])
    kb.dma("pool", lambda: nc.gpsimd.dma_start(out=ident_b[:], in_=ident_in[:, :]), writes=[B_const])
    kb.op("dve", lambda: nc.vector.memset(eps_t[:], EPS), writes=[B_const])
    mhalf = G.sb("mhalf", [128, 1], F32)
    kb.op("dve", lambda: nc.vector.memset(mhalf[:], -0.5), writes=[B_const])

    def rstd_from(out_ap, ssq_ap, n, R, W):
        kb.op("dve", lambda: nc.vector.tensor_scalar(out=out_ap, in0=ssq_ap, scalar1=1.0 / n, scalar2=EPS, op0=ALU.mult, op1=ALU.add), R, W)
        kb.op("pool", lambda: nc.gpsimd.tensor_tensor(out=out_ap, in0=out_ap, in1=mhalf[:], op=ALU.pow), [B_const], W)
    kb.barrier()

    with Phase(kb) as ph:
        c2T = ph.sb("c2T", [128, 2, 16], F32)
        s2T = ph.sb("s2T", [128, 2, 16], BF16)
        bm2 = ph.sb("bm2", [2, 6 * D], F32)
        B_c2 = Buf(); B_s2 = Buf(); B_bm = Buf()
        for r in range(2):
            kb.dma("sp", lambda r=r: nc.sync.dma_start(out=c2T[:, r, :], in_=c2[r, :].rearrange("(p c) -> p c", c=16)), writes=[B_c2])
            kb.dma("sp", lambda r=r: nc.sync.dma_start(out=bm2[r:r + 1, :], in_=b_mod[:, :]), writes=[B_bm])
        kb.op("act", lambda: nc.scalar.activation(out=s2T[:], in_=c2T[:], func=AF.Silu), reads=[B_c2], writes=[B_s2])
        wmv = w_mod.rearrange("(p c) n -> p c n", c=16)
        NB = 24
        NWB = 3
        wb = [ph.sb(f"wmb{i}", [128, 16, 512], BF16) for i in range(NWB)]
        Bwb = [Buf() for _ in range(NWB)]
        pm = [ph.ps(f"pm{i}", [2, 512], F32) for i in range(2)]
        Bpm = [Buf(), Buf()]
        mrow = [ph.sb(f"mrow{i}", [2, 512], F32) for i in range(2)]
        Bmr = [Buf(), Buf()]

        def ldw(j):
            s_ = j % NWB
            for hh in range(2):
                kb.dma("pool", lambda j=j, s_=s_, hh=hh: nc.gpsimd.dma_start(out=wb[s_][:, hh * 8:(hh + 1) * 8, :], in_=wmv[:, hh * 8:(hh + 1) * 8, j * 512:(j + 1) * 512]), writes=[Bwb[s_]])
        ldw(0); ldw(1)
        for j in range(NB):
            s = j % 2; sw = j % NWB
            if j + 2 < NB:
                ldw(j + 2)
            for c in range(16):
                kb.op("pe", lambda c=c, s=s, sw=sw: nc.tensor.matmul(pm[s][:], lhsT=s2T[:, :, c], rhs=wb[sw][:, c, :], start=(c == 0), stop=(c == 15)),
                      reads=[B_s2, Bwb[sw]], writes=[Bpm[s]])
            kb.op("dve", lambda j=j, s=s: nc.vector.tensor_tensor(out=mrow[s][:], in0=pm[s][:], in1=bm2[:, j * 512:(j + 1) * 512], op=ALU.add),
                  reads=[Bpm[s], B_bm], writes=[Bmr[s]])
            kb.dma("sp", lambda j=j, s=s: nc.sync.dma_start(out=mod_d[:, j * 512:(j + 1) * 512], in_=mrow[s][:]), reads=[Bmr[s]])
    if stop_after <= 0:
        kb.finish(); return nc

    def bcast_row(ph, name, src_ap):
        t = ph.sb(name, [128, D], F32)
        b = Buf(name)
        kb.dma("sp", lambda: nc.sync.dma_start(out=t[:], in_=src_ap.partition_broadcast(128)), writes=[b])
        return t, b

    with Phase(kb) as ph:
        g1, Bg1 = bcast_row(ph, "g1", pre1[0:1, :])
        Gm = []
        for r in range(2):
            sc, Bsc = bcast_row(ph, f"sc{r}", mod_d[r:r + 1, D:2 * D])
            sh, Bsh = bcast_row(ph, f"sh{r}", mod_d[r:r + 1, 0:D])
            kb.op("dve", lambda sc=sc: nc.vector.scalar_tensor_tensor(out=sc[:], in0=sc[:], scalar=1.0, in1=g1[:], op0=ALU.add, op1=ALU.mult),
                  reads=[Bg1], writes=[Bsc])
            Gm.append((sc, Bsc, sh, Bsh))
        NT = TK // 128
        xt = [ph.sb(f"xt{i}", [128, D], F32) for i in range(3)]; Bxt = [Buf() for _ in range(3)]
        junk = ph.sb("junk", [128, D], BF16); Bjunk = Buf()
        ssq = [ph.sb(f"ssq{i}", [128, 1], F32) for i in range(2)]; Bssq = [Buf(), Buf()]
        rstd = [ph.sb(f"rstd{i}", [128, 1], F32) for i in range(2)]; Brstd = [Buf(), Buf()]
        h1 = [ph.sb(f"h1{i}", [128, D], F32) for i in range(2)]; Bh1 = [Buf(), Buf()]
        hb = [ph.sb(f"hb{i}", [128, D], BF16) for i in range(2)]; Bhb = [Buf(), Buf()]
        pt = [ph.ps(f"pt{i}", [128, 8, 128], BF16) for i in range(4)]; Bpt = [Buf() for _ in range(4)]
        hT = [ph.sb(f"hTt{i}", [128, NCH, 512], BF16) for i in range(2)]; BhT = [Buf(), Buf()]

        def ldx(i):
            if i < NT:
                kb.dma("sp", lambda: nc.sync.dma_start(out=xt[i % 3][:], in_=xk[i * 128:(i + 1) * 128, :]), writes=[Bxt[i % 3]])
        ldx(0); ldx(1)

        def stA(i):
            s3 = i % 3; s = i % 2
            ldx(i + 2)
            r = 1 if i < LC // 128 else 0
            sc, Bsc, sh, Bsh = Gm[r]
            kb.op("act", lambda: nc.scalar.activation(out=junk[:], in_=xt[s3][:], func=AF.Square, accum_out=ssq[s][:]),
                  reads=[Bxt[s3]], writes=[Bjunk, Bssq[s]])
            rstd_from(rstd[s][:], ssq[s][:], D, [Bssq[s]], [Brstd[s]])
            kb.op("dve", lambda: nc.vector.scalar_tensor_tensor(out=h1[s][:], in0=xt[s3][:], scalar=rstd[s][:], in1=sc[:], op0=ALU.mult, op1=ALU.mult),
                  reads=[Bxt[s3], Brstd[s], Bsc], writes=[Bh1[s]])
            kb.op("pool", lambda: nc.gpsimd.tensor_tensor(out=hb[s][:], in0=h1[s][:], in1=sh[:], op=ALU.add),
                  reads=[Bh1[s], Bsh], writes=[Bhb[s]])

        def stB(i):
            s = i % 2
            if i < 2:
                grp, gi, gn_ = 0, i, 2
            else:
                grp, gi, gn_ = 1 + (i - 2) // 4, (i - 2) % 4, 4
            sg_ = grp % 2
            for hf in range(2):
                pi = (2 * i + hf) % 4
                for c in range(8):
                    cc = hf * 8 + c
                    kb.op("pe", lambda c=c, cc=cc: nc.tensor.transpose(out=pt[pi][:, c, :], in_=hb[s][:, cc * 128:(cc + 1) * 128], identity=ident_b[:]),
                          reads=[Bhb[s]], writes=[Bpt[pi]])
                dst = hT[sg_][:, hf * 8:(hf + 1) * 8, gi * 128:(gi + 1) * 128]
                if hf == 0:
                    kb.op("act", lambda: nc.scalar.copy(out=dst, in_=pt[pi][:]), reads=[Bpt[pi]], writes=[BhT[sg_]])
                else:
                    kb.op("dve", lambda: nc.vector.tensor_copy(out=dst, in_=pt[pi][:]), reads=[Bpt[pi]], writes=[BhT[sg_]])
            if gi == gn_ - 1:
                tk0 = 0 if grp == 0 else LC + (grp - 1) * 512
                nn = gn_ * 128
                for hh in range(2):
                    kb.dma("sp", lambda hh=hh: nc.sync.dma_start(out=hT_d[:, hh * 8:(hh + 1) * 8, tk0:tk0 + nn], in_=hT[sg_][:, hh * 8:(hh + 1) * 8, :nn]), reads=[BhT[sg_]])

        for i in range(NT + 1):
            if i < NT:
                stA(i)
            if i >= 1:
                stB(i - 1)
    if stop_after <= 1:
        kb.finish(); return nc

    def mm(out_, lhsT, rhs, st, sp, R, W):
        return kb.op("pe", lambda: nc.tensor.matmul(out_, lhsT=lhsT, rhs=rhs, start=st, stop=sp), R, W)

    def tr(out_, in_, idn, R, W):
        return kb.op("pe", lambda: nc.tensor.transpose(out=out_, in_=in_, identity=idn), R, W)

    def act(out_, in_, func, R, W, **kw):
        return kb.op("act", lambda: nc.scalar.activation(out=out_, in_=in_, func=func, **kw), R, W)

    def tt(eng, out_, in0, in1, op, R, W):
        e = nc.vector if eng == "dve" else nc.gpsimd
        return kb.op(eng, lambda: e.tensor_tensor(out=out_, in0=in0, in1=in1, op=op), R, W)

    def stt(out_, in0, scalar, in1, op0, op1, R, W):
        return kb.op("dve", lambda: nc.vector.scalar_tensor_tensor(out=out_, in0=in0, scalar=scalar, in1=in1, op0=op0, op1=op1), R, W)

    def ts(eng, out_, in0, s1, s2, op0, op1, R, W):
        e = nc.vector if eng == "dve" else nc.gpsimd
        if op1 is None:
            return kb.op(eng, lambda: e.tensor_scalar(out=out_, in0=in0, scalar1=s1, scalar2=None, op0=op0), R, W)
        return kb.op(eng, lambda: e.tensor_scalar(out=out_, in0=in0, scalar1=s1, scalar2=s2, op0=op0, op1=op1), R, W)

    def cp(eng, out_, in_, R, W):
        if eng == "act":
            return kb.op("act", lambda: nc.scalar.copy(out=out_, in_=in_), R, W)
        e = nc.vector if eng == "dve" else nc.gpsimd
        return kb.op(eng, lambda: e.tensor_copy(out=out_, in_=in_), R, W)

    def ld(q, out_, in_, W, R=()):
        e = nc.sync if q == "sp" else nc.gpsimd
        return kb.dma(q, lambda: e.dma_start(out=out_, in_=in_), R, W)

    qn_in = din("q_norm", [128, 1]); kn_in = din("k_norm", [128, 1])
    rm_in = din("rotm", [128, 128]); cos_in = din("cosT", [128, T]); sin_in = din("sinT", [128, T])
    qaT_d = dscr("qaT_d", [8, 128, T], BF16); qrT_d = dscr("qrT_d", [4, 128, T], BF16)
    sgT_d = dscr("sgT_d", [8, 128, T], BF16); gaT_d = dscr("gaT_d", [16, 128, T], BF16); grT_d = dscr("grT_d", [16, 128, T], BF16)
    kaT_d = dscr("kaT_d", [2, 128, TK], BF16); krT_d = dscr("krT_d", [4, 128, TK], BF16)
    va_d = dscr("va_d", [TK, 256], BF16); vr_d = dscr("vr_d", [TK, 1024], BF16)
    ones_b = G.sb("ones_b", [128, 128], BF16)
    kb.op("dve", lambda: nc.vector.memset(ones_b[:], 1.0), writes=[B_const])
    kb.barrier()

    with Phase(kb) as ph:
        qn_t = ph.sb("qn_t", [128, 1], F32); kn_t = ph.sb("kn_t", [128, 1], F32)
        rm_b = ph.sb("rm_b", [128, 128], BF16)
        cos_t = ph.sb("cos_t", [128, T], F32); sin_t = ph.sb("sin_t", [128, T], F32)
        Bc = Buf()
        ld("sp", qn_t[:], qn_in[:, :], [Bc]); ld("sp", kn_t[:], kn_in[:, :], [Bc])
        ld("pool", rm_b[:], rm_in[:, :], [Bc])
        ld("sp", cos_t[:], cos_in[:, :], [Bc]); ld("sp", sin_t[:], sin_in[:, :], [Bc])
        wv = w_in.rearrange("(c p) n -> p c n", p=128)
        Wt = [ph.sb(f"Wt{i}", [128, NCH, 768], BF16) for i in range(2)]; BW = [Buf(), Buf()]
        hB = [ph.sb(f"hB{i}", [128, NCH, 512], BF16) for i in range(2)]; BhB = [Buf(), Buf()]
        pz = [ph.ps(f"pz{i}", [128, 512], F32) for i in range(4)]; Bpz = [Buf() for _ in range(4)]
        pq = [ph.ps(f"pq{i}", [128, 512], F32) for i in range(2)]; Bpq = [Buf() for _ in range(2)]
        pr = [ph.ps(f"pr{i}", [128, 512], F32) for i in range(2)]; Bpr = [Buf() for _ in range(2)]
        sqb = [ph.sb(f"sqb{i}", [128, 512], BF16) for i in range(2)]; Bsq = [Buf() for _ in range(2)]
        sd = [ph.sb(f"sd{i}", [128, 512], F32) for i in range(2)]; Bsd = [Buf() for _ in range(2)]
        qnb = [ph.sb(f"qnb{i}", [128, 512], BF16) for i in range(3)]; Bqn = [Buf() for _ in range(3)]
        t1 = [ph.sb(f"t1{i}", [128, 512], F32) for i in range(2)]; Bt1 = [Buf() for _ in range(2)]
        t2 = [ph.sb(f"t2{i}", [128, 512], F32) for i in range(2)]; Bt2 = [Buf() for _ in range(2)]
        ob = [ph.sb(f"ob{i}", [128, 512], BF16) for i in range(4)]; Bob = [Buf() for _ in range(4)]
        cnt = dict(z=0, q=0, r=0, sq=0, sd=0, qn=0, t=0, ob=0, w=0, h=0)
        deferred = []

        def nxt(k, n):
            v = cnt[k] % n
            cnt[k] += 1
            return v

        def run_deferred():
            todo = list(deferred)
            deferred.clear()
            for f in todo:
                f()

        def rope_and_store(iq, N, t0, dst):
            ir = nxt("r", 2)
            mm(pr[ir][:, :N], rm_b[:], qnb[iq][:, :N], True, True, [Bqn[iq], Bc], [Bpr[ir]])

            def fin():
                it = nxt("t", 2); io = nxt("ob", 4)
                tt("dve", t1[it][:, :N], qnb[iq][:, :N], cos_t[:, t0:t0 + N], ALU.mult, [Bqn[iq], Bc], [Bt1[it]])
                tt("dve", t2[it][:, :N], pr[ir][:, :N], sin_t[:, t0:t0 + N], ALU.mult, [Bpr[ir], Bc], [Bt2[it]])
                tt("pool", ob[io][:, :N], t1[it][:, :N], t2[it][:, :N], ALU.add, [Bt1[it], Bt2[it]], [Bob[io]])
                ld("pool", dst, ob[io][:, :N], [], [Bob[io]])
            deferred.append(fin)

        def evac(kind, iz, N, t0, dst, rope):
            if kind in ("silu", "sig"):
                io = nxt("ob", 4)
                act(ob[io][:, :N], pz[iz][:, :N], AF.Silu if kind == "silu" else AF.Sigmoid, [Bpz[iz]], [Bob[io]])
                ld("pool", dst, ob[io][:, :N], [], [Bob[io]])
                return
            if kind[0] == "r":
                iq = nxt("qn", 3)
                sc = (128.0 ** -0.5) if kind == "rk" else 1.0
                act(qnb[iq][:, :N], pz[iz][:, :N], AF.Copy, [Bpz[iz]], [Bqn[iq]], scale=sc)
                if rope:
                    deferred.append(lambda: rope_and_store(iq, N, t0, dst))
                else:
                    ld("pool", dst, qnb[iq][:, :N], [], [Bqn[iq]])
                return
            gt = qn_t if kind == "nq" else kn_t
            isq = nxt("sq", 2)
            act(sqb[isq][:, :N], pz[iz][:, :N], AF.Square, [Bpz[iz]], [Bsq[isq]])

            def st2():
                ip = nxt("q", 2)
                mm(pq[ip][:, :N], ones_b[:], sqb[isq][:, :N], True, True, [Bsq[isq], B_const], [Bpq[ip]])
                isd = nxt("sd", 2)
                act(sd[isd][:, :N], pq[ip][:, :N], AF.Sqrt, [Bpq[ip], B_const], [Bsd[isd]], bias=eps_t[:], scale=1.0 / 128)
                kb.op("dve", lambda: nc.vector.reciprocal(out=sd[isd][:, :N], in_=sd[isd][:, :N]), [], [Bsd[isd]])
                iq = nxt("qn", 3)
                stt(qnb[iq][:, :N], pz[iz][:, :N], gt[:], sd[isd][:, :N], ALU.mult, ALU.mult, [Bpz[iz], Bsd[isd], Bc], [Bqn[iq]])
                if rope:
                    deferred.append(lambda: rope_and_store(iq, N, t0, dst))
                else:
                    ld("pool", dst, qnb[iq][:, :N], [], [Bqn[iq]])
            deferred.append(st2)

        groups = []
        for g in range(2):
            groups.append((g * 512, 512, "nq", qaT_d, g * 4, False))
        groups.append((1024, 512, "rq", qrT_d, 0, False))
        for g in range(2):
            groups.append((1536 + g * 512, 512, "silu", sgT_d, g * 4, False))
        for g in range(4):
            groups.append((2560 + g * 512, 512, "sig", gaT_d, g * 4, False))
        for g in range(4):
            groups.append((4608 + g * 512, 512, "sig", grT_d, g * 4, False))
        groups.append((-1, 768, "k", None, 0, True))
        for (c0, ncol, kind, dstT, h0, kv) in groups:
            iw = nxt("w", 2)
            if kind == "k":
                for hh in range(2):
                    ld("pool", Wt[iw][:, hh * 8:(hh + 1) * 8, 0:256], wv[:, hh * 8:(hh + 1) * 8, 6656:6912], [BW[iw]])
                    ld("pool", Wt[iw][:, hh * 8:(hh + 1) * 8, 256:768], wv[:, hh * 8:(hh + 1) * 8, 7168:7680], [BW[iw]])
            else:
                for hh in range(2):
                    ld("pool", Wt[iw][:, hh * 8:(hh + 1) * 8, 0:512], wv[:, hh * 8:(hh + 1) * 8, c0:c0 + 512], [BW[iw]])
            blks = range(0, 9) if kv else range(1, 9)
            for blk in blks:
                ih = nxt("h", 2)
                tk0 = 0 if blk == 0 else LC + (blk - 1) * 512
                N = LC if blk == 0 else 512
                for hh in range(2):
                    ld("sp", hB[ih][:, hh * 8:(hh + 1) * 8, :N], hT_d[:, hh * 8:(hh + 1) * 8, tk0:tk0 + N], [BhB[ih]])
                for ch in range(ncol // 128):
                    iz = nxt("z", 4)
                    for c in range(NCH):
                        mm(pz[iz][:, :N], Wt[iw][:, c, ch * 128:(ch + 1) * 128], hB[ih][:, c, :N], c == 0, c == NCH - 1, [BW[iw], BhB[ih]], [Bpz[iz]])
                    run_deferred()
                    t0 = tk0 - LC
                    if kind == "k":
                        if ch < 2:
                            evac("nk", iz, N, t0, kaT_d[ch, :, tk0:tk0 + N], rope=(blk > 0))
                        else:
                            evac("rk", iz, N, t0, krT_d[ch - 2, :, tk0:tk0 + N], rope=(blk > 0))
                    else:
                        evac(kind, iz, N, t0, dstT[h0 + ch, :, t0:t0 + N], rope=True)
        run_deferred(); run_deferred(); run_deferred()
        pv = pz
        vo = [ph.sb(f"vo{i}", [128, 512], BF16) for i in range(2)]; Bvo = [Buf(), Buf()]
        vgroups = [(6912, 256, va_d, 0), (7680, 512, vr_d, 0), (8192, 512, vr_d, 512)]
        for (c0, ncol, dstT, dc0) in vgroups:
            iw = nxt("w", 2)
            for hh in range(2):
                ld("pool", Wt[iw][:, hh * 8:(hh + 1) * 8, 0:ncol], wv[:, hh * 8:(hh + 1) * 8, c0:c0 + ncol], [BW[iw]])
            for blk in range(0, 9):
                ih = nxt("h", 2)
                tk0 = 0 if blk == 0 else LC + (blk - 1) * 512
                N = LC if blk == 0 else 512
                for hh in range(2):
                    ld("sp", hB[ih][:, hh * 8:(hh + 1) * 8, :N], hT_d[:, hh * 8:(hh + 1) * 8, tk0:tk0 + N], [BhB[ih]])
                for tl in range(N // 128):
                    iz = nxt("z", 4)
                    for c in range(NCH):
                        mm(pv[iz][:, :ncol], hB[ih][:, c, tl * 128:(tl + 1) * 128], Wt[iw][:, c, :ncol], c == 0, c == NCH - 1, [BW[iw], BhB[ih]], [Bpz[iz]])
                    io = nxt("ob", 2)
                    if tl % 2 == 0:
                        cp("act", vo[io][:, :ncol], pv[iz][:, :ncol], [Bpz[iz]], [Bvo[io]])
                    else:
                        cp("dve", vo[io][:, :ncol], pv[iz][:, :ncol], [Bpz[iz]], [Bvo[io]])
                    r0 = tk0 + tl * 128
                    ld("pool", dstT[r0:r0 + 128, dc0:dc0 + ncol], vo[io][:, :ncol], [], [Bvo[io]])
    if stop_after <= 2:
        kb.finish(); return nc

    oaT_d = dscr("oaT_d", [8, 128, T], BF16)
    orT_d = dscr("orT_d", [8, 128, T], BF16)

    ones_f = G.sb("ones_f", [128, 128], F32)
    kb.op("dve", lambda: nc.vector.memset(ones_f[:], 1.0), writes=[B_const])
    kb.barrier()
    with Phase(kb) as ph:
        KT = ph.sb("KT", [128, TK], BF16); BKT = Buf()
        Vt = ph.sb("Vt", [128, TK // 128, 128], BF16); BVt = Buf()
        QT = [ph.sb(f"QT{i}", [128, 512], BF16) for i in range(2)]; BQT = [Buf(), Buf()]
        NS = 4
        ps_s = [ph.ps(f"ps_s{i}", [128, 512], F32) for i in range(NS)]; Bps = [Buf() for _ in range(NS)]
        po = [ph.ps(f"po{i}", [128, 512], F32) for i in range(2)]; Bpo = [Buf(), Buf()]
        pd = [ph.ps(f"pd{i}", [128, 512], F32) for i in range(2)]; Bpd = [Buf(), Buf()]
        pT = [ph.sb(f"pT{i}", [128, 512], BF16) for i in range(NS)]; BpT = [Buf() for _ in range(NS)]
        accE = [ph.sb(f"accE{i}", [128, 512], F32) for i in range(2)]; BaE = [Buf(), Buf()]
        accO = [ph.sb(f"accO{i}", [128, 512], F32) for i in range(2)]; BaO = [Buf(), Buf()]
        rd = [ph.sb(f"rd{i}", [128, 512], F32) for i in range(2)]; Brd = [Buf(), Buf()]
        oo = [ph.sb(f"oo{i}", [128, 512], BF16) for i in range(2)]; Boo = [Buf(), Buf()]
        NKT = TK // 128
        it = 0
        sidx = 0
        for g in range(0 if not SKIP3 else 2, 2):
            ld("sp", KT[:], kaT_d[g, :, :], [BKT])
            ld("sp", Vt[:], va_d[:, g * 128:(g + 1) * 128].rearrange("(n p) d -> p n d", p=128), [BVt])
            for j in range(4):
                h = g * 4 + j
                for qb in range(T // 512):
                    iq = it % 2; it += 1
                    t0 = qb * 512
                    ld("sp", QT[iq][:], qaT_d[h, :, t0:t0 + 512], [BQT[iq]])
                    s_of = {}

                    def issue_s(kt):
                        nonlocal sidx
                        s_of[kt] = sidx % NS; sidx += 1
                        sn = s_of[kt]
                        mm(ps_s[sn][:], KT[:, kt * 128:(kt + 1) * 128], QT[iq][:], True, True, [BKT, BQT[iq]], [Bps[sn]])
                    issue_s(0); issue_s(1)
                    for kt in range(NKT):
                        if kt + 2 < NKT:
                            issue_s(kt + 2)
                        sc_ = s_of[kt]
                        act(pT[sc_][:], ps_s[sc_][:], AF.Exp, [Bps[sc_]], [BpT[sc_]], scale=128.0 ** -0.5)
                        mm(po[iq][:], Vt[:, kt, :], pT[sc_][:], kt == 0, kt == NKT - 1, [BVt, BpT[sc_]], [Bpo[iq]])
                        if kt % 2 == 0:
                            mm(pd[iq][:], ones_b[:], pT[sc_][:], kt == 0, False, [B_const, BpT[sc_]], [Bpd[iq]])
                        else:
                            if kt == 1:
                                cp("dve", accO[iq][:], pT[sc_][:], [BpT[sc_]], [BaO[iq]])
                            else:
                                tt("dve", accO[iq][:], accO[iq][:], pT[sc_][:], ALU.add, [BpT[sc_]], [BaO[iq]])
                    mm(pd[iq][:], ones_f[:], accO[iq][:], False, True, [B_const, BaO[iq]], [Bpd[iq]])
                    kb.op("dve", lambda iq=iq: nc.vector.reciprocal(out=rd[iq][:], in_=pd[iq][:]), [Bpd[iq]], [Brd[iq]])
                    tt("dve", oo[iq][:], po[iq][:], rd[iq][:], ALU.mult, [Bpo[iq], Brd[iq]], [Boo[iq]])
                    ld("pool", oaT_d[h, :, t0:t0 + 512], oo[iq][:], [], [Boo[iq]])
    if stop_after <= 3:
        kb.finish(); return nc

    rdec_in = din("ret_decay", [1, 8])
    gn_in = din("ret_gn", [128, 8])
    tabs_in = din("ret_tabs", [6, 128, 128])
    pcols_in = din("ret_pcols", [128, 6])
    with Phase(kb) as ph:
        Bt = Buf()
        lg = ph.sb("lg", [128, 8], F32)
        tabs = ph.sb("tabs", [128, 6, 128], F32)
        pcols = ph.sb("pcols", [128, 6], F32)
        gn_t = ph.sb("gn_t", [128, 8], F32)
        ld("sp", lg[:], rdec_in[0:1, :].partition_broadcast(128), [Bt])
        ld("sp", tabs[:], tabs_in.rearrange("k p i -> p k i"), [Bt])
        ld("sp", pcols[:], pcols_in[:, :], [Bt])
        ld("sp", gn_t[:], gn_in[:, :], [Bt])
        act(lg[:], lg[:], AF.Exp, [], [Bt])
        act(lg[:], lg[:], AF.Ln, [], [Bt], bias=1.0)
        ts("dve", lg[:], lg[:], -1.0, None, ALU.mult, None, [], [Bt])
        kT = ph.sb("kT", [128, TK], BF16); BkT = Buf()
        qT = ph.sb("qT", [128, T], BF16); BqT = Buf()
        V = ph.sb("V", [128, TK // 128, 256], BF16); BV = Buf()
        sg = ph.sb("sg", [128, 2, T], BF16); Bsg = Buf()
        wq = ph.sb("wq", [128, 2, 128], F32); Bwq = Buf()
        wk = ph.sb("wk", [128, 6], F32); Bwk = Buf()
        gch = ph.sb("gch", [128, 2], F32); Bgch = Buf()
        MT = ph.sb("MT", [128, 128], F32); BMT = Buf()
        mtmp = ph.sb("mtmp", [128, 2, 128], F32); Bmtmp = Buf()
        kw = ph.sb("kw", [128, TK // 128, 2, 128], BF16); Bkw = Buf()
        S32 = [ph.sb(f"S32{i}", [128, 256], F32) for i in range(2)]; BS32 = [Buf(), Buf()]
        SB = [ph.sb(f"SB{i}", [128, 32, 256], BF16) for i in range(2)]; BSB = [Buf(), Buf()]
        ptk_ = ph.ps("ptk", [128, 2, 128], BF16); ptk = [ptk_[:, 0, :], ptk_[:, 0, :]]; _b = Buf(); Bptk = [_b, _b]
        pu = [ph.ps(f"pu{i}", [128, 256], F32) for i in range(2)]; Bpu = [Buf(), Buf()]
        pS_ = ph.ps("pS", [128, 2, 128], F32); pS = [pS_[:, 0, :], pS_[:, 0, :]]; _b2 = Buf(); BpS = [_b2, _b2]
        pO = [ph.ps(f"pO{i}", [128, 256], F32) for i in range(2)]; BpO = [Buf(), Buf()]
        pT2 = [ph.ps(f"pT2{i}", [128, 2, 128], BF16) for i in range(2)]; BpT2 = [Buf(), Buf()]
        Sm = [ph.sb(f"Sm{i}", [128, 128], BF16) for i in range(2)]; BSm = [Buf(), Buf()]
        qw = [ph.sb(f"qw{i}", [128, 2, 128], BF16) for i in range(2)]; Bqw = [Buf(), Buf()]
        bst = [ph.sb(f"bst{i}", [128, 6], F32) for i in range(2)]; Bbst = [Buf(), Buf()]
        mv = [ph.sb(f"mv{i}", [128, 2], F32) for i in range(2)]; Bmv = [Buf(), Buf()]
        rs_ = [ph.sb(f"rs{i}", [128, 1], F32) for i in range(2)]; Brs = [Buf(), Buf()]
        on = [ph.sb(f"on{i}", [128, 256], BF16) for i in range(2)]; Bon = [Buf(), Buf()]
        orb = [ph.sb(f"orb{i}", [128, 2, 512], BF16) for i in range(2)]; Borb = [Buf(), Buf()]
        NKT = TK // 128
        iu = 0
        for h in range(4):
            fcol = lg[:, h:h + 1]; bcol = lg[:, 4 + h:5 + h]
            ld("sp", kT[:], krT_d[h, :, :], [BkT])
            ld("sp", qT[:], qrT_d[h, :, :], [BqT])
            ld("sp", V[:], vr_d[:, h * 256:(h + 1) * 256].rearrange("(n p) d -> p n d", p=128), [BV])
            ld("sp", sg[:], sgT_d[2 * h:2 * h + 2, :, :].rearrange("c p t -> p c t"), [Bsg])
            act(wq[:, 0, :], tabs[:, 0, :], AF.Exp, [Bt], [Bwq], scale=fcol)
            act(wq[:, 1, :], tabs[:, 1, :], AF.Exp, [Bt], [Bwq], scale=bcol)
            for k in range(6):
                act(wk[:, k:k + 1], pcols[:, k:k + 1], AF.Exp, [Bt], [Bwk], scale=(fcol if k in (0, 2, 3) else bcol))
            act(gch[:, 0:1], fcol, AF.Exp, [Bt], [Bgch], scale=128.0)
            act(gch[:, 1:2], bcol, AF.Exp, [Bt], [Bgch], scale=128.0)
            act(mtmp[:, 0, :], tabs[:, 2, :], AF.Exp, [Bt], [Bmtmp], scale=fcol)
            act(mtmp[:, 1, :], tabs[:, 4, :], AF.Exp, [Bt], [Bmtmp], scale=bcol)
            tt("dve", mtmp[:, 0, :], mtmp[:, 0, :], tabs[:, 3, :], ALU.mult, [Bt], [Bmtmp])
            tt("dve", mtmp[:, 1, :], mtmp[:, 1, :], tabs[:, 5, :], ALU.mult, [Bt], [Bmtmp])
            tt("dve", MT[:], mtmp[:, 0, :], mtmp[:, 1, :], ALU.add, [Bmtmp], [BMT])
            if P4STOP == 1: break
            for n in range(NKT):
                ip = n % 2
                tr(ptk[ip], kT[:, n * 128:(n + 1) * 128], ident_b[:], [BkT, B_const], [Bptk[ip]])
                if n == 0:
                    cf, cb = 2, 4
                elif n == 1:
                    cf, cb = 3, 5
                else:
                    cf, cb = 0, 1
                if P4VAR != 1:
                    ts("dve", kw[:, n, 0, :], ptk[ip], wk[:, cf:cf + 1], None, ALU.mult, None, [Bptk[ip], Bwk], [Bkw])
                if P4VAR == 0:
                    act(kw[:, n, 1, :], ptk[ip], AF.Copy, [Bptk[ip], Bwk], [Bkw], scale=wk[:, cb:cb + 1])
                if P4VAR in (1, 2):
                    ts("dve", kw[:, n, 1, :], ptk[ip], wk[:, cb:cb + 1], None, ALU.mult, None, [Bptk[ip], Bwk], [Bkw])
            if P4STOP == 2: break
            for d_ in range(2):
                i0 = iu % 2; iu += 1
                mm(pu[i0][:], kw[:, 0, d_, :], V[:, 0, :], True, False, [Bkw, BV], [Bpu[i0]])
                mm(pu[i0][:], kw[:, 1, d_, :], V[:, 1, :], False, True, [Bkw, BV], [Bpu[i0]])
                cp("dve", S32[d_][:], pu[i0][:], [Bpu[i0]], [BS32[d_]])
                first = 0 if d_ == 0 else 31
                cp("act", SB[d_][:, first, :], S32[d_][:], [BS32[d_]], [BSB[d_]])
                order = range(0, 31) if d_ == 0 else range(31, 0, -1)
                for c in order:
                    i0 = iu % 2; iu += 1
                    mm(pu[i0][:], kw[:, 2 + c, d_, :], V[:, 2 + c, :], True, True, [Bkw, BV], [Bpu[i0]])
                    stt(S32[d_][:], S32[d_][:], gch[:, d_:d_ + 1], pu[i0][:], ALU.mult, ALU.add, [Bpu[i0], Bgch], [BS32[d_]])
                    nxtc = c + 1 if d_ == 0 else c - 1
                    cp("act", SB[d_][:, nxtc, :], S32[d_][:], [BS32[d_]], [BSB[d_]])
            if P4STOP == 3: break
            for c in range(32):
                i2 = c % 2
                t0 = c * 128
                mm(pS[i2], kT[:, LC + t0:LC + t0 + 128], qT[:, t0:t0 + 128], True, True, [BkT, BqT], [BpS[i2]])
                tt("dve", Sm[i2][:], pS[i2], MT[:], ALU.mult, [BpS[i2], BMT], [BSm[i2]])
                tt("pool", qw[i2][:, 0, :], qT[:, t0:t0 + 128], wq[:, 0, :], ALU.mult, [BqT, Bwq], [Bqw[i2]])
                tt("pool", qw[i2][:, 1, :], qT[:, t0:t0 + 128], wq[:, 1, :], ALU.mult, [BqT, Bwq], [Bqw[i2]])
                mm(pO[i2][:], Sm[i2][:], V[:, 2 + c, :], True, False, [BSm[i2], BV], [BpO[i2]])
                mm(pO[i2][:], qw[i2][:, 0, :], SB[0][:, c, :], False, False, [Bqw[i2], BSB[0]], [BpO[i2]])
                mm(pO[i2][:], qw[i2][:, 1, :], SB[1][:, c, :], False, True, [Bqw[i2], BSB[1]], [BpO[i2]])
                kb.op("dve", lambda i2=i2: nc.vector.bn_stats(out=bst[i2][:], in_=pO[i2][:]), [BpO[i2]], [Bbst[i2]])
                kb.op("dve", lambda i2=i2: nc.vector.bn_aggr(out=mv[i2][:], in_=bst[i2][:]), [Bbst[i2]], [Bmv[i2]])
                rstd_from(rs_[i2][:], mv[i2][:, 1:2], 1.0, [Bmv[i2]], [Brs[i2]])
                ts("dve", on[i2][:], pO[i2][:], mv[i2][:, 0:1], rs_[i2][:], ALU.subtract, ALU.mult, [BpO[i2], Bmv[i2], Brs[i2]], [Bon[i2]])
                io = (c // 4) % 2
                for k in range(2):
                    tr(pT2[i2][:, k, :], on[i2][:, k * 128:(k + 1) * 128], ident_b[:], [Bon[i2], B_const], [BpT2[i2]])
                for k in range(2):
                    stt(orb[io][:, k, (c % 4) * 128:(c % 4 + 1) * 128], pT2[i2][:, k, :], gn_t[:, 2 * h + k:2 * h + k + 1], sg[:, k, t0:t0 + 128],
                        ALU.mult, ALU.mult, [BpT2[i2], Bt, Bsg], [Borb[io]])
                if c % 4 == 3:
                    tb = (c // 4) * 512
                    ld("pool", orT_d[2 * h:2 * h + 2, :, tb:tb + 512].rearrange("c p t -> p c t"), orb[io][:], [], [Borb[io]])
    if stop_after <= 4:
        kb.finish(); return nc

    w_oa_in = din("w_o_att", [1024, D]); w_or_in = din("w_o_ret", [1024, D]); w_out_in = din("w_out", [D, D])
    w_r_in = din("w_router", [D, 16])
    yT_d = dscr("yT_d", [128, NCH, T], BF16)
    x1_d = dscr("x1_d", [T, D], F32)
    h2_d = dscr("h2_d", [T, D], BF16)
    AFF = G.sb("AFF", [128, 32, 16], F32); BAFF = Buf()
    with Phase(kb) as ph:
        Woa = ph.sb("Woa", [128, 8, D], BF16); Wor = ph.sb("Wor", [128, 8, D], BF16); BWo = Buf()
        for hh in range(4):
            ld("pool", Woa[:, hh * 2:(hh + 1) * 2, :], w_oa_in.rearrange("(c p) n -> p c n", p=128)[:, hh * 2:(hh + 1) * 2, :], [BWo])
            ld("pool", Wor[:, hh * 2:(hh + 1) * 2, :], w_or_in.rearrange("(c p) n -> p c n", p=128)[:, hh * 2:(hh + 1) * 2, :], [BWo])
        oa = [ph.sb(f"oa{i}", [128, 8, 512], BF16) for i in range(2)]; Boa = [Buf(), Buf()]
        orr = [ph.sb(f"orr{i}", [128, 8, 512], BF16) for i in range(2)]; Borr = [Buf(), Buf()]
        ga = [ph.sb(f"ga{i}", [128, 16, 512], BF16) for i in range(2)]; Bga = [Buf(), Buf()]
        gr = [ph.sb(f"gr{i}", [128, 16, 512], BF16) for i in range(2)]; Bgr = [Buf(), Buf()]
        pA = [ph.ps(f"pA{i}", [128, 512], F32) for i in range(2)]; BpA = [Buf(), Buf()]
        pB = [ph.ps(f"pB{i}", [128, 512], F32) for i in range(2)]; BpB = [Buf(), Buf()]
        ta = [ph.sb(f"ta{i}", [128, 512], F32) for i in range(2)]; Bta = [Buf(), Buf()]
        tb_ = [ph.sb(f"tb{i}", [128, 512], F32) for i in range(2)]; Btb = [Buf(), Buf()]
        yTb = [ph.sb(f"yTb{i}", [128, NCH, 512], BF16) for i in range(2)]; ByT = [Buf(), Buf()]
        k = 0
        for blk in range(T // 512):
            ib = blk % 2
            t0 = blk * 512
            ld("sp", oa[ib][:], oaT_d[:, :, t0:t0 + 512].rearrange("c p t -> p c t"), [Boa[ib]])
            ld("sp", orr[ib][:], orT_d[:, :, t0:t0 + 512].rearrange("c p t -> p c t"), [Borr[ib]])
            ld("sp", ga[ib][:], gaT_d[:, :, t0:t0 + 512].rearrange("c p t -> p c t"), [Bga[ib]])
            ld("sp", gr[ib][:], grT_d[:, :, t0:t0 + 512].rearrange("c p t -> p c t"), [Bgr[ib]])
            for dm in range(16):
                i2 = k % 2; k += 1
                for f in range(8):
                    mm(pA[i2][:], Woa[:, f, dm * 128:(dm + 1) * 128], oa[ib][:, f, :], f == 0, f == 7, [BWo, Boa[ib]], [BpA[i2]])
                for f in range(8):
                    mm(pB[i2][:], Wor[:, f, dm * 128:(dm + 1) * 128], orr[ib][:, f, :], f == 0, f == 7, [BWo, Borr[ib]], [BpB[i2]])
                tt("dve", ta[i2][:], pA[i2][:], ga[ib][:, dm, :], ALU.mult, [BpA[i2], Bga[ib]], [Bta[i2]])
                tt("dve", tb_[i2][:], pB[i2][:], gr[ib][:, dm, :], ALU.mult, [BpB[i2], Bgr[ib]], [Btb[i2]])
                tt("pool", yTb[ib][:, dm, :], ta[i2][:], tb_[i2][:], ALU.add, [Bta[i2], Btb[i2]], [ByT[ib]])
            ld("pool", yT_d[:, :, t0:t0 + 512], yTb[ib][:], [], [ByT[ib]])
    if stop_after <= 5:
        kb.finish(); return nc

    with Phase(kb) as ph:
        Wo = ph.sb("Wo", [128, NCH, D], BF16); BWo2 = Buf()
        for hh in range(8):
            ld("pool", Wo[:, hh * 2:(hh + 1) * 2, :], w_out_in.rearrange("(c p) n -> p c n", p=128)[:, hh * 2:(hh + 1) * 2, :], [BWo2])
        wr = ph.sb("wr", [128, NCH, 16], F32); Bwr = Buf()
        ld("sp", wr[:], w_r_in.rearrange("(c p) e -> p c e", p=128), [Bwr])
        gt1, Bgt1 = bcast_row(ph, "gt1", mod_d[0:1, 2 * D:3 * D])
        po1, Bpo1 = bcast_row(ph, "po1", post1[0:1, :])
        tt("dve", gt1[:], gt1[:], po1[:], ALU.mult, [Bpo1], [Bgt1])
        g2, Bg2 = bcast_row(ph, "g2", mod_d[0:1, 4 * D:5 * D])
        ld("sp", po1[:], pre2[0:1, :].partition_broadcast(128), [Bpo1])
        stt(g2[:], g2[:], 1.0, po1[:], ALU.add, ALU.mult, [Bpo1], [Bg2])
        s2, Bs2 = bcast_row(ph, "s2", mod_d[0:1, 3 * D:4 * D])
        yTt = [ph.sb(f"yTt{i}", [128, NCH, 128], BF16) for i in range(3)]; ByTt = [Buf() for _ in range(3)]
        xt = [ph.sb(f"xt{i}", [128, D], F32) for i in range(3)]; Bxt = [Buf() for _ in range(3)]
        pY = [ph.ps(f"pY{i}", [128, 512], F32) for i in range(4)]; BpY = [Buf() for _ in range(4)]
        ptr = [ph.ps(f"ptr{i}", [128, 4, 128], F32) for i in range(2)]; Bptr = [Buf(), Buf()]
        pL = ph.ps("pL", [128, 16], F32); BpL = Buf()
        Ysb = [ph.sb(f"Ysb{i}", [128, D], F32) for i in range(2)]; BY = [Buf(), Buf()]
        junk = ph.sb("junk", [128, D], BF16); Bjunk = Buf()
        sq = [ph.sb(f"sq5{i}", [128, 4], F32) for i in range(2)]; Bsq5 = [Buf(), Buf()]
        x1t = [ph.sb(f"x1t{i}", [128, D], F32) for i in range(2)]; Bx1 = [Buf(), Buf()]
        h2f = [ph.sb(f"h2f{i}", [128, D], F32) for i in range(2)]; Bh2f = [Buf(), Buf()]
        h2b = [ph.sb(f"h2b{i}", [128, D], BF16) for i in range(2)]; Bh2b = [Buf(), Buf()]
        h2T = ph.sb("h2T", [128, NCH, 128], F32); Bh2T = Buf()
        lgt = [ph.sb(f"lgt{i}", [128, 16], F32) for i in range(2)]; Blgt = [Buf(), Buf()]
        NT5 = T // 128

        def ld5(i):
            if i < NT5:
                t0_ = i * 128
                ld("sp", yTt[i % 3][:], yT_d[:, :, t0_:t0_ + 128], [ByTt[i % 3]])
                ld("sp", xt[i % 3][:], xk[LC + t0_:LC + t0_ + 128, :], [Bxt[i % 3]])
        ld5(0); ld5(1)

        def s5A(i):
            s = i % 2; s3 = i % 3
            t0 = i * 128
            ld5(i + 2)
            for db in range(4):
                for c in range(NCH):
                    mm(pY[db][:], yTt[s3][:, c, :], Wo[:, c, db * 512:(db + 1) * 512], c == 0, c == NCH - 1, [ByTt[s3], BWo2], [BpY[db]])
            for db in range(4):
                cp("act", Ysb[s][:, db * 512:(db + 1) * 512], pY[db][:], [BpY[db]], [BY[s]])
            act(junk[:], Ysb[s][:], AF.Square, [BY[s]], [Bjunk, Bsq5[s]], accum_out=sq[s][:, 0:1])
            rstd_from(sq[s][:, 1:2], sq[s][:, 0:1], D, [], [Bsq5[s]])
            stt(Ysb[s][:], Ysb[s][:], sq[s][:, 1:2], gt1[:], ALU.mult, ALU.mult, [Bsq5[s], Bgt1], [BY[s]])
            tt("dve", x1t[s][:], Ysb[s][:], xt[s3][:], ALU.add, [BY[s], Bxt[s3]], [Bx1[s]])
            ld("pool", x1_d[t0:t0 + 128, :], x1t[s][:], [], [Bx1[s]])

        def s5B(i):
            s = i % 2
            t0 = i * 128
            act(junk[:], x1t[s][:], AF.Square, [Bx1[s]], [Bjunk, Bsq5[s]], accum_out=sq[s][:, 2:3])
            rstd_from(sq[s][:, 3:4], sq[s][:, 2:3], D, [], [Bsq5[s]])
            stt(h2f[s][:], x1t[s][:], sq[s][:, 3:4], g2[:], ALU.mult, ALU.mult, [Bx1[s], Bsq5[s], Bg2], [Bh2f[s]])
            tt("dve", h2f[s][:], h2f[s][:], s2[:], ALU.add, [Bs2], [Bh2f[s]])

        def s5C(i):
            s = i % 2
            t0 = i * 128
            cp("pool", h2b[s][:], h2f[s][:], [Bh2f[s]], [Bh2b[s]])
            ld("pool", h2_d[t0:t0 + 128, :], h2b[s][:], [], [Bh2b[s]])
            for q4 in range(4):
                ip = q4 % 2
                for c in range(4):
                    cc = q4 * 4 + c
                    tr(ptr[ip][:, c, :], h2f[s][:, cc * 128:(cc + 1) * 128], ident_f[:], [Bh2f[s], B_const], [Bptr[ip]])
                cp("dve" if q4 % 2 == 0 else "act", h2T[:, q4 * 4:(q4 + 1) * 4, :], ptr[ip][:], [Bptr[ip]], [Bh2T])
            for c in range(NCH):
                mm(pL[:], h2T[:, c, :], wr[:, c, :], c == 0, c == NCH - 1, [Bh2T, Bwr], [BpL])
            cp("dve", AFF[:, i, :], pL[:], [BpL], [BAFF])

        s5A(0)
        for i in range(NT5):
            if i + 1 < NT5:
                s5A(i + 1)
            s5B(i)
            s5C(i)
        mx = ph.sb("mx", [128, 32], F32); Bmx = Buf()
        kb.op("dve", lambda: nc.vector.tensor_reduce(out=mx[:], in_=AFF[:], axis=AX.X, op=ALU.max), [BAFF], [Bmx])
        tt("dve", AFF[:], AFF[:], mx[:].unsqueeze(2).to_broadcast([128, 32, 16]), ALU.subtract, [Bmx], [BAFF])
        act(AFF[:], AFF[:], AF.Exp, [], [BAFF])
        kb.op("dve", lambda: nc.vector.tensor_reduce(out=mx[:], in_=AFF[:], axis=AX.X, op=ALU.add), [BAFF], [Bmx])
        kb.op("dve", lambda: nc.vector.reciprocal(out=mx[:], in_=mx[:]), [], [Bmx])
        tt("dve", AFF[:], AFF[:], mx[:].unsqueeze(2).to_broadcast([128, 32, 16]), ALU.mult, [Bmx], [BAFF])
        if "aff_d" in dbg:
            aff_d = dscr("aff_d", [128, 32, 16], F32)
            ld("sp", aff_d[:, :, :], AFF[:], [], [BAFF])
    if stop_after <= 6:
        kb.finish(); return nc

    NE = NEXP
    wg_in = din("w_gate", [NE, D, D]); wu_in = din("w_up", [NE, D, D]); wd_in = din("w_down", [NE, D, D])
    iota_in = din("iota512", [128, 512]); tri_in = din("tri", [128, 128]); tvc_in = din("tvc", [128, 32, 2])
    y2_d = dscr("y2_d", [T, D], F32)
    By2 = Buf()
    pos = G.sb("pos", [128, 32, 16], F32); maskf = G.sb("maskf", [128, 32, 16], F32)
    affh = G.sb("affh", [128, 32, 16], BF16); affl = G.sb("affl", [128, 32, 16], BF16)
    Bsel = Buf()
    with Phase(kb) as ph:
        zt = ph.sb("zt", [128, D], F32); Bz = Buf()
        kb.op("pool", lambda: nc.gpsimd.memset(zt[:], 0.0), [], [Bz])
        for i in range(T // 128):
            kb.dma("sp", lambda i=i: nc.sync.dma_start(out=y2_d[i * 128:(i + 1) * 128, :], in_=zt[:]), [Bz], [])
        lo = ph.sb("lo", [128, 16], F32); hi = ph.sb("hi", [128, 16], F32); mid = ph.sb("mid", [128, 16], F32); Bl = Buf()
        cmpb = ph.sb("cmpb", [128, 32, 16], BF16); Bcmp = Buf()
        partb = ph.sb("partb", [128, 16], BF16); Bpart = Buf()
        selu = ph.sb("selu", [128, 2, 16], U32); Bsu = Buf()
        tri_b = ph.sb("tri_b", [128, 128], BF16); Btri = Buf()
        ld("pool", tri_b[:], tri_in[:, :], [Btri])
        pc = ph.ps("pc", [128, 16], F32); Bpc = Buf()
        pcs = ph.ps("pcs", [128, 512], F32); Bpcs = Buf()
        pw = ph.ps("pw", [128, 512], F32); Bpw = Buf()
        maskb = ph.sb("maskb", [128, 32, 16], BF16); Bmb = Buf()
        tcum = ph.sb("tcum", [128, 32, 16], F32); Btc = Buf()
        kb.op("dve", lambda: nc.vector.memset(lo[:], 0.0), [], [Bl])
        kb.op("dve", lambda: nc.vector.memset(hi[:], 1.0), [], [Bl])
        for it in range(NBISECT):
            tt("dve", mid[:], lo[:], hi[:], ALU.add, [], [Bl])
            ts("dve", mid[:], mid[:], 0.5, None, ALU.mult, None, [], [Bl])
            tt("dve", cmpb[:], AFF[:], mid[:].unsqueeze(1).to_broadcast([128, 32, 16]), ALU.is_ge, [BAFF, Bl], [Bcmp])
            with nc.allow_low_precision(reason="exact small integer counts"):
                kb.op("dve", lambda: nc.vector.tensor_reduce(out=partb[:], in_=cmpb[:].rearrange("p t e -> p e t"), axis=AX.X, op=ALU.add), [Bcmp], [Bpart])
            mm(pc[:], ones_b[:], partb[:], True, True, [Bpart, B_const], [Bpc])
            ts("dve", selu[:, 0, :], pc[:], 511.5, None, ALU.is_ge, None, [Bpc], [Bsu])
            ts("dve", selu[:, 1, :], pc[:], 511.5, None, ALU.is_lt, None, [Bpc], [Bsu])
            kb.op("dve", lambda: nc.vector.copy_predicated(out=lo[:], mask=selu[:, 0, :], data=mid[:]), [Bsu], [Bl])
            kb.op("dve", lambda: nc.vector.copy_predicated(out=hi[:], mask=selu[:, 1, :], data=mid[:]), [Bsu], [Bl])
        tt("dve", maskb[:], AFF[:], lo[:].unsqueeze(1).to_broadcast([128, 32, 16]), ALU.is_ge, [BAFF, Bl], [Bmb])
        cp("dve", maskf[:], maskb[:], [Bmb], [Bsel])
        mbf = maskb[:].rearrange("p t e -> p (t e)")
        mm(pcs[:], ones_b[:], mbf, True, True, [Bmb, B_const], [Bpcs])
        mm(pw[:], tri_b[:], mbf, True, True, [Bmb, Btri], [Bpw])
        kb.op("dve", lambda: nc.vector.memset(tcum[:, 0, :], 0.0), [], [Btc])
        for t in range(1, 32):
            tt("dve", tcum[:, t, :], tcum[:, t - 1, :], pcs[:, (t - 1) * 16:t * 16], ALU.add, [Bpcs], [Btc])
        tt("dve", pos[:].rearrange("p t e -> p (t e)"), pw[:], tcum[:].rearrange("p t e -> p (t e)"), ALU.add, [Bpw, Btc], [Bsel])
        cp("dve", affh[:], AFF[:], [BAFF], [Bsel])
        tt("dve", affl[:], AFF[:], affh[:], ALU.subtract, [BAFF], [Bsel])
        if "sel_d" in dbg:
            sel_d = dscr("sel_d", [2, 128, 32, 16], F32)
            ld("sp", sel_d[0], pos[:], [], [Bsel]); ld("sp", sel_d[1], maskf[:], [], [Bsel])
    if stop_after <= 7:
        kb.finish(); return nc

    with Phase(kb) as ph:
        iota = ph.sb("iota", [128, 512], F32); Bio = Buf()
        ld("sp", iota[:], iota_in[:, :], [Bio])
        tv = [ph.sb(f"tv{i}", [128, 32, 4], BF16) for i in range(2)]; Btv = [Buf(), Buf()]
        for i in range(2):
            ld("pool", tv[i][:, :, 0:2], tvc_in[:, :, :], [Btv[i]])
        Pm = [ph.sb(f"Pm{i}", [128, 32, 128], BF16) for i in range(2)]; BPm = [Buf(), Buf()]
        pidx = ph.ps("pidx", [128, 4], F32); Bpidx = Buf()
        idxf = ph.sb("idxf", [128, 4], F32); Bidxf = Buf()
        idx_i = ph.sb("idx_i", [128, 2, 4], I32); Bidx = [[Buf() for _ in range(4)] for _ in range(2)]
        wsl = ph.sb("wsl", [128, 2, 4], F32)
        xg = ph.sb("xg", [128, 4, D], BF16); Bxg = [Buf() for _ in range(4)]
        ptx = ph.ps("ptx", [128, 8, 128], BF16); Bptx = Buf()
        xgT = [ph.sb(f"xgT{i}", [128, NCH, 512], BF16) for i in range(2)]; BxgT = [Buf(), Buf()]
        NR = 4
        Wr = [ph.sb(f"Wr{i}", [128, NCH, 512], BF16) for i in range(NR)]; BWr = [Buf() for _ in range(NR)]
        pa = [ph.ps(f"pa{i}", [128, 512], F32) for i in range(2)]; Bpa = [Buf(), Buf()]
        pb = [ph.ps(f"pb{i}", [128, 512], F32) for i in range(2)]; Bpb = [Buf(), Buf()]
        pyo = [ph.ps(f"pyo{i}", [128, 512], F32) for i in range(2)]; Bpyo = [Buf(), Buf()]
        sa = [ph.sb(f"sa{i}", [128, 512], F32) for i in range(2)]; Bsa = [Buf(), Buf()]
        hT = ph.sb("hTe", [128, NCH, 512], BF16); BhT = Buf()
        Ysb = ph.sb("Ye", [128, 4, D], F32); BYe = [Buf() for _ in range(4)]

        def A_tv(e):
            s = e % 2
            cp("pool", tv[s][:, :, 2], affh[:, :, e], [Bsel], [Btv[s]])
            cp("pool", tv[s][:, :, 3], affl[:, :, e], [Bsel], [Btv[s]])

        def A_pm(e, q):
            b = q % 2
            for t in range(32):
                ts("dve", Pm[b][:, t, :], iota[:, q * 128:(q + 1) * 128], pos[:, t, e:e + 1], maskf[:, t, e:e + 1], ALU.is_equal, ALU.mult, [Bio, Bsel], [BPm[b]])

        def A_idx(e, q):
            s = e % 2; b = q % 2
            for t in range(32):
                mm(pidx[:], Pm[b][:, t, :], tv[s][:, t, :], t == 0, t == 31, [BPm[b], Btv[s]], [Bpidx])
            cp("dve", idxf[:], pidx[:], [Bpidx], [Bidxf])
            tt("dve", idx_i[:, s, q:q + 1], idxf[:, 0:1], idxf[:, 1:2], ALU.add, [Bidxf], [Bidx[s][q]])
            tt("dve", wsl[:, s, q:q + 1], idxf[:, 2:3], idxf[:, 3:4], ALU.add, [Bidxf], [Bidx[s][q]])
            kb.dma("pool", lambda: nc.gpsimd.indirect_dma_start(
                out=xg[:, q, :], out_offset=None, in_=h2_d[:, :],
                in_offset=bass.IndirectOffsetOnAxis(ap=idx_i[:, s, q:q + 1], axis=0)), [Bidx[s][q]], [Bxg[q]])

        def A_tr(e, q):
            s = e % 2
            for cg in range(2):
                for c in range(8):
                    cc = cg * 8 + c
                    tr(ptx[:, c, :], xg[:, q, cc * 128:(cc + 1) * 128], ident_b[:], [Bxg[q], B_const], [Bptx])
                cp("act" if cg == 0 else "dve", xgT[s][:, cg * 8:(cg + 1) * 8, q * 128:(q + 1) * 128], ptx[:], [Bptx], [BxgT[s]])

        pieces = []
        for e in range(NE):
            for fg in range(4):
                pieces.append((wg_in, e, fg)); pieces.append((wu_in, e, fg))
            for db in range(4):
                pieces.append((wd_in, e, db))
        pstate = dict(loaded=0)

        def load_piece(k):
            wt, e, j = pieces[k]
            r = k % NR
            wvw = wt[e].rearrange("(c p) n -> p c n", p=128)
            for hh in range(2):
                ld("pool", Wr[r][:, hh * 8:(hh + 1) * 8, :], wvw[:, hh * 8:(hh + 1) * 8, j * 512:(j + 1) * 512], [BWr[r]])

        def need(k):
            while pstate["loaded"] <= min(k + NR - 2, len(pieces) - 1):
                load_piece(pstate["loaded"]); pstate["loaded"] += 1

        def stageB(e, hooks):
            s = e % 2
            kbase = e * 12
            ii = 0
            for fg in range(4):
                kg = kbase + fg * 2; ku = kg + 1
                need(ku)
                rg = kg % NR; ru = ku % NR
                for fc in range(4):
                    i2 = ii % 2; ii += 1
                    for c in range(NCH):
                        mm(pa[i2][:], Wr[rg][:, c, fc * 128:(fc + 1) * 128], xgT[s][:, c, :], c == 0, c == NCH - 1, [BWr[rg], BxgT[s]], [Bpa[i2]])
                    for c in range(NCH):
                        mm(pb[i2][:], Wr[ru][:, c, fc * 128:(fc + 1) * 128], xgT[s][:, c, :], c == 0, c == NCH - 1, [BWr[ru], BxgT[s]], [Bpb[i2]])
                    act(sa[i2][:], pa[i2][:], AF.Silu, [Bpa[i2]], [Bsa[i2]])
                    tt("dve", hT[:, fg * 4 + fc, :], sa[i2][:], pb[i2][:], ALU.mult, [Bsa[i2], Bpb[i2]], [BhT])
                hooks("fg", fg)
            jj = 0
            for db in range(4):
                kd = kbase + 8 + db
                need(kd)
                rd_ = kd % NR
                for sc in range(4):
                    i2 = jj % 2; jj += 1
                    for fcn in range(NCH):
                        mm(pyo[i2][:], hT[:, fcn, sc * 128:(sc + 1) * 128], Wr[rd_][:, fcn, :], fcn == 0, fcn == NCH - 1, [BhT, BWr[rd_]], [Bpyo[i2]])
                    if jj % 2 == 0:
                        act(Ysb[:, sc, db * 512:(db + 1) * 512], pyo[i2][:], AF.Copy, [Bpyo[i2], Bidx[s][sc]], [BYe[sc]], scale=wsl[:, s, sc:sc + 1])
                    else:
                        ts("dve", Ysb[:, sc, db * 512:(db + 1) * 512], pyo[i2][:], wsl[:, s, sc:sc + 1], None, ALU.mult, None, [Bpyo[i2], Bidx[s][sc]], [BYe[sc]])
                hooks("db", db)
            for sc in range(4):
                kb.dma("pool", lambda sc=sc, s=s: nc.gpsimd.indirect_dma_start(
                    out=y2_d[:, :], out_offset=bass.IndirectOffsetOnAxis(ap=idx_i[:, s, sc:sc + 1], axis=0),
                    in_=Ysb[:, sc, :], in_offset=None, compute_op=ALU.add), [BYe[sc], Bidx[s][sc]], [By2])

        kb.barrier()
        A_tv(0)
        for q in range(4):
            A_pm(0, q); A_idx(0, q)
        for q in range(4):
            A_tr(0, q)
        for e in range(NE):
            nx = e + 1

            def hooks(kind, j, nx=nx):
                if nx >= NE:
                    return
                if kind == "fg":
                    A_idx(nx, j)
                    if j + 1 < 4:
                        A_pm(nx, j + 1)
                else:
                    A_tr(nx, j)
            if nx < NE:
                A_tv(nx)
                A_pm(nx, 0)
            stageB(e, hooks)
    if stop_after <= 8:
        kb.finish(); return nc

    with Phase(kb) as ph:
        gt2, Bgt2 = bcast_row(ph, "gt2", mod_d[0:1, 5 * D:6 * D])
        po2, Bpo2 = bcast_row(ph, "po2", post2[0:1, :])
        tt("dve", gt2[:], gt2[:], po2[:], ALU.mult, [Bpo2], [Bgt2])
        yt = [ph.sb(f"y2t{i}", [128, D], F32) for i in range(3)]; Byt = [Buf() for _ in range(3)]
        x1t = [ph.sb(f"x1u{i}", [128, D], F32) for i in range(3)]; Bx1 = [Buf() for _ in range(3)]
        junk = ph.sb("junk8", [128, D], BF16); Bjunk = Buf()
        sq = [ph.sb(f"sq8{i}", [128, 2], F32) for i in range(2)]; Bsq = [Buf(), Buf()]
        ot = [ph.sb(f"ot{i}", [128, D], F32) for i in range(2)]; Bot = [Buf(), Buf()]
        NT8 = T // 128

        def ld8(i):
            if i < NT8:
                ld("sp", yt[i % 3][:], y2_d[i * 128:(i + 1) * 128, :], [Byt[i % 3]])
                ld("sp", x1t[i % 3][:], x1_d[i * 128:(i + 1) * 128, :], [Bx1[i % 3]])
        ld8(0); ld8(1)
        for i in range(NT8):
            s = i % 2; s3 = i % 3
            t0 = i * 128
            ld8(i + 2)
            act(junk[:], yt[s3][:], AF.Square, [Byt[s3]], [Bjunk, Bsq[s]], accum_out=sq[s][:, 0:1])
            rstd_from(sq[s][:, 1:2], sq[s][:, 0:1], D, [], [Bsq[s]])
            stt(yt[s3][:], yt[s3][:], sq[s][:, 1:2], gt2[:], ALU.mult, ALU.mult, [Bsq[s], Bgt2], [Byt[s3]])
            tt("dve", ot[s][:], yt[s3][:], x1t[s3][:], ALU.add, [Byt[s3], Bx1[s3]], [Bot[s]])
            ld("pool", out[t0:t0 + 128, :], ot[s][:], [], [Bot[s]])

    kb.finish()
    return nc


def make_inputs(inp, b):
    L = 0
    Rm, cosT, sinT = rope_consts()
    m = dict(
        xk=np.ascontiguousarray(np.concatenate([inp["ctx"][b], inp["x"][b]], axis=0)),
        c2=np.ascontiguousarray(np.stack([inp["c"][b], inp["c_ctx"]], axis=0)),
        w_mod=inp["w_mod"][L], b_mod=inp["b_mod"][L][None, :],
        pre1=inp["pre_norm1"][L][None, :], post1=inp["post_norm1"][L][None, :],
        pre2=inp["pre_norm2"][L][None, :], post2=inp["post_norm2"][L][None, :],
        w_in=inp["w_in"][L],
        ident=np.eye(128, dtype=np.float32),
        q_norm=inp["q_norm"][L][:, None], k_norm=inp["k_norm"][L][:, None],
        rotm=Rm, cosT=cosT, sinT=sinT,
        w_o_att=inp["w_o_att"][L], w_o_ret=inp["w_o_ret"][L], w_out=inp["w_out"][L], w_router=inp["w_router"][L],
        w_gate=inp["w_gate"][L][:NEXP], w_up=inp["w_up"][L][:NEXP], w_down=inp["w_down"][L][:NEXP],
        iota512=np.ascontiguousarray(np.broadcast_to(np.arange(512, dtype=np.float32), (128, 512))),
        tri=np.triu(np.ones((128, 128), np.float32), 1),
        tvc=np.ascontiguousarray(np.stack([np.broadcast_to(np.arange(128, dtype=np.float32)[:, None], (128, 32)),
                                           np.broadcast_to(128.0 * np.arange(32, dtype=np.float32)[None, :], (128, 32))], axis=-1)),
        ret_decay=inp["ret_decay"][L].reshape(1, 8), ret_gn=np.ascontiguousarray(inp["ret_gn"][L].reshape(8, 128).T),
    )
    ii = np.arange(128, dtype=np.float32)
    jj = ii[:, None]; i2 = ii[None, :]
    tabs = np.stack([np.broadcast_to(i2 + 1, (128, 128)), np.broadcast_to(128 - i2, (128, 128)),
                     np.maximum(i2 - jj, 0), (i2 >= jj).astype(np.float32),
                     np.maximum(jj - i2, 0), (jj >= i2).astype(np.float32)]).astype(np.float32)
    m["ret_tabs"] = np.ascontiguousarray(tabs)
    p = ii
    m["ret_pcols"] = np.ascontiguousarray(np.stack([127 - p, p, 255 - p, 127 - p, p, 128 + p], axis=1).astype(np.float32))
    return m


_NC_CACHE = {}


def kernel(**inputs):
    inp = {k: np.asarray(v) for k, v in inputs.items()}
    if "nc" not in _NC_CACHE:
        _NC_CACHE["nc"] = build()
    nc = _NC_CACHE["nc"]
    B = inp["x"].shape[0]
    maps = [make_inputs(inp, b) for b in range(B)]
    work = [0, 1, 4, 5]
    zero = {kk: np.zeros_like(v) for kk, v in maps[0].items()}
    in_maps = [zero] * 8
    in_maps = list(in_maps)
    for b in range(B):
        in_maps[work[b]] = maps[b]
    res = run_bass_kernel_spmd(nc, in_maps, core_ids=list(range(8)))
    out = np.stack([np.asarray(res.results[work[b]]["out"]) for b in range(B)], axis=0)
    return out.astype(np.float32)
```

```python
import numpy as np
import contextlib
import concourse.bass as bass
import concourse.mybir as mybir
from concourse.bass_utils import run_bass_kernel_spmd

F32 = mybir.dt.float32
BF16 = mybir.dt.bfloat16
I32 = mybir.dt.int32
U32 = mybir.dt.uint32
AF = mybir.ActivationFunctionType
ALU = mybir.AluOpType
AX = mybir.AxisListType

D = 2048
T = 4096
LC = 256
TK = T + LC
NCH = 16
QW = 6656
INW = 8704
EPS = 1e-6


class Buf:
    __slots__ = ("w", "r", "name")

    def __init__(self, name=""):
        self.w = None
        self.r = {}
        self.name = name


class KB:
    def __init__(self, nc):
        self.nc = nc
        self.es = contextlib.ExitStack()
        self.E = dict(pe=nc.tensor, act=nc.scalar, dve=nc.vector, pool=nc.gpsimd, sp=nc.sync)
        self.clk = {}
        self.seen = {e: {} for e in self.E}
        self.nsem = 0
        for e in ("pe", "act", "dve", "pool"):
            self._new_clk(e)
        self.dpool = {}
        for q, n in (("sp", 20), ("pool", 12), ("act", 4)):
            self.dpool[q] = [[self._sem(f"d{q}{i}"), 0] for i in range(n)]
        self.dnext = {q: 0 for q in self.dpool}

    def _sem(self, name):
        self.nsem += 1
        sm = self.es.enter_context(self.nc.semaphore(name + f"_{self.nsem}"))
        if not hasattr(self, "_keep"):
            self._keep = []
        self._keep.append(sm)
        return sm

    def _new_clk(self, e):
        self.clk[e] = [self._sem("clk" + e), 0]

    def wait(self, eng, tok):
        sem, val = tok
        k = id(sem)
        if eng == "pe" and sem is self.clk["pe"][0]:
            return
        if self.seen[eng].get(k, 0) >= val:
            return
        self.E[eng].wait_ge(sem, val)
        self.seen[eng][k] = val

    def _deps(self, eng, reads, writes):
        for b in reads:
            if b.w is not None:
                self.wait(eng, b.w)
        for b in writes:
            if b.w is not None:
                self.wait(eng, b.w)
            for k, (sem, val) in b.r.items():
                self.wait(eng, (sem, val))

    def _commit(self, tok, reads, writes):
        sem, val = tok
        k = id(sem)
        for b in reads:
            if k not in b.r or b.r[k][1] < val:
                b.r[k] = (sem, val)
        for b in writes:
            b.w = tok
            b.r = {}

    def op(self, eng, fn, reads=(), writes=()):
        self._deps(eng, reads, writes)
        ins = fn()
        c = self.clk[eng]
        c[1] += 1
        ins.then_inc(c[0], 1)
        tok = (c[0], c[1])
        self._commit(tok, reads, writes)
        return tok

    def dma(self, q, fn, reads=(), writes=()):
        self._deps(q, reads, writes)
        pool = self.dpool[q]
        i = self.dnext[q]
        self.dnext[q] = (i + 1) % len(pool)
        ent = pool[i]
        if ent[1] > 0:
            self.wait(q, (ent[0], ent[1]))
        ins = fn()
        ent[1] += 16
        ins.then_inc(ent[0], 16)
        tok = (ent[0], ent[1])
        self._commit(tok, reads, writes)
        return tok

    def _alltoks(self):
        toks = []
        for e, c in self.clk.items():
            if c[1] > 0:
                toks.append((c[0], c[1]))
        for q, pool in self.dpool.items():
            for ent in pool:
                if ent[1] > 0:
                    toks.append((ent[0], ent[1]))
        return toks

    def barrier(self):
        toks = self._alltoks()
        for e in self.E:
            for t in toks:
                self.wait(e, t)

    def new_phase(self):
        self.barrier()
        for e in ("pe", "act", "dve", "pool"):
            if self.clk[e][1] > 0:
                self._new_clk(e)

    def finish(self, eng="sp"):
        for t in self._alltoks():
            self.wait(eng, t)


class Phase:
    def __init__(self, kb):
        self.kb = kb
        self.es = contextlib.ExitStack()

    def __enter__(self):
        return self

    def __exit__(self, *a):
        self.kb.new_phase()
        self.es.close()
        return False

    _n = [0]

    def sb(self, name, shape, dt):
        Phase._n[0] += 1
        t = self.es.enter_context(self.kb.nc.sbuf_tensor(f"{name}_s{Phase._n[0]}", list(shape), dt))
        return t

    def ps(self, name, shape, dt):
        Phase._n[0] += 1
        return self.es.enter_context(self.kb.nc.psum_tensor(f"{name}_p{Phase._n[0]}", list(shape), dt))


def rope_consts():
    Rm = np.zeros((128, 128), np.float32)
    for a in range(2):
        for j in range(32):
            Rm[a * 64 + 32 + j, a * 64 + j] = -1.0
            Rm[a * 64 + j, a * 64 + 32 + j] = 1.0
    t = np.arange(T)
    r = (t // 64).astype(np.float32)
    cl = (t % 64).astype(np.float32)
    inv = (10000.0 ** (-np.arange(32, dtype=np.float32) / 32)).astype(np.float32)
    ang_r = r[:, None] * inv
    ang_c = cl[:, None] * inv
    ang = np.concatenate([ang_r, ang_r, ang_c, ang_c], axis=-1)
    cosT = np.ascontiguousarray(np.cos(ang).T.astype(np.float32))
    sinT = np.ascontiguousarray(np.sin(ang).T.astype(np.float32))
    return Rm, cosT, sinT


P4STOP = 0
P4VAR = 2
SKIP3 = 0
NEXP = 16
NBISECT = 26


def build(dbg=(), stop_after=99):
    nc = bass.Bass("TRN2", target_bir_lowering=False)
    kb = KB(nc)
    dbg = set(dbg)

    def din(name, shape, dt=F32):
        return nc.dram_tensor(name, list(shape), dt, kind="ExternalInput").ap()

    def dscr(name, shape, dt):
        kind = "ExternalOutput" if name in dbg else "Internal"
        return nc.dram_tensor(name, list(shape), dt, kind=kind).ap()

    xk = din("xk", [TK, D])
    c2 = din("c2", [2, D])
    w_mod = din("w_mod", [D, 6 * D])
    b_mod = din("b_mod", [1, 6 * D])
    pre1 = din("pre1", [1, D]); post1 = din("post1", [1, D]); pre2 = din("pre2", [1, D]); post2 = din("post2", [1, D])
    w_in = din("w_in", [D, INW])
    ident_in = din("ident", [128, 128])
    out = nc.dram_tensor("out", [T, D], F32, kind="ExternalOutput").ap()

    mod_d = dscr("mod_d", [2, 6 * D], F32)
    hT_d = dscr("hT_d", [128, NCH, TK], BF16)

    G = Phase(kb)
    ident_f = G.sb("ident_f", [128, 128], F32)
    ident_b = G.sb("ident_b", [128, 128], BF16)
    eps_t = G.sb("eps_t", [128, 1], F32)
    B_const = Buf("const")
    kb.dma("sp", lambda: nc.sync.dma_start(out=ident_f[:], in_=ident_in[:, :]), writes=[B_const])
    kb.dma("pool", lambda: nc.gpsimd.dma_start(out=ident_b[:], in_=ident_in[:, :]), writes=[B_const])
    kb.op("dve", lambda: nc.vector.memset(eps_t[:], EPS), writes=[B_const])
    mhalf = G.sb("mhalf", [128, 1], F32)
    kb.op("dve", lambda: nc.vector.memset(mhalf[:], -0.5), writes=[B_const])

    def rstd_from(out_ap, ssq_ap, n, R, W):
        kb.op("dve", lambda: nc.vector.tensor_scalar(out=out_ap, in0=ssq_ap, scalar1=1.0 / n, scalar2=EPS, op0=ALU.mult, op1=ALU.add), R, W)
        kb.op("pool", lambda: nc.gpsimd.tensor_tensor(out=out_ap, in0=out_ap, in1=mhalf[:], op=ALU.pow), [B_const], W)
    kb.barrier()

    with Phase(kb) as ph:
        c2T = ph.sb("c2T", [128, 2, 16], F32)
        s2T = ph.sb("s2T", [128, 2, 16], BF16)
        bm2 = ph.sb("bm2", [2, 6 * D], F32)
        B_c2 = Buf(); B_s2 = Buf(); B_bm = Buf()
        for r in range(2):
            kb.dma("sp", lambda r=r: nc.sync.dma_start(out=c2T[:, r, :], in_=c2[r, :].rearrange("(p c) -> p c", c=16)), writes=[B_c2])
            kb.dma("sp", lambda r=r: nc.sync.dma_start(out=bm2[r:r + 1, :], in_=b_mod[:, :]), writes=[B_bm])
        kb.op("act", lambda: nc.scalar.activation(out=s2T[:], in_=c2T[:], func=AF.Silu), reads=[B_c2], writes=[B_s2])
        wmv = w_mod.rearrange("(p c) n -> p c n", c=16)
        NB = 24
        NWB = 3
        wb = [ph.sb(f"wmb{i}", [128, 16, 512], BF16) for i in range(NWB)]
        Bwb = [Buf() for _ in range(NWB)]
        pm = [ph.ps(f"pm{i}", [2, 512], F32) for i in range(2)]
        Bpm = [Buf(), Buf()]
        mrow = [ph.sb(f"mrow{i}", [2, 512], F32) for i in range(2)]
        Bmr = [Buf(), Buf()]

        def ldw(j):
            s_ = j % NWB
            kb.dma("pool", lambda j=j, s_=s_: nc.gpsimd.dma_start(out=wb[s_][:], in_=wmv[:, :, j * 512:(j + 1) * 512]), writes=[Bwb[s_]])
        ldw(0); ldw(1)
        for j in range(NB):
            s = j % 2; sw = j % NWB
            if j + 2 < NB:
                ldw(j + 2)
            for c in range(16):
                kb.op("pe", lambda c=c, s=s, sw=sw: nc.tensor.matmul(pm[s][:], lhsT=s2T[:, :, c], rhs=wb[sw][:, c, :], start=(c == 0), stop=(c == 15)),
                      reads=[B_s2, Bwb[sw]], writes=[Bpm[s]])
            kb.op("dve", lambda j=j, s=s: nc.vector.tensor_tensor(out=mrow[s][:], in0=pm[s][:], in1=bm2[:, j * 512:(j + 1) * 512], op=ALU.add),
                  reads=[Bpm[s], B_bm], writes=[Bmr[s]])
            kb.dma("sp", lambda j=j, s=s: nc.sync.dma_start(out=mod_d[:, j * 512:(j + 1) * 512], in_=mrow[s][:]), reads=[Bmr[s]])
    if stop_after <= 0:
        kb.finish(); return nc

    def bcast_row(ph, name, src_ap):
        t = ph.sb(name, [128, D], F32)
        b = Buf(name)
        kb.dma("sp", lambda: nc.sync.dma_start(out=t[:], in_=src_ap.partition_broadcast(128)), writes=[b])
        return t, b

    with Phase(kb) as ph:
        g1, Bg1 = bcast_row(ph, "g1", pre1[0:1, :])
        Gm = []
        for r in range(2):
            sc, Bsc = bcast_row(ph, f"sc{r}", mod_d[r:r + 1, D:2 * D])
            sh, Bsh = bcast_row(ph, f"sh{r}", mod_d[r:r + 1, 0:D])
            kb.op("dve", lambda sc=sc: nc.vector.scalar_tensor_tensor(out=sc[:], in0=sc[:], scalar=1.0, in1=g1[:], op0=ALU.add, op1=ALU.mult),
                  reads=[Bg1], writes=[Bsc])
            Gm.append((sc, Bsc, sh, Bsh))
        NT = TK // 128
        xt = [ph.sb(f"xt{i}", [128, D], F32) for i in range(3)]; Bxt = [Buf() for _ in range(3)]
        junk = ph.sb("junk", [128, D], BF16); Bjunk = Buf()
        ssq = [ph.sb(f"ssq{i}", [128, 1], F32) for i in range(2)]; Bssq = [Buf(), Buf()]
        rstd = [ph.sb(f"rstd{i}", [128, 1], F32) for i in range(2)]; Brstd = [Buf(), Buf()]
        h1 = [ph.sb(f"h1{i}", [128, D], F32) for i in range(2)]; Bh1 = [Buf(), Buf()]
        hb = [ph.sb(f"hb{i}", [128, D], BF16) for i in range(2)]; Bhb = [Buf(), Buf()]
        pt = [ph.ps(f"pt{i}", [128, 8, 128], BF16) for i in range(4)]; Bpt = [Buf() for _ in range(4)]
        hT = [ph.sb(f"hTt{i}", [128, NCH, 512], BF16) for i in range(2)]; BhT = [Buf(), Buf()]

        def ldx(i):
            if i < NT:
                kb.dma("sp", lambda: nc.sync.dma_start(out=xt[i % 3][:], in_=xk[i * 128:(i + 1) * 128, :]), writes=[Bxt[i % 3]])
        ldx(0); ldx(1)

        def stA(i):
            s3 = i % 3; s = i % 2
            ldx(i + 2)
            r = 1 if i < LC // 128 else 0
            sc, Bsc, sh, Bsh = Gm[r]
            kb.op("act", lambda: nc.scalar.activation(out=junk[:], in_=xt[s3][:], func=AF.Square, accum_out=ssq[s][:]),
                  reads=[Bxt[s3]], writes=[Bjunk, Bssq[s]])
            rstd_from(rstd[s][:], ssq[s][:], D, [Bssq[s]], [Brstd[s]])
            kb.op("dve", lambda: nc.vector.scalar_tensor_tensor(out=h1[s][:], in0=xt[s3][:], scalar=rstd[s][:], in1=sc[:], op0=ALU.mult, op1=ALU.mult),
                  reads=[Bxt[s3], Brstd[s], Bsc], writes=[Bh1[s]])
            kb.op("pool", lambda: nc.gpsimd.tensor_tensor(out=hb[s][:], in0=h1[s][:], in1=sh[:], op=ALU.add),
                  reads=[Bh1[s], Bsh], writes=[Bhb[s]])

        def stB(i):
            s = i % 2
            if i < 2:
                grp, gi, gn_ = 0, i, 2
            else:
                grp, gi, gn_ = 1 + (i - 2) // 4, (i - 2) % 4, 4
            sg_ = grp % 2
            for hf in range(2):
                pi = (2 * i + hf) % 4
                for c in range(8):
                    cc = hf * 8 + c
                    kb.op("pe", lambda c=c, cc=cc: nc.tensor.transpose(out=pt[pi][:, c, :], in_=hb[s][:, cc * 128:(cc + 1) * 128], identity=ident_b[:]),
                          reads=[Bhb[s]], writes=[Bpt[pi]])
                dst = hT[sg_][:, hf * 8:(hf + 1) * 8, gi * 128:(gi + 1) * 128]
                if hf == 0:
                    kb.op("act", lambda: nc.scalar.copy(out=dst, in_=pt[pi][:]), reads=[Bpt[pi]], writes=[BhT[sg_]])
                else:
                    kb.op("dve", lambda: nc.vector.tensor_copy(out=dst, in_=pt[pi][:]), reads=[Bpt[pi]], writes=[BhT[sg_]])
            if gi == gn_ - 1:
                tk0 = 0 if grp == 0 else LC + (grp - 1) * 512
                nn = gn_ * 128
                for hh in range(2):
                    kb.dma("sp", lambda hh=hh: nc.sync.dma_start(out=hT_d[:, hh * 8:(hh + 1) * 8, tk0:tk0 + nn], in_=hT[sg_][:, hh * 8:(hh + 1) * 8, :nn]), reads=[BhT[sg_]])

        for i in range(NT + 1):
            if i < NT:
                stA(i)
            if i >= 1:
                stB(i - 1)
    if stop_after <= 1:
        kb.finish(); return nc

    def mm(out_, lhsT, rhs, st, sp, R, W):
        return kb.op("pe", lambda: nc.tensor.matmul(out_, lhsT=lhsT, rhs=rhs, start=st, stop=sp), R, W)

    def tr(out_, in_, idn, R, W):
        return kb.op("pe", lambda: nc.tensor.transpose(out=out_, in_=in_, identity=idn), R, W)

    def act(out_, in_, func, R, W, **kw):
        return kb.op("act", lambda: nc.scalar.activation(out=out_, in_=in_, func=func, **kw), R, W)

    def tt(eng, out_, in0, in1, op, R, W):
        e = nc.vector if eng == "dve" else nc.gpsimd
        return kb.op(eng, lambda: e.tensor_tensor(out=out_, in0=in0, in1=in1, op=op), R, W)

    def stt(out_, in0, scalar, in1, op0, op1, R, W):
        return kb.op("dve", lambda: nc.vector.scalar_tensor_tensor(out=out_, in0=in0, scalar=scalar, in1=in1, op0=op0, op1=op1), R, W)

    def ts(eng, out_, in0, s1, s2, op0, op1, R, W):
        e = nc.vector if eng == "dve" else nc.gpsimd
        if op1 is None:
            return kb.op(eng, lambda: e.tensor_scalar(out=out_, in0=in0, scalar1=s1, scalar2=None, op0=op0), R, W)
        return kb.op(eng, lambda: e.tensor_scalar(out=out_, in0=in0, scalar1=s1, scalar2=s2, op0=op0, op1=op1), R, W)

    def cp(eng, out_, in_, R, W):
        if eng == "act":
            return kb.op("act", lambda: nc.scalar.copy(out=out_, in_=in_), R, W)
        e = nc.vector if eng == "dve" else nc.gpsimd
        return kb.op(eng, lambda: e.tensor_copy(out=out_, in_=in_), R, W)

    def ld(q, out_, in_, W, R=()):
        e = nc.sync if q == "sp" else nc.gpsimd
        return kb.dma(q, lambda: e.dma_start(out=out_, in_=in_), R, W)

    qn_in = din("q_norm", [128, 1]); kn_in = din("k_norm", [128, 1])
    rm_in = din("rotm", [128, 128]); cos_in = din("cosT", [128, T]); sin_in = din("sinT", [128, T])
    qaT_d = dscr("qaT_d", [8, 128, T], BF16); qrT_d = dscr("qrT_d", [4, 128, T], BF16)
    sgT_d = dscr("sgT_d", [8, 128, T], BF16); gaT_d = dscr("gaT_d", [16, 128, T], BF16); grT_d = dscr("grT_d", [16, 128, T], BF16)
    kaT_d = dscr("kaT_d", [2, 128, TK], BF16); krT_d = dscr("krT_d", [4, 128, TK], BF16)
    va_d = dscr("va_d", [TK, 256], BF16); vr_d = dscr("vr_d", [TK, 1024], BF16)
    ones_b = G.sb("ones_b", [128, 128], BF16)
    kb.op("dve", lambda: nc.vector.memset(ones_b[:], 1.0), writes=[B_const])
    kb.barrier()

    with Phase(kb) as ph:
        qn_t = ph.sb("qn_t", [128, 1], F32); kn_t = ph.sb("kn_t", [128, 1], F32)
        rm_b = ph.sb("rm_b", [128, 128], BF16)
        cos_t = ph.sb("cos_t", [128, T], F32); sin_t = ph.sb("sin_t", [128, T], F32)
        Bc = Buf()
        ld("sp", qn_t[:], qn_in[:, :], [Bc]); ld("sp", kn_t[:], kn_in[:, :], [Bc])
        ld("pool", rm_b[:], rm_in[:, :], [Bc])
        ld("sp", cos_t[:], cos_in[:, :], [Bc]); ld("sp", sin_t[:], sin_in[:, :], [Bc])
        wv = w_in.rearrange("(c p) n -> p c n", p=128)
        Wt = [ph.sb(f"Wt{i}", [128, NCH, 768], BF16) for i in range(2)]; BW = [Buf(), Buf()]
        hB = [ph.sb(f"hB{i}", [128, NCH, 512], BF16) for i in range(2)]; BhB = [Buf(), Buf()]
        pz = [ph.ps(f"pz{i}", [128, 512], F32) for i in range(4)]; Bpz = [Buf() for _ in range(4)]
        pq = [ph.ps(f"pq{i}", [128, 512], F32) for i in range(2)]; Bpq = [Buf() for _ in range(2)]
        pr = [ph.ps(f"pr{i}", [128, 512], F32) for i in range(2)]; Bpr = [Buf() for _ in range(2)]
        sqb = [ph.sb(f"sqb{i}", [128, 512], BF16) for i in range(2)]; Bsq = [Buf() for _ in range(2)]
        sd = [ph.sb(f"sd{i}", [128, 512], F32) for i in range(2)]; Bsd = [Buf() for _ in range(2)]
        qnb = [ph.sb(f"qnb{i}", [128, 512], BF16) for i in range(3)]; Bqn = [Buf() for _ in range(3)]
        t1 = [ph.sb(f"t1{i}", [128, 512], F32) for i in range(2)]; Bt1 = [Buf() for _ in range(2)]
        t2 = [ph.sb(f"t2{i}", [128, 512], F32) for i in range(2)]; Bt2 = [Buf() for _ in range(2)]
        ob = [ph.sb(f"ob{i}", [128, 512], BF16) for i in range(4)]; Bob = [Buf() for _ in range(4)]
        cnt = dict(z=0, q=0, r=0, sq=0, sd=0, qn=0, t=0, ob=0, w=0, h=0)
        deferred = []

        def nxt(k, n):
            v = cnt[k] % n
            cnt[k] += 1
            return v

        def run_deferred():
            todo = list(deferred)
            deferred.clear()
            for f in todo:
                f()

        def rope_and_store(iq, N, t0, dst):
            ir = nxt("r", 2)
            mm(pr[ir][:, :N], rm_b[:], qnb[iq][:, :N], True, True, [Bqn[iq], Bc], [Bpr[ir]])

            def fin():
                it = nxt("t", 2); io = nxt("ob", 4)
                tt("dve", t1[it][:, :N], qnb[iq][:, :N], cos_t[:, t0:t0 + N], ALU.mult, [Bqn[iq], Bc], [Bt1[it]])
                tt("dve", t2[it][:, :N], pr[ir][:, :N], sin_t[:, t0:t0 + N], ALU.mult, [Bpr[ir], Bc], [Bt2[it]])
                tt("pool", ob[io][:, :N], t1[it][:, :N], t2[it][:, :N], ALU.add, [Bt1[it], Bt2[it]], [Bob[io]])
                ld("pool", dst, ob[io][:, :N], [], [Bob[io]])
            deferred.append(fin)

        def evac(kind, iz, N, t0, dst, rope):
            if kind in ("silu", "sig"):
                io = nxt("ob", 4)
                act(ob[io][:, :N], pz[iz][:, :N], AF.Silu if kind == "silu" else AF.Sigmoid, [Bpz[iz]], [Bob[io]])
                ld("pool", dst, ob[io][:, :N], [], [Bob[io]])
                return
            if kind[0] == "r":
                iq = nxt("qn", 3)
                sc = (128.0 ** -0.5) if kind == "rk" else 1.0
                act(qnb[iq][:, :N], pz[iz][:, :N], AF.Copy, [Bpz[iz]], [Bqn[iq]], scale=sc)
                if rope:
                    deferred.append(lambda: rope_and_store(iq, N, t0, dst))
                else:
                    ld("pool", dst, qnb[iq][:, :N], [], [Bqn[iq]])
                return
            gt = qn_t if kind == "nq" else kn_t
            isq = nxt("sq", 2)
            act(sqb[isq][:, :N], pz[iz][:, :N], AF.Square, [Bpz[iz]], [Bsq[isq]])

            def st2():
                ip = nxt("q", 2)
                mm(pq[ip][:, :N], ones_b[:], sqb[isq][:, :N], True, True, [Bsq[isq], B_const], [Bpq[ip]])
                isd = nxt("sd", 2)
                act(sd[isd][:, :N], pq[ip][:, :N], AF.Sqrt, [Bpq[ip], B_const], [Bsd[isd]], bias=eps_t[:], scale=1.0 / 128)
                kb.op("dve", lambda: nc.vector.reciprocal(out=sd[isd][:, :N], in_=sd[isd][:, :N]), [], [Bsd[isd]])
                iq = nxt("qn", 3)
                stt(qnb[iq][:, :N], pz[iz][:, :N], gt[:], sd[isd][:, :N], ALU.mult, ALU.mult, [Bpz[iz], Bsd[isd], Bc], [Bqn[iq]])
                if rope:
                    deferred.append(lambda: rope_and_store(iq, N, t0, dst))
                else:
                    ld("pool", dst, qnb[iq][:, :N], [], [Bqn[iq]])
            deferred.append(st2)

        groups = []
        for g in range(2):
            groups.append((g * 512, 512, "nq", qaT_d, g * 4, False))
        groups.append((1024, 512, "rq", qrT_d, 0, False))
        for g in range(2):
            groups.append((1536 + g * 512, 512, "silu", sgT_d, g * 4, False))
        for g in range(4):
            groups.append((2560 + g * 512, 512, "sig", gaT_d, g * 4, False))
        for g in range(4):
            groups.append((4608 + g * 512, 512, "sig", grT_d, g * 4, False))
        groups.append((-1, 768, "k", None, 0, True))
        for (c0, ncol, kind, dstT, h0, kv) in groups:
            iw = nxt("w", 2)
            if kind == "k":
                BWa = Buf(); BWb = Buf()
                ld("pool", Wt[iw][:, :, 0:256], wv[:, :, 6656:6912], [BW[iw]])
                ld("pool", Wt[iw][:, :, 256:768], wv[:, :, 7168:7680], [BW[iw]])
            else:
                ld("pool", Wt[iw][:, :, 0:512], wv[:, :, c0:c0 + 512], [BW[iw]])
            blks = range(0, 9) if kv else range(1, 9)
            for blk in blks:
                ih = nxt("h", 2)
                tk0 = 0 if blk == 0 else LC + (blk - 1) * 512
                N = LC if blk == 0 else 512
                ld("sp", hB[ih][:, :, :N], hT_d[:, :, tk0:tk0 + N], [BhB[ih]])
                for ch in range(ncol // 128):
                    iz = nxt("z", 4)
                    for c in range(NCH):
                        mm(pz[iz][:, :N], Wt[iw][:, c, ch * 128:(ch + 1) * 128], hB[ih][:, c, :N], c == 0, c == NCH - 1, [BW[iw], BhB[ih]], [Bpz[iz]])
                    run_deferred()
                    t0 = tk0 - LC
                    if kind == "k":
                        if ch < 2:
                            evac("nk", iz, N, t0, kaT_d[ch, :, tk0:tk0 + N], rope=(blk > 0))
                        else:
                            evac("rk", iz, N, t0, krT_d[ch - 2, :, tk0:tk0 + N], rope=(blk > 0))
                    else:
                        evac(kind, iz, N, t0, dstT[h0 + ch, :, t0:t0 + N], rope=True)
        run_deferred(); run_deferred(); run_deferred()
        pv = pz
        vo = [ph.sb(f"vo{i}", [128, 512], BF16) for i in range(2)]; Bvo = [Buf(), Buf()]
        vgroups = [(6912, 256, va_d, 0), (7680, 512, vr_d, 0), (8192, 512, vr_d, 512)]
        for (c0, ncol, dstT, dc0) in vgroups:
            iw = nxt("w", 2)
            ld("pool", Wt[iw][:, :, 0:ncol], wv[:, :, c0:c0 + ncol], [BW[iw]])
            for blk in range(0, 9):
                ih = nxt("h", 2)
                tk0 = 0 if blk == 0 else LC + (blk - 1) * 512
                N = LC if blk == 0 else 512
                ld("sp", hB[ih][:, :, :N], hT_d[:, :, tk0:tk0 + N], [BhB[ih]])
                for tl in range(N // 128):
                    iz = nxt("z", 4)
                    for c in range(NCH):
                        mm(pv[iz][:, :ncol], hB[ih][:, c, tl * 128:(tl + 1) * 128], Wt[iw][:, c, :ncol], c == 0, c == NCH - 1, [BW[iw], BhB[ih]], [Bpz[iz]])
                    io = nxt("ob", 2)
                    if tl % 2 == 0:
                        cp("act", vo[io][:, :ncol], pv[iz][:, :ncol], [Bpz[iz]], [Bvo[io]])
                    else:
                        cp("dve", vo[io][:, :ncol], pv[iz][:, :ncol], [Bpz[iz]], [Bvo[io]])
                    r0 = tk0 + tl * 128
                    ld("pool", dstT[r0:r0 + 128, dc0:dc0 + ncol], vo[io][:, :ncol], [], [Bvo[io]])
    if stop_after <= 2:
        kb.finish(); return nc

    oaT_d = dscr("oaT_d", [8, 128, T], BF16)
    orT_d = dscr("orT_d", [8, 128, T], BF16)

    ones_f = G.sb("ones_f", [128, 128], F32)
    kb.op("dve", lambda: nc.vector.memset(ones_f[:], 1.0), writes=[B_const])
    kb.barrier()
    with Phase(kb) as ph:
        KT = ph.sb("KT", [128, TK], BF16); BKT = Buf()
        Vt = ph.sb("Vt", [128, TK // 128, 128], BF16); BVt = Buf()
        QT = [ph.sb(f"QT{i}", [128, 512], BF16) for i in range(2)]; BQT = [Buf(), Buf()]
        NS = 2
        ps_s = [ph.ps(f"ps_s{i}", [128, 1024], F32) for i in range(NS)]; Bps = [Buf() for _ in range(NS)]
        po = [ph.ps(f"po{i}", [128, 512], F32) for i in range(2)]; Bpo = [Buf(), Buf()]
        pd = [ph.ps(f"pd{i}", [128, 512], F32) for i in range(2)]; Bpd = [Buf(), Buf()]
        NPT = 3
        pT = [ph.sb(f"pT{i}", [128, 1024], BF16) for i in range(NPT)]; BpT = [Buf() for _ in range(NPT)]
        accO = [ph.sb(f"accO{i}", [128, 512], F32) for i in range(2)]; BaO = [Buf(), Buf()]
        rd = [ph.sb(f"rd{i}", [128, 512], F32) for i in range(2)]; Brd = [Buf(), Buf()]
        oo = [ph.sb(f"oo{i}", [128, 512], BF16) for i in range(2)]; Boo = [Buf(), Buf()]
        NKT = TK // 128
        NPR = NKT // 2
        it = 0
        pend = []
        sidx = 0
        pidx_ = 0
        for g in range(0 if not SKIP3 else 2, 2):
            ld("sp", KT[:], kaT_d[g, :, :], [BKT])
            ld("sp", Vt[:], va_d[:, g * 128:(g + 1) * 128].rearrange("(n p) d -> p n d", p=128), [BVt])
            for j in range(4):
                h = g * 4 + j
                for qb in range(T // 512):
                    iq = it % 2; it += 1
                    t0 = qb * 512
                    ld("sp", QT[iq][:], qaT_d[h, :, t0:t0 + 512], [BQT[iq]])
                    s_of = {}

                    def issue_s(pr):
                        nonlocal sidx
                        s_of[pr] = sidx % NS; sidx += 1
                        sn = s_of[pr]
                        for u in range(2):
                            kt = 2 * pr + u
                            mm(ps_s[sn][:, u * 512:(u + 1) * 512], KT[:, kt * 128:(kt + 1) * 128], QT[iq][:], True, True, [BKT, BQT[iq]], [Bps[sn]])
                    issue_s(0)
                    for pr in range(NPR):
                        if pr + 1 < NPR:
                            issue_s(pr + 1)
                        if pr == 2 and pend:
                            pend.pop(0)()
                        sc_ = s_of[pr]
                        ip = pidx_ % NPT; pidx_ += 1
                        act(pT[ip][:], ps_s[sc_][:], AF.Exp, [Bps[sc_]], [BpT[ip]], scale=128.0 ** -0.5)
                        for u in range(2):
                            kt = 2 * pr + u
                            mm(po[iq][:], Vt[:, kt, :], pT[ip][:, u * 512:(u + 1) * 512], kt == 0, kt == NKT - 1, [BVt, BpT[ip]], [Bpo[iq]])
                        mm(pd[iq][:], ones_b[:], pT[ip][:, 0:512], pr == 0, False, [B_const, BpT[ip]], [Bpd[iq]])
                        if pr == 0:
                            cp("dve", accO[iq][:], pT[ip][:, 512:1024], [BpT[ip]], [BaO[iq]])
                        else:
                            tt("dve", accO[iq][:], accO[iq][:], pT[ip][:, 512:1024], ALU.add, [BpT[ip]], [BaO[iq]])

                    def epilogue(iq=iq, h=h, t0=t0):
                        mm(pd[iq][:], ones_f[:], accO[iq][:], False, True, [B_const, BaO[iq]], [Bpd[iq]])
                        kb.op("dve", lambda: nc.vector.reciprocal(out=rd[iq][:], in_=pd[iq][:]), [Bpd[iq]], [Brd[iq]])
                        tt("dve", oo[iq][:], po[iq][:], rd[iq][:], ALU.mult, [Bpo[iq], Brd[iq]], [Boo[iq]])
                        ld("pool", oaT_d[h, :, t0:t0 + 512], oo[iq][:], [], [Boo[iq]])
                    pend.append(epilogue)
        while pend:
            pend.pop(0)()
    if stop_after <= 3:
        kb.finish(); return nc

    rdec_in = din("ret_decay", [1, 8])
    gn_in = din("ret_gn", [128, 8])
    tabs_in = din("ret_tabs", [6, 128, 128])
    pcols_in = din("ret_pcols", [128, 6])
    with Phase(kb) as ph:
        Bt = Buf()
        lg = ph.sb("lg", [128, 8], F32)
        tabs = ph.sb("tabs", [128, 6, 128], F32)
        pcols = ph.sb("pcols", [128, 6], F32)
        gn_t = ph.sb("gn_t", [128, 8], F32)
        ld("sp", lg[:], rdec_in[0:1, :].partition_broadcast(128), [Bt])
        ld("sp", tabs[:], tabs_in.rearrange("k p i -> p k i"), [Bt])
        ld("sp", pcols[:], pcols_in[:, :], [Bt])
        ld("sp", gn_t[:], gn_in[:, :], [Bt])
        act(lg[:], lg[:], AF.Exp, [], [Bt])
        act(lg[:], lg[:], AF.Ln, [], [Bt], bias=1.0)
        ts("dve", lg[:], lg[:], -1.0, None, ALU.mult, None, [], [Bt])
        kT = ph.sb("kT", [128, TK], BF16); BkT = Buf()
        qT = ph.sb("qT", [128, T], BF16); BqT = Buf()
        V = ph.sb("V", [128, TK // 128, 256], BF16); BV = Buf()
        sg = ph.sb("sg", [128, 2, T], BF16); Bsg = Buf()
        wq = ph.sb("wq", [128, 2, 128], F32); Bwq = Buf()
        wk = ph.sb("wk", [128, 6], F32); Bwk = Buf()
        gch = ph.sb("gch", [128, 2], F32); Bgch = Buf()
        MT = ph.sb("MT", [128, 128], F32); BMT = Buf()
        mtmp = ph.sb("mtmp", [128, 2, 128], F32); Bmtmp = Buf()
        kw = ph.sb("kw", [128, TK // 128, 2, 128], BF16); Bkw = Buf()
        S32 = [ph.sb(f"S32{i}", [128, 256], F32) for i in range(2)]; BS32 = [Buf(), Buf()]
        SB = [ph.sb(f"SB{i}", [128, 32, 256], BF16) for i in range(2)]; BSB = [Buf(), Buf()]
        ptk = [ph.ps(f"ptk{i}", [128, 2, 128], BF16) for i in range(2)]; Bptk = [Buf(), Buf()]
        pu = [ph.ps(f"pu{i}", [128, 256], F32) for i in range(2)]; Bpu = [Buf(), Buf()]
        pS = ph.ps("pS", [128, 128], F32); BpS = Buf()
        pO = [ph.ps(f"pO{i}", [128, 256], F32) for i in range(3)]; BpO = [Buf() for _ in range(3)]
        Sm = [ph.sb(f"Sm{i}", [128, 128], BF16) for i in range(3)]; BSm = [Buf() for _ in range(3)]
        qw = [ph.sb(f"qw{i}", [128, 2, 128], BF16) for i in range(3)]; Bqw = [Buf() for _ in range(3)]
        Oall = ph.sb("Oall", [128, 32, 256], F32); BOall = [Buf() for _ in range(32)]
        bst = ph.sb("bst", [128, 32, 6], F32); Bbst = [Buf() for _ in range(32)]
        mv = ph.sb("mv", [128, 32, 2], F32); Bmv = Buf()
        rs_ = ph.sb("rs", [128, 32], F32); Brs = Buf()
        mh32 = ph.sb("mh32", [128, 32], F32)
        kb.op("dve", lambda: nc.vector.memset(mh32[:], -0.5), [], [Bt])
        on = [ph.sb(f"on{i}", [128, 256], BF16) for i in range(2)]; Bon = [Buf(), Buf()]
        orb = [ph.sb(f"orb{i}", [128, 2, 512], BF16) for i in range(2)]; Borb = [Buf(), Buf()]
        NKT = TK // 128
        for h in range(4):
            fcol = lg[:, h:h + 1]; bcol = lg[:, 4 + h:5 + h]
            ld("sp", kT[:], krT_d[h, :, :], [BkT])
            ld("sp", qT[:], qrT_d[h, :, :], [BqT])
            ld("sp", V[:], vr_d[:, h * 256:(h + 1) * 256].rearrange("(n p) d -> p n d", p=128), [BV])
            ld("sp", sg[:], sgT_d[2 * h:2 * h + 2, :, :].rearrange("c p t -> p c t"), [Bsg])
            act(wq[:, 0, :], tabs[:, 0, :], AF.Exp, [Bt], [Bwq], scale=fcol)
            act(wq[:, 1, :], tabs[:, 1, :], AF.Exp, [Bt], [Bwq], scale=bcol)
            for k in range(6):
                act(wk[:, k:k + 1], pcols[:, k:k + 1], AF.Exp, [Bt], [Bwk], scale=(fcol if k in (0, 2, 3) else bcol))
            act(gch[:, 0:1], fcol, AF.Exp, [Bt], [Bgch], scale=128.0)
            act(gch[:, 1:2], bcol, AF.Exp, [Bt], [Bgch], scale=128.0)
            act(mtmp[:, 0, :], tabs[:, 2, :], AF.Exp, [Bt], [Bmtmp], scale=fcol)
            act(mtmp[:, 1, :], tabs[:, 4, :], AF.Exp, [Bt], [Bmtmp], scale=bcol)
            tt("dve", mtmp[:, 0, :], mtmp[:, 0, :], tabs[:, 3, :], ALU.mult, [Bt], [Bmtmp])
            tt("dve", mtmp[:, 1, :], mtmp[:, 1, :], tabs[:, 5, :], ALU.mult, [Bt], [Bmtmp])
            tt("dve", MT[:], mtmp[:, 0, :], mtmp[:, 1, :], ALU.add, [Bmtmp], [BMT])
            for n in range(NKT):
                ip = n % 2
                tr(ptk[ip][:, 0, :], kT[:, n * 128:(n + 1) * 128], ident_b[:], [BkT, B_const], [Bptk[ip]])
                if n == 0:
                    cf, cb = 2, 4
                elif n == 1:
                    cf, cb = 3, 5
                else:
                    cf, cb = 0, 1
                ts("dve", kw[:, n, 0, :], ptk[ip][:, 0, :], wk[:, cf:cf + 1], None, ALU.mult, None, [Bptk[ip], Bwk], [Bkw])
                ts("dve", kw[:, n, 1, :], ptk[ip][:, 0, :], wk[:, cb:cb + 1], None, ALU.mult, None, [Bptk[ip], Bwk], [Bkw])
            for d_ in range(2):
                mm(pu[d_][:], kw[:, 0, d_, :], V[:, 0, :], True, False, [Bkw, BV], [Bpu[d_]])
                mm(pu[d_][:], kw[:, 1, d_, :], V[:, 1, :], False, True, [Bkw, BV], [Bpu[d_]])
                cp("dve", S32[d_][:], pu[d_][:], [Bpu[d_]], [BS32[d_]])
                first = 0 if d_ == 0 else 31
                cp("act", SB[d_][:, first, :], S32[d_][:], [BS32[d_]], [BSB[d_]])
            for k in range(31):
                for d_ in range(2):
                    c = k if d_ == 0 else 31 - k
                    mm(pu[d_][:], kw[:, 2 + c, d_, :], V[:, 2 + c, :], True, True, [Bkw, BV], [Bpu[d_]])
                    stt(S32[d_][:], S32[d_][:], gch[:, d_:d_ + 1], pu[d_][:], ALU.mult, ALU.add, [Bpu[d_], Bgch], [BS32[d_]])
                    nxtc = c + 1 if d_ == 0 else c - 1
                    cp("act", SB[d_][:, nxtc, :], S32[d_][:], [BS32[d_]], [BSB[d_]])
            for c in range(32):
                i3 = c % 3
                t0 = c * 128
                mm(pS[:], kT[:, LC + t0:LC + t0 + 128], qT[:, t0:t0 + 128], True, True, [BkT, BqT], [BpS])
                tt("dve", Sm[i3][:], pS[:], MT[:], ALU.mult, [BpS, BMT], [BSm[i3]])
                tt("pool", qw[i3][:, 0, :], qT[:, t0:t0 + 128], wq[:, 0, :], ALU.mult, [BqT, Bwq], [Bqw[i3]])
                tt("pool", qw[i3][:, 1, :], qT[:, t0:t0 + 128], wq[:, 1, :], ALU.mult, [BqT, Bwq], [Bqw[i3]])
                mm(pO[i3][:], Sm[i3][:], V[:, 2 + c, :], True, False, [BSm[i3], BV], [BpO[i3]])
                mm(pO[i3][:], qw[i3][:, 0, :], SB[0][:, c, :], False, False, [Bqw[i3], BSB[0]], [BpO[i3]])
                mm(pO[i3][:], qw[i3][:, 1, :], SB[1][:, c, :], False, True, [Bqw[i3], BSB[1]], [BpO[i3]])
                cp("act", Oall[:, c, :], pO[i3][:], [BpO[i3]], [BOall[c]])
                kb.op("dve", lambda c=c: nc.vector.bn_stats(out=bst[:, c, :], in_=Oall[:, c, :]), [BOall[c]], [Bbst[c]])
            for c in range(32):
                kb.op("dve", lambda c=c: nc.vector.bn_aggr(out=mv[:, c, :], in_=bst[:, c, :]), [Bbst[c]], [Bmv])
            kb.op("dve", lambda: nc.vector.tensor_scalar(out=rs_[:], in0=mv[:, :, 1], scalar1=EPS, scalar2=None, op0=ALU.add), [Bmv], [Brs])
            kb.op("pool", lambda: nc.gpsimd.tensor_tensor(out=rs_[:], in0=rs_[:], in1=mh32[:], op=ALU.pow), [Bt], [Brs])
            for c in range(32):
                i2 = c % 2
                t0 = c * 128
                ts("dve", on[i2][:], Oall[:, c, :], mv[:, c, 0:1], rs_[:, c:c + 1], ALU.subtract, ALU.mult, [BOall[c], Bmv, Brs], [Bon[i2]])
                io = (c // 4) % 2
                for k in range(2):
                    tr(ptk[i2][:, k, :], on[i2][:, k * 128:(k + 1) * 128], ident_b[:], [Bon[i2], B_const], [Bptk[i2]])
                for k in range(2):
                    stt(orb[io][:, k, (c % 4) * 128:(c % 4 + 1) * 128], ptk[i2][:, k, :], gn_t[:, 2 * h + k:2 * h + k + 1], sg[:, k, t0:t0 + 128],
                        ALU.mult, ALU.mult, [Bptk[i2], Bt, Bsg], [Borb[io]])
                if c % 4 == 3:
                    tb = (c // 4) * 512
                    ld("pool", orT_d[2 * h:2 * h + 2, :, tb:tb + 512].rearrange("c p t -> p c t"), orb[io][:], [], [Borb[io]])
    if stop_after <= 4:
        kb.finish(); return nc

    w_oa_in = din("w_o_att", [1024, D]); w_or_in = din("w_o_ret", [1024, D]); w_out_in = din("w_out", [D, D])
    w_r_in = din("w_router", [D, 16])
    yT_d = dscr("yT_d", [128, NCH, T], BF16)
    x1_d = dscr("x1_d", [T, D], F32)
    h2_d = dscr("h2_d", [T, D], BF16)
    AFF = G.sb("AFF", [128, 32, 16], F32); BAFF = Buf()
    with Phase(kb) as ph:
        Woa = ph.sb("Woa", [128, 8, D], BF16); Wor = ph.sb("Wor", [128, 8, D], BF16); BWo = Buf()
        BWo_b = Buf()
        ld("pool", Woa[:], w_oa_in.rearrange("(c p) n -> p c n", p=128), [BWo])
        ld("pool", Wor[:], w_or_in.rearrange("(c p) n -> p c n", p=128), [BWo_b])
        oa = [ph.sb(f"oa{i}", [128, 8, 512], BF16) for i in range(2)]; Boa = [Buf(), Buf()]
        orr = [ph.sb(f"orr{i}", [128, 8, 512], BF16) for i in range(2)]; Borr = [Buf(), Buf()]
        ga = [ph.sb(f"ga{i}", [128, 16, 512], BF16) for i in range(2)]; Bga = [Buf(), Buf()]
        gr = [ph.sb(f"gr{i}", [128, 16, 512], BF16) for i in range(2)]; Bgr = [Buf(), Buf()]
        pA = [ph.ps(f"pA{i}", [128, 512], F32) for i in range(2)]; BpA = [Buf(), Buf()]
        pB = [ph.ps(f"pB{i}", [128, 512], F32) for i in range(2)]; BpB = [Buf(), Buf()]
        ta = [ph.sb(f"ta{i}", [128, 512], F32) for i in range(2)]; Bta = [Buf(), Buf()]
        tb_ = [ph.sb(f"tb{i}", [128, 512], F32) for i in range(2)]; Btb = [Buf(), Buf()]
        yTb = [ph.sb(f"yTb{i}", [128, NCH, 512], BF16) for i in range(2)]; ByT = [Buf(), Buf()]
        k = 0
        for blk in range(T // 512):
            ib = blk % 2
            t0 = blk * 512
            ld("sp", oa[ib][:], oaT_d[:, :, t0:t0 + 512].rearrange("c p t -> p c t"), [Boa[ib]])
            ld("sp", orr[ib][:], orT_d[:, :, t0:t0 + 512].rearrange("c p t -> p c t"), [Borr[ib]])
            ld("sp", ga[ib][:], gaT_d[:, :, t0:t0 + 512].rearrange("c p t -> p c t"), [Bga[ib]])
            ld("sp", gr[ib][:], grT_d[:, :, t0:t0 + 512].rearrange("c p t -> p c t"), [Bgr[ib]])
            for dm in range(16):
                i2 = k % 2; k += 1
                for f in range(8):
                    mm(pA[i2][:], Woa[:, f, dm * 128:(dm + 1) * 128], oa[ib][:, f, :], f == 0, f == 7, [BWo, Boa[ib]], [BpA[i2]])
                for f in range(8):
                    mm(pB[i2][:], Wor[:, f, dm * 128:(dm + 1) * 128], orr[ib][:, f, :], f == 0, f == 7, [BWo_b, Borr[ib]], [BpB[i2]])
                tt("dve", ta[i2][:], pA[i2][:], ga[ib][:, dm, :], ALU.mult, [BpA[i2], Bga[ib]], [Bta[i2]])
                tt("dve", tb_[i2][:], pB[i2][:], gr[ib][:, dm, :], ALU.mult, [BpB[i2], Bgr[ib]], [Btb[i2]])
                tt("pool", yTb[ib][:, dm, :], ta[i2][:], tb_[i2][:], ALU.add, [Bta[i2], Btb[i2]], [ByT[ib]])
            ld("pool", yT_d[:, :, t0:t0 + 512], yTb[ib][:], [], [ByT[ib]])
    if stop_after <= 5:
        kb.finish(); return nc

    with Phase(kb) as ph:
        Wo = ph.sb("Wo", [128, NCH, D], BF16); BWo2 = Buf()
        ld("pool", Wo[:], w_out_in.rearrange("(c p) n -> p c n", p=128), [BWo2])
        wr = ph.sb("wr", [128, NCH, 16], F32); Bwr = Buf()
        ld("sp", wr[:], w_r_in.rearrange("(c p) e -> p c e", p=128), [Bwr])
        gt1, Bgt1 = bcast_row(ph, "gt1", mod_d[0:1, 2 * D:3 * D])
        po1, Bpo1 = bcast_row(ph, "po1", post1[0:1, :])
        tt("dve", gt1[:], gt1[:], po1[:], ALU.mult, [Bpo1], [Bgt1])
        g2, Bg2 = bcast_row(ph, "g2", mod_d[0:1, 4 * D:5 * D])
        ld("sp", po1[:], pre2[0:1, :].partition_broadcast(128), [Bpo1])
        stt(g2[:], g2[:], 1.0, po1[:], ALU.add, ALU.mult, [Bpo1], [Bg2])
        s2, Bs2 = bcast_row(ph, "s2", mod_d[0:1, 3 * D:4 * D])
        yTt = [ph.sb(f"yTt{i}", [128, NCH, 128], BF16) for i in range(3)]; ByTt = [Buf() for _ in range(3)]
        xt = [ph.sb(f"xt{i}", [128, D], F32) for i in range(3)]; Bxt = [Buf() for _ in range(3)]
        pY = [ph.ps(f"pY{i}", [128, 512], F32) for i in range(4)]; BpY = [Buf() for _ in range(4)]
        ptr = [ph.ps(f"ptr{i}", [128, 4, 128], F32) for i in range(2)]; Bptr = [Buf(), Buf()]
        pL = ph.ps("pL", [128, 16], F32); BpL = Buf()
        Ysb = [ph.sb(f"Ysb{i}", [128, D], F32) for i in range(2)]; BY = [Buf(), Buf()]
        junk = ph.sb("junk", [128, D], BF16); Bjunk = Buf()
        sq = [ph.sb(f"sq5{i}", [128, 4], F32) for i in range(2)]; Bsq5 = [Buf(), Buf()]
        x1t = [ph.sb(f"x1t{i}", [128, D], F32) for i in range(2)]; Bx1 = [Buf(), Buf()]
        h2f = [ph.sb(f"h2f{i}", [128, D], F32) for i in range(2)]; Bh2f = [Buf(), Buf()]
        h2b = [ph.sb(f"h2b{i}", [128, D], BF16) for i in range(2)]; Bh2b = [Buf(), Buf()]
        h2T = ph.sb("h2T", [128, NCH, 128], F32); Bh2T = Buf()
        lgt = [ph.sb(f"lgt{i}", [128, 16], F32) for i in range(2)]; Blgt = [Buf(), Buf()]
        NT5 = T // 128

        def ld5(i):
            if i < NT5:
                t0_ = i * 128
                ld("sp", yTt[i % 3][:], yT_d[:, :, t0_:t0_ + 128], [ByTt[i % 3]])
                ld("sp", xt[i % 3][:], xk[LC + t0_:LC + t0_ + 128, :], [Bxt[i % 3]])
        ld5(0); ld5(1)

        def s5A(i):
            s = i % 2; s3 = i % 3
            t0 = i * 128
            ld5(i + 2)
            for db in range(4):
                for c in range(NCH):
                    mm(pY[db][:], yTt[s3][:, c, :], Wo[:, c, db * 512:(db + 1) * 512], c == 0, c == NCH - 1, [ByTt[s3], BWo2], [BpY[db]])
            for db in range(4):
                cp("act", Ysb[s][:, db * 512:(db + 1) * 512], pY[db][:], [BpY[db]], [BY[s]])
            act(junk[:], Ysb[s][:], AF.Square, [BY[s]], [Bjunk, Bsq5[s]], accum_out=sq[s][:, 0:1])
            rstd_from(sq[s][:, 1:2], sq[s][:, 0:1], D, [], [Bsq5[s]])
            stt(Ysb[s][:], Ysb[s][:], sq[s][:, 1:2], gt1[:], ALU.mult, ALU.mult, [Bsq5[s], Bgt1], [BY[s]])
            tt("dve", x1t[s][:], Ysb[s][:], xt[s3][:], ALU.add, [BY[s], Bxt[s3]], [Bx1[s]])
            ld("pool", x1_d[t0:t0 + 128, :], x1t[s][:], [], [Bx1[s]])

        def s5B(i):
            s = i % 2
            t0 = i * 128
            act(junk[:], x1t[s][:], AF.Square, [Bx1[s]], [Bjunk, Bsq5[s]], accum_out=sq[s][:, 2:3])
            rstd_from(sq[s][:, 3:4], sq[s][:, 2:3], D, [], [Bsq5[s]])
            stt(h2f[s][:], x1t[s][:], sq[s][:, 3:4], g2[:], ALU.mult, ALU.mult, [Bx1[s], Bsq5[s], Bg2], [Bh2f[s]])
            tt("dve", h2f[s][:], h2f[s][:], s2[:], ALU.add, [Bs2], [Bh2f[s]])

        def s5C(i):
            s = i % 2
            t0 = i * 128
            cp("pool", h2b[s][:], h2f[s][:], [Bh2f[s]], [Bh2b[s]])
            ld("pool", h2_d[t0:t0 + 128, :], h2b[s][:], [], [Bh2b[s]])
            for q4 in range(4):
                ip = q4 % 2
                for c in range(4):
                    cc = q4 * 4 + c
                    tr(ptr[ip][:, c, :], h2f[s][:, cc * 128:(cc + 1) * 128], ident_f[:], [Bh2f[s], B_const], [Bptr[ip]])
                cp("dve" if q4 % 2 == 0 else "act", h2T[:, q4 * 4:(q4 + 1) * 4, :], ptr[ip][:], [Bptr[ip]], [Bh2T])
            for c in range(NCH):
                mm(pL[:], h2T[:, c, :], wr[:, c, :], c == 0, c == NCH - 1, [Bh2T, Bwr], [BpL])
            cp("dve", AFF[:, i, :], pL[:], [BpL], [BAFF])

        s5A(0)
        for i in range(NT5):
            if i + 1 < NT5:
                s5A(i + 1)
            s5B(i)
            s5C(i)
        mx = ph.sb("mx", [128, 32], F32); Bmx = Buf()
        kb.op("dve", lambda: nc.vector.tensor_reduce(out=mx[:], in_=AFF[:], axis=AX.X, op=ALU.max), [BAFF], [Bmx])
        tt("dve", AFF[:], AFF[:], mx[:].unsqueeze(2).to_broadcast([128, 32, 16]), ALU.subtract, [Bmx], [BAFF])
        act(AFF[:], AFF[:], AF.Exp, [], [BAFF])
        kb.op("dve", lambda: nc.vector.tensor_reduce(out=mx[:], in_=AFF[:], axis=AX.X, op=ALU.add), [BAFF], [Bmx])
        kb.op("dve", lambda: nc.vector.reciprocal(out=mx[:], in_=mx[:]), [], [Bmx])
        tt("dve", AFF[:], AFF[:], mx[:].unsqueeze(2).to_broadcast([128, 32, 16]), ALU.mult, [Bmx], [BAFF])
        if "aff_d" in dbg:
            aff_d = dscr("aff_d", [128, 32, 16], F32)
            ld("sp", aff_d[:, :, :], AFF[:], [], [BAFF])
    if stop_after <= 6:
        kb.finish(); return nc

    NE = NEXP
    wg_in = din("w_gate", [NE, D, D]); wu_in = din("w_up", [NE, D, D]); wd_in = din("w_down", [NE, D, D])
    iota_in = din("iota512", [128, 512]); tri_in = din("tri", [128, 128]); tvc_in = din("tvc", [128, 32, 2])
    y2_d = dscr("y2_d", [T, D], F32)
    By2 = Buf()
    pos = G.sb("pos", [128, 32, 16], F32); maskf = G.sb("maskf", [128, 32, 16], F32)
    affh = G.sb("affh", [128, 32, 16], BF16); affl = G.sb("affl", [128, 32, 16], BF16)
    Bsel = Buf()
    with Phase(kb) as ph:
        zt = ph.sb("zt", [128, D], F32); Bz = Buf()
        kb.op("pool", lambda: nc.gpsimd.memset(zt[:], 0.0), [], [Bz])
        for i in range(T // 128):
            kb.dma("sp", lambda i=i: nc.sync.dma_start(out=y2_d[i * 128:(i + 1) * 128, :], in_=zt[:]), [Bz], [])
        lo = ph.sb("lo", [128, 16], F32); hi = ph.sb("hi", [128, 16], F32); mid = ph.sb("mid", [128, 16], F32); Bl = Buf()
        cmpb = ph.sb("cmpb", [128, 32, 16], BF16); Bcmp = Buf()
        partb = ph.sb("partb", [128, 16], BF16); Bpart = Buf()
        selu = ph.sb("selu", [128, 2, 16], U32); Bsu = Buf()
        tri_b = ph.sb("tri_b", [128, 128], BF16); Btri = Buf()
        ld("pool", tri_b[:], tri_in[:, :], [Btri])
        pc = ph.ps("pc", [128, 16], F32); Bpc = Buf()
        pcs = ph.ps("pcs", [128, 512], F32); Bpcs = Buf()
        pw = ph.ps("pw", [128, 512], F32); Bpw = Buf()
        maskb = ph.sb("maskb", [128, 32, 16], BF16); Bmb = Buf()
        tcum = ph.sb("tcum", [128, 32, 16], F32); Btc = Buf()
        kb.op("dve", lambda: nc.vector.memset(lo[:], 0.0), [], [Bl])
        kb.op("dve", lambda: nc.vector.memset(hi[:], 1.0), [], [Bl])
        for it in range(NBISECT):
            tt("dve", mid[:], lo[:], hi[:], ALU.add, [], [Bl])
            ts("dve", mid[:], mid[:], 0.5, None, ALU.mult, None, [], [Bl])
            tt("dve", cmpb[:], AFF[:], mid[:].unsqueeze(1).to_broadcast([128, 32, 16]), ALU.is_ge, [BAFF, Bl], [Bcmp])
            with nc.allow_low_precision(reason="exact small integer counts"):
                kb.op("dve", lambda: nc.vector.tensor_reduce(out=partb[:], in_=cmpb[:].rearrange("p t e -> p e t"), axis=AX.X, op=ALU.add), [Bcmp], [Bpart])
            mm(pc[:], ones_b[:], partb[:], True, True, [Bpart, B_const], [Bpc])
            ts("dve", selu[:, 0, :], pc[:], 511.5, None, ALU.is_ge, None, [Bpc], [Bsu])
            ts("dve", selu[:, 1, :], pc[:], 511.5, None, ALU.is_lt, None, [Bpc], [Bsu])
            kb.op("dve", lambda: nc.vector.copy_predicated(out=lo[:], mask=selu[:, 0, :], data=mid[:]), [Bsu], [Bl])
            kb.op("dve", lambda: nc.vector.copy_predicated(out=hi[:], mask=selu[:, 1, :], data=mid[:]), [Bsu], [Bl])
        tt("dve", maskb[:], AFF[:], lo[:].unsqueeze(1).to_broadcast([128, 32, 16]), ALU.is_ge, [BAFF, Bl], [Bmb])
        cp("dve", maskf[:], maskb[:], [Bmb], [Bsel])
        mbf = maskb[:].rearrange("p t e -> p (t e)")
        mm(pcs[:], ones_b[:], mbf, True, True, [Bmb, B_const], [Bpcs])
        mm(pw[:], tri_b[:], mbf, True, True, [Bmb, Btri], [Bpw])
        kb.op("dve", lambda: nc.vector.memset(tcum[:, 0, :], 0.0), [], [Btc])
        for t in range(1, 32):
            tt("dve", tcum[:, t, :], tcum[:, t - 1, :], pcs[:, (t - 1) * 16:t * 16], ALU.add, [Bpcs], [Btc])
        tt("dve", pos[:].rearrange("p t e -> p (t e)"), pw[:], tcum[:].rearrange("p t e -> p (t e)"), ALU.add, [Bpw, Btc], [Bsel])
        cp("dve", affh[:], AFF[:], [BAFF], [Bsel])
        tt("dve", affl[:], AFF[:], affh[:], ALU.subtract, [BAFF], [Bsel])
        if "sel_d" in dbg:
            sel_d = dscr("sel_d", [2, 128, 32, 16], F32)
            ld("sp", sel_d[0], pos[:], [], [Bsel]); ld("sp", sel_d[1], maskf[:], [], [Bsel])
    if stop_after <= 7:
        kb.finish(); return nc

    with Phase(kb) as ph:
        iota = ph.sb("iota", [128, 512], F32); Bio = Buf()
        ld("sp", iota[:], iota_in[:, :], [Bio])
        tv = [ph.sb(f"tv{i}", [128, 32, 4], BF16) for i in range(2)]; Btv = [Buf(), Buf()]
        for i in range(2):
            ld("pool", tv[i][:, :, 0:2], tvc_in[:, :, :], [Btv[i]])
        Pm = [ph.sb(f"Pm{i}", [128, 32, 128], BF16) for i in range(2)]; BPm = [Buf(), Buf()]
        pidx = ph.ps("pidx", [128, 4], F32); Bpidx = Buf()
        idxf = ph.sb("idxf", [128, 4], F32); Bidxf = Buf()
        idx_i = ph.sb("idx_i", [128, 2, 4], I32); Bidx = [[Buf() for _ in range(4)] for _ in range(2)]
        wsl = ph.sb("wsl", [128, 2, 4], F32)
        xg = ph.sb("xg", [128, 4, D], BF16); Bxg = [Buf() for _ in range(4)]
        ptx = ph.ps("ptx", [128, 8, 128], BF16); Bptx = Buf()
        xgT = [ph.sb(f"xgT{i}", [128, NCH, 512], BF16) for i in range(2)]; BxgT = [Buf(), Buf()]
        NR = 5
        Wr = [ph.sb(f"Wr{i}", [128, NCH, 512], BF16) for i in range(NR)]; BWr = [Buf() for _ in range(NR)]
        pa = [ph.ps(f"pa{i}", [128, 512], F32) for i in range(2)]; Bpa = [Buf(), Buf()]
        pb = [ph.ps(f"pb{i}", [128, 512], F32) for i in range(2)]; Bpb = [Buf(), Buf()]
        pyo = [ph.ps(f"pyo{i}", [128, 512], F32) for i in range(2)]; Bpyo = [Buf(), Buf()]
        sa = [ph.sb(f"sa{i}", [128, 512], F32) for i in range(2)]; Bsa = [Buf(), Buf()]
        hT = ph.sb("hTe", [128, NCH, 512], BF16); BhT = Buf()
        Ysb = ph.sb("Ye", [128, 4, D], BF16); BYe = [Buf() for _ in range(4)]

        def A_tv(e):
            s = e % 2
            cp("pool", tv[s][:, :, 2], affh[:, :, e], [Bsel], [Btv[s]])
            cp("pool", tv[s][:, :, 3], affl[:, :, e], [Bsel], [Btv[s]])

        def A_pm(e, q):
            b = q % 2
            for t in range(32):
                ts("dve", Pm[b][:, t, :], iota[:, q * 128:(q + 1) * 128], pos[:, t, e:e + 1], maskf[:, t, e:e + 1], ALU.is_equal, ALU.mult, [Bio, Bsel], [BPm[b]])

        def A_idx(e, q):
            s = e % 2; b = q % 2
            for t in range(32):
                mm(pidx[:], Pm[b][:, t, :], tv[s][:, t, :], t == 0, t == 31, [BPm[b], Btv[s]], [Bpidx])
            cp("dve", idxf[:], pidx[:], [Bpidx], [Bidxf])
            tt("dve", idx_i[:, s, q:q + 1], idxf[:, 0:1], idxf[:, 1:2], ALU.add, [Bidxf], [Bidx[s][q]])
            tt("dve", wsl[:, s, q:q + 1], idxf[:, 2:3], idxf[:, 3:4], ALU.add, [Bidxf], [Bidx[s][q]])

        def A_gather(e, q):
            s = e % 2
            kb.dma("pool", lambda: nc.gpsimd.indirect_dma_start(
                out=xg[:, q, :], out_offset=None, in_=h2_d[:, :],
                in_offset=bass.IndirectOffsetOnAxis(ap=idx_i[:, s, q:q + 1], axis=0)), [Bidx[s][q]], [Bxg[q]])

        def A_tr(e, q):
            s = e % 2
            for cg in range(2):
                for c in range(8):
                    cc = cg * 8 + c
                    tr(ptx[:, c, :], xg[:, q, cc * 128:(cc + 1) * 128], ident_b[:], [Bxg[q], B_const], [Bptx])
                cp("act" if cg == 0 else "dve", xgT[s][:, cg * 8:(cg + 1) * 8, q * 128:(q + 1) * 128], ptx[:], [Bptx], [BxgT[s]])

        pieces = []
        for e in range(NE):
            for fg in range(4):
                pieces.append((wg_in, e, fg)); pieces.append((wu_in, e, fg))
            for db in range(4):
                pieces.append((wd_in, e, db))
        pstate = dict(loaded=0)

        def load_piece(k):
            wt, e, j = pieces[k]
            r = k % NR
            wvw = wt[e].rearrange("(c p) n -> p c n", p=128)
            ld("pool", Wr[r][:], wvw[:, :, j * 512:(j + 1) * 512], [BWr[r]])

        def need(k):
            while pstate["loaded"] <= min(k + NR - 2, len(pieces) - 1):
                load_piece(pstate["loaded"]); pstate["loaded"] += 1

        def stageB(e, hooks):
            s = e % 2
            kbase = e * 12
            ii = 0
            for fg in range(4):
                kg = kbase + fg * 2; ku = kg + 1
                need(ku)
                rg = kg % NR; ru = ku % NR
                for fc in range(4):
                    i2 = ii % 2; ii += 1
                    for c in range(NCH):
                        mm(pa[i2][:], Wr[rg][:, c, fc * 128:(fc + 1) * 128], xgT[s][:, c, :], c == 0, c == NCH - 1, [BWr[rg], BxgT[s]], [Bpa[i2]])
                    for c in range(NCH):
                        mm(pb[i2][:], Wr[ru][:, c, fc * 128:(fc + 1) * 128], xgT[s][:, c, :], c == 0, c == NCH - 1, [BWr[ru], BxgT[s]], [Bpb[i2]])
                    act(sa[i2][:], pa[i2][:], AF.Silu, [Bpa[i2]], [Bsa[i2]])
                    tt("dve", hT[:, fg * 4 + fc, :], sa[i2][:], pb[i2][:], ALU.mult, [Bsa[i2], Bpb[i2]], [BhT])
                hooks("fg", fg)
            jj = 0
            for db in range(4):
                kd = kbase + 8 + db
                need(kd)
                rd_ = kd % NR
                for sc in range(4):
                    i2 = jj % 2; jj += 1
                    for fcn in range(NCH):
                        mm(pyo[i2][:], hT[:, fcn, sc * 128:(sc + 1) * 128], Wr[rd_][:, fcn, :], fcn == 0, fcn == NCH - 1, [BhT, BWr[rd_]], [Bpyo[i2]])
                    if jj % 2 == 0:
                        act(Ysb[:, sc, db * 512:(db + 1) * 512], pyo[i2][:], AF.Copy, [Bpyo[i2], Bidx[s][sc]], [BYe[sc]], scale=wsl[:, s, sc:sc + 1])
                    else:
                        ts("dve", Ysb[:, sc, db * 512:(db + 1) * 512], pyo[i2][:], wsl[:, s, sc:sc + 1], None, ALU.mult, None, [Bpyo[i2], Bidx[s][sc]], [BYe[sc]])
                hooks("db", db)
            for sc in range(4):
                kb.dma("pool", lambda sc=sc, s=s: nc.gpsimd.indirect_dma_start(
                    out=y2_d[:, :], out_offset=bass.IndirectOffsetOnAxis(ap=idx_i[:, s, sc:sc + 1], axis=0),
                    in_=Ysb[:, sc, :], in_offset=None, compute_op=ALU.add), [BYe[sc], Bidx[s][sc]], [By2])

        kb.barrier()
        A_tv(0)
        for q in range(4):
            A_pm(0, q); A_idx(0, q); A_gather(0, q)
        for q in range(4):
            A_tr(0, q)
        for e in range(NE):
            nx = e + 1

            def hooks(kind, j, nx=nx):
                if nx >= NE:
                    return
                if kind == "fg":
                    if j >= 1:
                        A_gather(nx, j - 1)
                    A_idx(nx, j)
                    if j + 1 < 4:
                        A_pm(nx, j + 1)
                else:
                    if j == 0:
                        A_gather(nx, 3)
                    A_tr(nx, j)
            if nx < NE:
                A_tv(nx)
                A_pm(nx, 0)
            stageB(e, hooks)
    if stop_after <= 8:
        kb.finish(); return nc

    with Phase(kb) as ph:
        gt2, Bgt2 = bcast_row(ph, "gt2", mod_d[0:1, 5 * D:6 * D])
        po2, Bpo2 = bcast_row(ph, "po2", post2[0:1, :])
        tt("dve", gt2[:], gt2[:], po2[:], ALU.mult, [Bpo2], [Bgt2])
        yt = [ph.sb(f"y2t{i}", [128, D], F32) for i in range(3)]; Byt = [Buf() for _ in range(3)]
        x1t = [ph.sb(f"x1u{i}", [128, D], F32) for i in range(3)]; Bx1 = [Buf() for _ in range(3)]
        junk = ph.sb("junk8", [128, D], BF16); Bjunk = Buf()
        sq = [ph.sb(f"sq8{i}", [128, 2], F32) for i in range(2)]; Bsq = [Buf(), Buf()]
        ot = [ph.sb(f"ot{i}", [128, D], F32) for i in range(2)]; Bot = [Buf(), Buf()]
        NT8 = T // 128

        def ld8(i):
            if i < NT8:
                ld("sp", yt[i % 3][:], y2_d[i * 128:(i + 1) * 128, :], [Byt[i % 3]])
                ld("sp", x1t[i % 3][:], x1_d[i * 128:(i + 1) * 128, :], [Bx1[i % 3]])
        ld8(0); ld8(1)
        for i in range(NT8):
            s = i % 2; s3 = i % 3
            t0 = i * 128
            ld8(i + 2)
            act(junk[:], yt[s3][:], AF.Square, [Byt[s3]], [Bjunk, Bsq[s]], accum_out=sq[s][:, 0:1])
            rstd_from(sq[s][:, 1:2], sq[s][:, 0:1], D, [], [Bsq[s]])
            stt(yt[s3][:], yt[s3][:], sq[s][:, 1:2], gt2[:], ALU.mult, ALU.mult, [Bsq[s], Bgt2], [Byt[s3]])
            tt("dve", ot[s][:], yt[s3][:], x1t[s3][:], ALU.add, [Byt[s3], Bx1[s3]], [Bot[s]])
            ld("pool", out[t0:t0 + 128, :], ot[s][:], [], [Bot[s]])

    kb.finish()
    return nc


def make_inputs(inp, b):
    L = 0
    Rm, cosT, sinT = rope_consts()
    m = dict(
        xk=np.ascontiguousarray(np.concatenate([inp["ctx"][b], inp["x"][b]], axis=0)),
        c2=np.ascontiguousarray(np.stack([inp["c"][b], inp["c_ctx"]], axis=0)),
        w_mod=inp["w_mod"][L], b_mod=inp["b_mod"][L][None, :],
        pre1=inp["pre_norm1"][L][None, :], post1=inp["post_norm1"][L][None, :],
        pre2=inp["pre_norm2"][L][None, :], post2=inp["post_norm2"][L][None, :],
        w_in=inp["w_in"][L],
        ident=np.eye(128, dtype=np.float32),
        q_norm=inp["q_norm"][L][:, None], k_norm=inp["k_norm"][L][:, None],
        rotm=Rm, cosT=cosT, sinT=sinT,
        w_o_att=inp["w_o_att"][L], w_o_ret=inp["w_o_ret"][L], w_out=inp["w_out"][L], w_router=inp["w_router"][L],
        w_gate=inp["w_gate"][L][:NEXP], w_up=inp["w_up"][L][:NEXP], w_down=inp["w_down"][L][:NEXP],
        iota512=np.ascontiguousarray(np.broadcast_to(np.arange(512, dtype=np.float32), (128, 512))),
        tri=np.triu(np.ones((128, 128), np.float32), 1),
        tvc=np.ascontiguousarray(np.stack([np.broadcast_to(np.arange(128, dtype=np.float32)[:, None], (128, 32)),
                                           np.broadcast_to(128.0 * np.arange(32, dtype=np.float32)[None, :], (128, 32))], axis=-1)),
        ret_decay=inp["ret_decay"][L].reshape(1, 8), ret_gn=np.ascontiguousarray(inp["ret_gn"][L].reshape(8, 128).T),
    )
    ii = np.arange(128, dtype=np.float32)
    jj = ii[:, None]; i2 = ii[None, :]
    tabs = np.stack([np.broadcast_to(i2 + 1, (128, 128)), np.broadcast_to(128 - i2, (128, 128)),
                     np.maximum(i2 - jj, 0), (i2 >= jj).astype(np.float32),
                     np.maximum(jj - i2, 0), (jj >= i2).astype(np.float32)]).astype(np.float32)
    m["ret_tabs"] = np.ascontiguousarray(tabs)
    p = ii
    m["ret_pcols"] = np.ascontiguousarray(np.stack([127 - p, p, 255 - p, 127 - p, p, 128 + p], axis=1).astype(np.float32))
    return m


_NC_CACHE = {}


def kernel(**inputs):
    inp = {k: np.asarray(v) for k, v in inputs.items()}
    if "nc" not in _NC_CACHE:
        _NC_CACHE["nc"] = build()
    nc = _NC_CACHE["nc"]
    B = inp["x"].shape[0]
    maps = [make_inputs(inp, b) for b in range(B)]
    in_maps = [maps[i % B] for i in range(8)]
    res = run_bass_kernel_spmd(nc, in_maps, core_ids=list(range(8)))
    out = np.stack([np.asarray(res.results[b]["out"]) for b in range(B)], axis=0)
    return out.astype(np.float32)
```

```python
import numpy as np
import contextlib
import concourse.bass as bass
import concourse.mybir as mybir
from concourse.bass_utils import run_bass_kernel_spmd

F32 = mybir.dt.float32
BF16 = mybir.dt.bfloat16
I32 = mybir.dt.int32
U32 = mybir.dt.uint32
AF = mybir.ActivationFunctionType
ALU = mybir.AluOpType
AX = mybir.AxisListType

D = 2048
T = 4096
LC = 256
TK = T + LC
NCH = 16
QW = 6656
INW = 8704
EPS = 1e-6


class Buf:
    __slots__ = ("w", "r", "name")

    def __init__(self, name=""):
        self.w = None
        self.r = {}
        self.name = name


class KB:
    def __init__(self, nc):
        self.nc = nc
        self.es = contextlib.ExitStack()
        self.E = dict(pe=nc.tensor, act=nc.scalar, dve=nc.vector, pool=nc.gpsimd, sp=nc.sync)
        self.clk = {}
        self.seen = {e: {} for e in self.E}
        self.nsem = 0
        for e in ("pe", "act", "dve", "pool"):
            self._new_clk(e)
        self.dpool = {}
        for q, n in (("sp", 20), ("pool", 12), ("act", 4)):
            self.dpool[q] = [[self._sem(f"d{q}{i}"), 0] for i in range(n)]
        self.dnext = {q: 0 for q in self.dpool}

    def _sem(self, name):
        self.nsem += 1
        sm = self.es.enter_context(self.nc.semaphore(name + f"_{self.nsem}"))
        if not hasattr(self, "_keep"):
            self._keep = []
        self._keep.append(sm)
        return sm

    def _new_clk(self, e):
        self.clk[e] = [self._sem("clk" + e), 0]

    def wait(self, eng, tok):
        sem, val = tok
        k = id(sem)
        if eng == "pe" and sem is self.clk["pe"][0]:
            return
        if self.seen[eng].get(k, 0) >= val:
            return
        self.E[eng].wait_ge(sem, val)
        self.seen[eng][k] = val

    def _deps(self, eng, reads, writes):
        for b in reads:
            if b.w is not None:
                self.wait(eng, b.w)
        for b in writes:
            if b.w is not None:
                self.wait(eng, b.w)
            for k, (sem, val) in b.r.items():
                self.wait(eng, (sem, val))

    def _commit(self, tok, reads, writes):
        sem, val = tok
        k = id(sem)
        for b in reads:
            if k not in b.r or b.r[k][1] < val:
                b.r[k] = (sem, val)
        for b in writes:
            b.w = tok
            b.r = {}

    def op(self, eng, fn, reads=(), writes=()):
        self._deps(eng, reads, writes)
        ins = fn()
        c = self.clk[eng]
        c[1] += 1
        ins.then_inc(c[0], 1)
        tok = (c[0], c[1])
        self._commit(tok, reads, writes)
        return tok

    def dma(self, q, fn, reads=(), writes=()):
        self._deps(q, reads, writes)
        pool = self.dpool[q]
        i = self.dnext[q]
        self.dnext[q] = (i + 1) % len(pool)
        ent = pool[i]
        if ent[1] > 0:
            self.wait(q, (ent[0], ent[1]))
        ins = fn()
        ent[1] += 16
        ins.then_inc(ent[0], 16)
        tok = (ent[0], ent[1])
        self._commit(tok, reads, writes)
        return tok

    def _alltoks(self):
        toks = []
        for e, c in self.clk.items():
            if c[1] > 0:
                toks.append((c[0], c[1]))
        for q, pool in self.dpool.items():
            for ent in pool:
                if ent[1] > 0:
                    toks.append((ent[0], ent[1]))
        return toks

    def barrier(self):
        toks = self._alltoks()
        for e in self.E:
            for t in toks:
                self.wait(e, t)

    def new_phase(self):
        self.barrier()
        for e in ("pe", "act", "dve", "pool"):
            if self.clk[e][1] > 0:
                self._new_clk(e)

    def finish(self, eng="sp"):
        for t in self._alltoks():
            self.wait(eng, t)


class Phase:
    def __init__(self, kb):
        self.kb = kb
        self.es = contextlib.ExitStack()

    def __enter__(self):
        return self

    def __exit__(self, *a):
        self.kb.new_phase()
        self.es.close()
        return False

    _n = [0]

    def sb(self, name, shape, dt):
        Phase._n[0] += 1
        t = self.es.enter_context(self.kb.nc.sbuf_tensor(f"{name}_s{Phase._n[0]}", list(shape), dt))
        return t

    def ps(self, name, shape, dt):
        Phase._n[0] += 1
        return self.es.enter_context(self.kb.nc.psum_tensor(f"{name}_p{Phase._n[0]}", list(shape), dt))


def rope_consts():
    Rm = np.zeros((128, 128), np.float32)
    for a in range(2):
        for j in range(32):
            Rm[a * 64 + 32 + j, a * 64 + j] = -1.0
            Rm[a * 64 + j, a * 64 + 32 + j] = 1.0
    t = np.arange(T)
    r = (t // 64).astype(np.float32)
    cl = (t % 64).astype(np.float32)
    inv = (10000.0 ** (-np.arange(32, dtype=np.float32) / 32)).astype(np.float32)
    ang_r = r[:, None] * inv
    ang_c = cl[:, None] * inv
    ang = np.concatenate([ang_r, ang_r, ang_c, ang_c], axis=-1)
    cosT = np.ascontiguousarray(np.cos(ang).T.astype(np.float32))
    sinT = np.ascontiguousarray(np.sin(ang).T.astype(np.float32))
    return Rm, cosT, sinT


P4STOP = 0
P4VAR = 2
SKIP3 = 0
NEXP = 16
NBISECT = 23


def build(dbg=(), stop_after=99):
    nc = bass.Bass("TRN2", target_bir_lowering=False)
    kb = KB(nc)
    dbg = set(dbg)

    def din(name, shape, dt=F32):
        return nc.dram_tensor(name, list(shape), dt, kind="ExternalInput").ap()

    def dscr(name, shape, dt):
        kind = "ExternalOutput" if name in dbg else "Internal"
        return nc.dram_tensor(name, list(shape), dt, kind=kind).ap()

    xk = din("xk", [TK, D])
    c2 = din("c2", [2, D])
    w_mod = din("w_mod", [D, 6 * D])
    b_mod = din("b_mod", [1, 6 * D])
    pre1 = din("pre1", [1, D]); post1 = din("post1", [1, D]); pre2 = din("pre2", [1, D]); post2 = din("post2", [1, D])
    w_in = din("w_in", [D, INW])
    ident_in = din("ident", [128, 128])
    out = nc.dram_tensor("out", [T, D], F32, kind="ExternalOutput").ap()

    mod_d = dscr("mod_d", [2, 6 * D], F32)
    hT_d = dscr("hT_d", [128, NCH, TK], BF16)

    G = Phase(kb)
    ident_f = G.sb("ident_f", [128, 128], F32)
    ident_b = G.sb("ident_b", [128, 128], BF16)
    eps_t = G.sb("eps_t", [128, 1], F32)
    B_const = Buf("const")
    kb.dma("sp", lambda: nc.sync.dma_start(out=ident_f[:], in_=ident_in[:, :]), writes=[B_const])
    kb.dma("pool", lambda: nc.gpsimd.dma_start(out=ident_b[:], in_=ident_in[:, :]), writes=[B_const])
    kb.op("dve", lambda: nc.vector.memset(eps_t[:], EPS), writes=[B_const])
    mhalf = G.sb("mhalf", [128, 1], F32)
    kb.op("dve", lambda: nc.vector.memset(mhalf[:], -0.5), writes=[B_const])

    def rstd_from(out_ap, ssq_ap, n, R, W):
        kb.op("dve", lambda: nc.vector.tensor_scalar(out=out_ap, in0=ssq_ap, scalar1=1.0 / n, scalar2=EPS, op0=ALU.mult, op1=ALU.add), R, W)
        kb.op("pool", lambda: nc.gpsimd.tensor_tensor(out=out_ap, in0=out_ap, in1=mhalf[:], op=ALU.pow), [B_const], W)
    kb.barrier()

    with Phase(kb) as ph:
        c2T = ph.sb("c2T", [128, 2, 16], F32)
        s2T = ph.sb("s2T", [128, 2, 16], BF16)
        bm2 = ph.sb("bm2", [2, 6 * D], F32)
        B_c2 = Buf(); B_s2 = Buf(); B_bm = Buf()
        for r in range(2):
            kb.dma("sp", lambda r=r: nc.sync.dma_start(out=c2T[:, r, :], in_=c2[r, :].rearrange("(p c) -> p c", c=16)), writes=[B_c2])
            kb.dma("sp", lambda r=r: nc.sync.dma_start(out=bm2[r:r + 1, :], in_=b_mod[:, :]), writes=[B_bm])
        kb.op("act", lambda: nc.scalar.activation(out=s2T[:], in_=c2T[:], func=AF.Silu), reads=[B_c2], writes=[B_s2])
        wmv = w_mod.rearrange("(p c) n -> p c n", c=16)
        NB = 24
        NWB = 3
        wb = [ph.sb(f"wmb{i}", [128, 16, 512], BF16) for i in range(NWB)]
        Bwb = [Buf() for _ in range(NWB)]
        pm = [ph.ps(f"pm{i}", [2, 512], F32) for i in range(2)]
        Bpm = [Buf(), Buf()]
        mrow = [ph.sb(f"mrow{i}", [2, 512], F32) for i in range(2)]
        Bmr = [Buf(), Buf()]

        def ldw(j):
            s_ = j % NWB
            kb.dma("pool", lambda j=j, s_=s_: nc.gpsimd.dma_start(out=wb[s_][:], in_=wmv[:, :, j * 512:(j + 1) * 512]), writes=[Bwb[s_]])
        ldw(0); ldw(1)
        for j in range(NB):
            s = j % 2; sw = j % NWB
            if j + 2 < NB:
                ldw(j + 2)
            for c in range(16):
                kb.op("pe", lambda c=c, s=s, sw=sw: nc.tensor.matmul(pm[s][:], lhsT=s2T[:, :, c], rhs=wb[sw][:, c, :], start=(c == 0), stop=(c == 15)),
                      reads=[B_s2, Bwb[sw]], writes=[Bpm[s]])
            kb.op("dve", lambda j=j, s=s: nc.vector.tensor_tensor(out=mrow[s][:], in0=pm[s][:], in1=bm2[:, j * 512:(j + 1) * 512], op=ALU.add),
                  reads=[Bpm[s], B_bm], writes=[Bmr[s]])
            kb.dma("sp", lambda j=j, s=s: nc.sync.dma_start(out=mod_d[:, j * 512:(j + 1) * 512], in_=mrow[s][:]), reads=[Bmr[s]])
    if stop_after <= 0:
        kb.finish(); return nc

    def bcast_row(ph, name, src_ap):
        t = ph.sb(name, [128, D], F32)
        b = Buf(name)
        kb.dma("sp", lambda: nc.sync.dma_start(out=t[:], in_=src_ap.partition_broadcast(128)), writes=[b])
        return t, b

    with Phase(kb) as ph:
        g1, Bg1 = bcast_row(ph, "g1", pre1[0:1, :])
        Gm = []
        for r in range(2):
            sc, Bsc = bcast_row(ph, f"sc{r}", mod_d[r:r + 1, D:2 * D])
            sh, Bsh = bcast_row(ph, f"sh{r}", mod_d[r:r + 1, 0:D])
            kb.op("dve", lambda sc=sc: nc.vector.scalar_tensor_tensor(out=sc[:], in0=sc[:], scalar=1.0, in1=g1[:], op0=ALU.add, op1=ALU.mult),
                  reads=[Bg1], writes=[Bsc])
            Gm.append((sc, Bsc, sh, Bsh))
        NT = TK // 128
        xt = [ph.sb(f"xt{i}", [128, D], F32) for i in range(3)]; Bxt = [Buf() for _ in range(3)]
        junk = ph.sb("junk", [128, D], BF16); Bjunk = Buf()
        ssq = [ph.sb(f"ssq{i}", [128, 1], F32) for i in range(2)]; Bssq = [Buf(), Buf()]
        rstd = [ph.sb(f"rstd{i}", [128, 1], F32) for i in range(2)]; Brstd = [Buf(), Buf()]
        h1 = [ph.sb(f"h1{i}", [128, D], F32) for i in range(2)]; Bh1 = [Buf(), Buf()]
        hb = [ph.sb(f"hb{i}", [128, D], BF16) for i in range(2)]; Bhb = [Buf(), Buf()]
        pt = [ph.ps(f"pt{i}", [128, 8, 128], BF16) for i in range(4)]; Bpt = [Buf() for _ in range(4)]
        hT = [ph.sb(f"hTt{i}", [128, NCH, 512], BF16) for i in range(2)]; BhT = [Buf(), Buf()]

        def ldx(i):
            if i < NT:
                kb.dma("sp", lambda: nc.sync.dma_start(out=xt[i % 3][:], in_=xk[i * 128:(i + 1) * 128, :]), writes=[Bxt[i % 3]])
        ldx(0); ldx(1)

        def stA(i):
            s3 = i % 3; s = i % 2
            ldx(i + 2)
            r = 1 if i < LC // 128 else 0
            sc, Bsc, sh, Bsh = Gm[r]
            kb.op("act", lambda: nc.scalar.activation(out=junk[:], in_=xt[s3][:], func=AF.Square, accum_out=ssq[s][:]),
                  reads=[Bxt[s3]], writes=[Bjunk, Bssq[s]])
            rstd_from(rstd[s][:], ssq[s][:], D, [Bssq[s]], [Brstd[s]])
            kb.op("dve", lambda: nc.vector.scalar_tensor_tensor(out=h1[s][:], in0=xt[s3][:], scalar=rstd[s][:], in1=sc[:], op0=ALU.mult, op1=ALU.mult),
                  reads=[Bxt[s3], Brstd[s], Bsc], writes=[Bh1[s]])
            kb.op("pool", lambda: nc.gpsimd.tensor_tensor(out=hb[s][:], in0=h1[s][:], in1=sh[:], op=ALU.add),
                  reads=[Bh1[s], Bsh], writes=[Bhb[s]])

        def stB(i):
            s = i % 2
            if i < 2:
                grp, gi, gn_ = 0, i, 2
            else:
                grp, gi, gn_ = 1 + (i - 2) // 4, (i - 2) % 4, 4
            sg_ = grp % 2
            for hf in range(2):
                pi = (2 * i + hf) % 4
                for c in range(8):
                    cc = hf * 8 + c
                    kb.op("pe", lambda c=c, cc=cc: nc.tensor.transpose(out=pt[pi][:, c, :], in_=hb[s][:, cc * 128:(cc + 1) * 128], identity=ident_b[:]),
                          reads=[Bhb[s]], writes=[Bpt[pi]])
                dst = hT[sg_][:, hf * 8:(hf + 1) * 8, gi * 128:(gi + 1) * 128]
                if hf == 0:
                    kb.op("act", lambda: nc.scalar.copy(out=dst, in_=pt[pi][:]), reads=[Bpt[pi]], writes=[BhT[sg_]])
                else:
                    kb.op("dve", lambda: nc.vector.tensor_copy(out=dst, in_=pt[pi][:]), reads=[Bpt[pi]], writes=[BhT[sg_]])
            if gi == gn_ - 1:
                tk0 = 0 if grp == 0 else LC + (grp - 1) * 512
                nn = gn_ * 128
                for hh in range(2):
                    kb.dma("sp", lambda hh=hh: nc.sync.dma_start(out=hT_d[:, hh * 8:(hh + 1) * 8, tk0:tk0 + nn], in_=hT[sg_][:, hh * 8:(hh + 1) * 8, :nn]), reads=[BhT[sg_]])

        for i in range(NT + 1):
            if i < NT:
                stA(i)
            if i >= 1:
                stB(i - 1)
    if stop_after <= 1:
        kb.finish(); return nc

    def mm(out_, lhsT, rhs, st, sp, R, W):
        return kb.op("pe", lambda: nc.tensor.matmul(out_, lhsT=lhsT, rhs=rhs, start=st, stop=sp), R, W)

    def tr(out_, in_, idn, R, W):
        return kb.op("pe", lambda: nc.tensor.transpose(out=out_, in_=in_, identity=idn), R, W)

    def act(out_, in_, func, R, W, **kw):
        return kb.op("act", lambda: nc.scalar.activation(out=out_, in_=in_, func=func, **kw), R, W)

    def tt(eng, out_, in0, in1, op, R, W):
        e = nc.vector if eng == "dve" else nc.gpsimd
        return kb.op(eng, lambda: e.tensor_tensor(out=out_, in0=in0, in1=in1, op=op), R, W)

    def stt(out_, in0, scalar, in1, op0, op1, R, W):
        return kb.op("dve", lambda: nc.vector.scalar_tensor_tensor(out=out_, in0=in0, scalar=scalar, in1=in1, op0=op0, op1=op1), R, W)

    def ts(eng, out_, in0, s1, s2, op0, op1, R, W):
        e = nc.vector if eng == "dve" else nc.gpsimd
        if op1 is None:
            return kb.op(eng, lambda: e.tensor_scalar(out=out_, in0=in0, scalar1=s1, scalar2=None, op0=op0), R, W)
        return kb.op(eng, lambda: e.tensor_scalar(out=out_, in0=in0, scalar1=s1, scalar2=s2, op0=op0, op1=op1), R, W)

    def cp(eng, out_, in_, R, W):
        if eng == "act":
            return kb.op("act", lambda: nc.scalar.copy(out=out_, in_=in_), R, W)
        e = nc.vector if eng == "dve" else nc.gpsimd
        return kb.op(eng, lambda: e.tensor_copy(out=out_, in_=in_), R, W)

    def ld(q, out_, in_, W, R=()):
        e = nc.sync if q == "sp" else nc.gpsimd
        return kb.dma(q, lambda: e.dma_start(out=out_, in_=in_), R, W)

    qn_in = din("q_norm", [128, 1]); kn_in = din("k_norm", [128, 1])
    rm_in = din("rotm", [128, 128]); cos_in = din("cosT", [128, T]); sin_in = din("sinT", [128, T])
    qaT_d = dscr("qaT_d", [8, 128, T], BF16); qrT_d = dscr("qrT_d", [4, 128, T], BF16)
    sgT_d = dscr("sgT_d", [8, 128, T], BF16); gaT_d = dscr("gaT_d", [16, 128, T], BF16); grT_d = dscr("grT_d", [16, 128, T], BF16)
    kaT_d = dscr("kaT_d", [2, 128, TK], BF16); krT_d = dscr("krT_d", [4, 128, TK], BF16)
    va_d = dscr("va_d", [TK, 256], BF16); vr_d = dscr("vr_d", [TK, 1024], BF16)
    ones_b = G.sb("ones_b", [128, 128], BF16)
    kb.op("dve", lambda: nc.vector.memset(ones_b[:], 1.0), writes=[B_const])
    kb.barrier()

    with Phase(kb) as ph:
        qn_t = ph.sb("qn_t", [128, 1], F32); kn_t = ph.sb("kn_t", [128, 1], F32)
        rm_b = ph.sb("rm_b", [128, 128], BF16)
        cos_t = ph.sb("cos_t", [128, T], F32); sin_t = ph.sb("sin_t", [128, T], F32)
        Bc = Buf()
        ld("sp", qn_t[:], qn_in[:, :], [Bc]); ld("sp", kn_t[:], kn_in[:, :], [Bc])
        ld("pool", rm_b[:], rm_in[:, :], [Bc])
        ld("sp", cos_t[:], cos_in[:, :], [Bc]); ld("sp", sin_t[:], sin_in[:, :], [Bc])
        wv = w_in.rearrange("(c p) n -> p c n", p=128)
        Wt = [ph.sb(f"Wt{i}", [128, NCH, 768], BF16) for i in range(2)]; BW = [Buf(), Buf()]
        hB = [ph.sb(f"hB{i}", [128, NCH, 512], BF16) for i in range(2)]; BhB = [Buf(), Buf()]
        pz = [ph.ps(f"pz{i}", [128, 512], F32) for i in range(4)]; Bpz = [Buf() for _ in range(4)]
        pq = [ph.ps(f"pq{i}", [128, 512], F32) for i in range(2)]; Bpq = [Buf() for _ in range(2)]
        pr = [ph.ps(f"pr{i}", [128, 512], F32) for i in range(2)]; Bpr = [Buf() for _ in range(2)]
        sqb = [ph.sb(f"sqb{i}", [128, 512], BF16) for i in range(2)]; Bsq = [Buf() for _ in range(2)]
        sd = [ph.sb(f"sd{i}", [128, 512], F32) for i in range(2)]; Bsd = [Buf() for _ in range(2)]
        qnb = [ph.sb(f"qnb{i}", [128, 512], BF16) for i in range(3)]; Bqn = [Buf() for _ in range(3)]
        t1 = [ph.sb(f"t1{i}", [128, 512], F32) for i in range(2)]; Bt1 = [Buf() for _ in range(2)]
        t2 = [ph.sb(f"t2{i}", [128, 512], F32) for i in range(2)]; Bt2 = [Buf() for _ in range(2)]
        ob = [ph.sb(f"ob{i}", [128, 512], BF16) for i in range(4)]; Bob = [Buf() for _ in range(4)]
        cnt = dict(z=0, q=0, r=0, sq=0, sd=0, qn=0, t=0, ob=0, w=0, h=0)
        deferred = []

        def nxt(k, n):
            v = cnt[k] % n
            cnt[k] += 1
            return v

        def run_deferred():
            todo = list(deferred)
            deferred.clear()
            for f in todo:
                f()

        def rope_and_store(iq, N, t0, dst):
            ir = nxt("r", 2)
            mm(pr[ir][:, :N], rm_b[:], qnb[iq][:, :N], True, True, [Bqn[iq], Bc], [Bpr[ir]])

            def fin():
                it = nxt("t", 2); io = nxt("ob", 4)
                tt("dve", t1[it][:, :N], qnb[iq][:, :N], cos_t[:, t0:t0 + N], ALU.mult, [Bqn[iq], Bc], [Bt1[it]])
                tt("dve", t2[it][:, :N], pr[ir][:, :N], sin_t[:, t0:t0 + N], ALU.mult, [Bpr[ir], Bc], [Bt2[it]])
                tt("pool", ob[io][:, :N], t1[it][:, :N], t2[it][:, :N], ALU.add, [Bt1[it], Bt2[it]], [Bob[io]])
                ld("pool", dst, ob[io][:, :N], [], [Bob[io]])
            deferred.append(fin)

        def evac(kind, iz, N, t0, dst, rope):
            if kind in ("silu", "sig"):
                io = nxt("ob", 4)
                act(ob[io][:, :N], pz[iz][:, :N], AF.Silu if kind == "silu" else AF.Sigmoid, [Bpz[iz]], [Bob[io]])
                ld("pool", dst, ob[io][:, :N], [], [Bob[io]])
                return
            if kind[0] == "r":
                iq = nxt("qn", 3)
                sc = (128.0 ** -0.5) if kind == "rk" else 1.0
                act(qnb[iq][:, :N], pz[iz][:, :N], AF.Copy, [Bpz[iz]], [Bqn[iq]], scale=sc)
                if rope:
                    deferred.append(lambda: rope_and_store(iq, N, t0, dst))
                else:
                    ld("pool", dst, qnb[iq][:, :N], [], [Bqn[iq]])
                return
            gt = qn_t if kind == "nq" else kn_t
            isq = nxt("sq", 2)
            act(sqb[isq][:, :N], pz[iz][:, :N], AF.Square, [Bpz[iz]], [Bsq[isq]])

            def st2():
                ip = nxt("q", 2)
                mm(pq[ip][:, :N], ones_b[:], sqb[isq][:, :N], True, True, [Bsq[isq], B_const], [Bpq[ip]])
                isd = nxt("sd", 2)
                act(sd[isd][:, :N], pq[ip][:, :N], AF.Sqrt, [Bpq[ip], B_const], [Bsd[isd]], bias=eps_t[:], scale=1.0 / 128)
                kb.op("dve", lambda: nc.vector.reciprocal(out=sd[isd][:, :N], in_=sd[isd][:, :N]), [], [Bsd[isd]])
                iq = nxt("qn", 3)
                stt(qnb[iq][:, :N], pz[iz][:, :N], gt[:], sd[isd][:, :N], ALU.mult, ALU.mult, [Bpz[iz], Bsd[isd], Bc], [Bqn[iq]])
                if rope:
                    deferred.append(lambda: rope_and_store(iq, N, t0, dst))
                else:
                    ld("pool", dst, qnb[iq][:, :N], [], [Bqn[iq]])
            deferred.append(st2)

        groups = []
        for g in range(2):
            groups.append((g * 512, 512, "nq", qaT_d, g * 4, False))
        groups.append((1024, 512, "rq", qrT_d, 0, False))
        for g in range(2):
            groups.append((1536 + g * 512, 512, "silu", sgT_d, g * 4, False))
        for g in range(4):
            groups.append((2560 + g * 512, 512, "sig", gaT_d, g * 4, False))
        for g in range(4):
            groups.append((4608 + g * 512, 512, "sig", grT_d, g * 4, False))
        groups.append((-1, 768, "k", None, 0, True))
        for (c0, ncol, kind, dstT, h0, kv) in groups:
            iw = nxt("w", 2)
            if kind == "k":
                BWa = Buf(); BWb = Buf()
                ld("pool", Wt[iw][:, :, 0:256], wv[:, :, 6656:6912], [BW[iw]])
                ld("pool", Wt[iw][:, :, 256:768], wv[:, :, 7168:7680], [BW[iw]])
            else:
                ld("pool", Wt[iw][:, :, 0:512], wv[:, :, c0:c0 + 512], [BW[iw]])
            blks = range(0, 9) if kv else range(1, 9)
            for blk in blks:
                ih = nxt("h", 2)
                tk0 = 0 if blk == 0 else LC + (blk - 1) * 512
                N = LC if blk == 0 else 512
                ld("sp", hB[ih][:, :, :N], hT_d[:, :, tk0:tk0 + N], [BhB[ih]])
                for ch in range(ncol // 128):
                    iz = nxt("z", 4)
                    for c in range(NCH):
                        mm(pz[iz][:, :N], Wt[iw][:, c, ch * 128:(ch + 1) * 128], hB[ih][:, c, :N], c == 0, c == NCH - 1, [BW[iw], BhB[ih]], [Bpz[iz]])
                    run_deferred()
                    t0 = tk0 - LC
                    if kind == "k":
                        if ch < 2:
                            evac("nk", iz, N, t0, kaT_d[ch, :, tk0:tk0 + N], rope=(blk > 0))
                        else:
                            evac("rk", iz, N, t0, krT_d[ch - 2, :, tk0:tk0 + N], rope=(blk > 0))
                    else:
                        evac(kind, iz, N, t0, dstT[h0 + ch, :, t0:t0 + N], rope=True)
        run_deferred(); run_deferred(); run_deferred()
        pv = pz
        vo = [ph.sb(f"vo{i}", [128, 512], BF16) for i in range(2)]; Bvo = [Buf(), Buf()]
        vgroups = [(6912, 256, va_d, 0), (7680, 512, vr_d, 0), (8192, 512, vr_d, 512)]
        for (c0, ncol, dstT, dc0) in vgroups:
            iw = nxt("w", 2)
            ld("pool", Wt[iw][:, :, 0:ncol], wv[:, :, c0:c0 + ncol], [BW[iw]])
            for blk in range(0, 9):
                ih = nxt("h", 2)
                tk0 = 0 if blk == 0 else LC + (blk - 1) * 512
                N = LC if blk == 0 else 512
                ld("sp", hB[ih][:, :, :N], hT_d[:, :, tk0:tk0 + N], [BhB[ih]])
                for tl in range(N // 128):
                    iz = nxt("z", 4)
                    for c in range(NCH):
                        mm(pv[iz][:, :ncol], hB[ih][:, c, tl * 128:(tl + 1) * 128], Wt[iw][:, c, :ncol], c == 0, c == NCH - 1, [BW[iw], BhB[ih]], [Bpz[iz]])
                    io = nxt("ob", 2)
                    if tl % 2 == 0:
                        cp("act", vo[io][:, :ncol], pv[iz][:, :ncol], [Bpz[iz]], [Bvo[io]])
                    else:
                        cp("dve", vo[io][:, :ncol], pv[iz][:, :ncol], [Bpz[iz]], [Bvo[io]])
                    r0 = tk0 + tl * 128
                    ld("pool", dstT[r0:r0 + 128, dc0:dc0 + ncol], vo[io][:, :ncol], [], [Bvo[io]])
    if stop_after <= 2:
        kb.finish(); return nc

    oaT_d = dscr("oaT_d", [8, 128, T], BF16)
    orT_d = dscr("orT_d", [8, 128, T], BF16)

    ones_f = G.sb("ones_f", [128, 128], F32)
    kb.op("dve", lambda: nc.vector.memset(ones_f[:], 1.0), writes=[B_const])
    kb.barrier()
    with Phase(kb) as ph:
        KT = ph.sb("KT", [128, TK], BF16); BKT = Buf()
        Vt = ph.sb("Vt", [128, TK // 128, 128], BF16); BVt = Buf()
        QT = [ph.sb(f"QT{i}", [128, 512], BF16) for i in range(2)]; BQT = [Buf(), Buf()]
        NS = 2
        ps_s = [ph.ps(f"ps_s{i}", [128, 1024], F32) for i in range(NS)]; Bps = [Buf() for _ in range(NS)]
        po = [ph.ps(f"po{i}", [128, 512], F32) for i in range(2)]; Bpo = [Buf(), Buf()]
        pd = [ph.ps(f"pd{i}", [128, 512], F32) for i in range(2)]; Bpd = [Buf(), Buf()]
        NPT = 3
        pT = [ph.sb(f"pT{i}", [128, 1024], BF16) for i in range(NPT)]; BpT = [Buf() for _ in range(NPT)]
        accO = [ph.sb(f"accO{i}", [128, 512], F32) for i in range(2)]; BaO = [Buf(), Buf()]
        rd = [ph.sb(f"rd{i}", [128, 512], F32) for i in range(2)]; Brd = [Buf(), Buf()]
        oo = [ph.sb(f"oo{i}", [128, 512], BF16) for i in range(2)]; Boo = [Buf(), Buf()]
        NKT = TK // 128
        NPR = NKT // 2
        it = 0
        pend = []
        sidx = 0
        pidx_ = 0
        for g in range(0 if not SKIP3 else 2, 2):
            ld("sp", KT[:], kaT_d[g, :, :], [BKT])
            ld("sp", Vt[:], va_d[:, g * 128:(g + 1) * 128].rearrange("(n p) d -> p n d", p=128), [BVt])
            for j in range(4):
                h = g * 4 + j
                for qb in range(T // 512):
                    iq = it % 2; it += 1
                    t0 = qb * 512
                    ld("sp", QT[iq][:], qaT_d[h, :, t0:t0 + 512], [BQT[iq]])
                    s_of = {}

                    def issue_s(pr):
                        nonlocal sidx
                        s_of[pr] = sidx % NS; sidx += 1
                        sn = s_of[pr]
                        for u in range(2):
                            kt = 2 * pr + u
                            mm(ps_s[sn][:, u * 512:(u + 1) * 512], KT[:, kt * 128:(kt + 1) * 128], QT[iq][:], True, True, [BKT, BQT[iq]], [Bps[sn]])
                    issue_s(0)
                    for pr in range(NPR):
                        if pr + 1 < NPR:
                            issue_s(pr + 1)
                        if pr == 2 and pend:
                            pend.pop(0)()
                        sc_ = s_of[pr]
                        ip = pidx_ % NPT; pidx_ += 1
                        act(pT[ip][:], ps_s[sc_][:], AF.Exp, [Bps[sc_]], [BpT[ip]], scale=128.0 ** -0.5)
                        for u in range(2):
                            kt = 2 * pr + u
                            mm(po[iq][:], Vt[:, kt, :], pT[ip][:, u * 512:(u + 1) * 512], kt == 0, kt == NKT - 1, [BVt, BpT[ip]], [Bpo[iq]])
                        mm(pd[iq][:], ones_b[:], pT[ip][:, 0:512], pr == 0, False, [B_const, BpT[ip]], [Bpd[iq]])
                        if pr == 0:
                            cp("dve", accO[iq][:], pT[ip][:, 512:1024], [BpT[ip]], [BaO[iq]])
                        else:
                            tt("dve", accO[iq][:], accO[iq][:], pT[ip][:, 512:1024], ALU.add, [BpT[ip]], [BaO[iq]])

                    def epilogue(iq=iq, h=h, t0=t0):
                        mm(pd[iq][:], ones_f[:], accO[iq][:], False, True, [B_const, BaO[iq]], [Bpd[iq]])
                        kb.op("dve", lambda: nc.vector.reciprocal(out=rd[iq][:], in_=pd[iq][:]), [Bpd[iq]], [Brd[iq]])
                        tt("dve", oo[iq][:], po[iq][:], rd[iq][:], ALU.mult, [Bpo[iq], Brd[iq]], [Boo[iq]])
                        ld("pool", oaT_d[h, :, t0:t0 + 512], oo[iq][:], [], [Boo[iq]])
                    pend.append(epilogue)
        while pend:
            pend.pop(0)()
    if stop_after <= 3:
        kb.finish(); return nc

    rdec_in = din("ret_decay", [1, 8])
    gn_in = din("ret_gn", [128, 8])
    tabs_in = din("ret_tabs", [6, 128, 128])
    pcols_in = din("ret_pcols", [128, 6])
    with Phase(kb) as ph:
        Bt = Buf()
        lg = ph.sb("lg", [128, 8], F32)
        tabs = ph.sb("tabs", [128, 6, 128], F32)
        pcols = ph.sb("pcols", [128, 6], F32)
        gn_t = ph.sb("gn_t", [128, 8], F32)
        ld("sp", lg[:], rdec_in[0:1, :].partition_broadcast(128), [Bt])
        ld("sp", tabs[:], tabs_in.rearrange("k p i -> p k i"), [Bt])
        ld("sp", pcols[:], pcols_in[:, :], [Bt])
        ld("sp", gn_t[:], gn_in[:, :], [Bt])
        act(lg[:], lg[:], AF.Exp, [], [Bt])
        act(lg[:], lg[:], AF.Ln, [], [Bt], bias=1.0)
        ts("dve", lg[:], lg[:], -1.0, None, ALU.mult, None, [], [Bt])
        kT = ph.sb("kT", [128, TK], BF16); BkT = Buf()
        qT = ph.sb("qT", [128, T], BF16); BqT = Buf()
        V = ph.sb("V", [128, TK // 128, 256], BF16); BV = Buf()
        sg = ph.sb("sg", [128, 2, T], BF16); Bsg = Buf()
        wq = ph.sb("wq", [128, 2, 128], F32); Bwq = Buf()
        wk = ph.sb("wk", [128, 6], F32); Bwk = Buf()
        gch = ph.sb("gch", [128, 2], F32); Bgch = Buf()
        MT = ph.sb("MT", [128, 128], F32); BMT = Buf()
        mtmp = ph.sb("mtmp", [128, 2, 128], F32); Bmtmp = Buf()
        kw = ph.sb("kw", [128, TK // 128, 2, 128], BF16); Bkw = Buf()
        S32 = [ph.sb(f"S32{i}", [128, 256], F32) for i in range(2)]; BS32 = [Buf(), Buf()]
        SB = [ph.sb(f"SB{i}", [128, 32, 256], BF16) for i in range(2)]; BSB = [Buf(), Buf()]
        ptk = [ph.ps(f"ptk{i}", [128, 2, 128], BF16) for i in range(2)]; Bptk = [Buf(), Buf()]
        pu = [ph.ps(f"pu{i}", [128, 256], F32) for i in range(2)]; Bpu = [Buf(), Buf()]
        pS = ph.ps("pS", [128, 128], F32); BpS = Buf()
        pO = [ph.ps(f"pO{i}", [128, 256], F32) for i in range(3)]; BpO = [Buf() for _ in range(3)]
        Sm = [ph.sb(f"Sm{i}", [128, 128], BF16) for i in range(3)]; BSm = [Buf() for _ in range(3)]
        qw = [ph.sb(f"qw{i}", [128, 2, 128], BF16) for i in range(3)]; Bqw = [Buf() for _ in range(3)]
        Oall = ph.sb("Oall", [128, 32, 256], F32); BOall = [Buf() for _ in range(32)]
        bst = ph.sb("bst", [128, 32, 6], F32); Bbst = [Buf() for _ in range(32)]
        mv = ph.sb("mv", [128, 32, 2], F32); Bmv = Buf()
        rs_ = ph.sb("rs", [128, 32], F32); Brs = Buf()
        mh32 = ph.sb("mh32", [128, 32], F32)
        kb.op("dve", lambda: nc.vector.memset(mh32[:], -0.5), [], [Bt])
        on = [ph.sb(f"on{i}", [128, 256], BF16) for i in range(2)]; Bon = [Buf(), Buf()]
        orb = [ph.sb(f"orb{i}", [128, 2, 512], BF16) for i in range(2)]; Borb = [Buf(), Buf()]
        NKT = TK // 128
        for h in range(4):
            fcol = lg[:, h:h + 1]; bcol = lg[:, 4 + h:5 + h]
            ld("sp", kT[:], krT_d[h, :, :], [BkT])
            ld("sp", qT[:], qrT_d[h, :, :], [BqT])
            ld("sp", V[:], vr_d[:, h * 256:(h + 1) * 256].rearrange("(n p) d -> p n d", p=128), [BV])
            ld("sp", sg[:], sgT_d[2 * h:2 * h + 2, :, :].rearrange("c p t -> p c t"), [Bsg])
            act(wq[:, 0, :], tabs[:, 0, :], AF.Exp, [Bt], [Bwq], scale=fcol)
            act(wq[:, 1, :], tabs[:, 1, :], AF.Exp, [Bt], [Bwq], scale=bcol)
            for k in range(6):
                act(wk[:, k:k + 1], pcols[:, k:k + 1], AF.Exp, [Bt], [Bwk], scale=(fcol if k in (0, 2, 3) else bcol))
            act(gch[:, 0:1], fcol, AF.Exp, [Bt], [Bgch], scale=128.0)
            act(gch[:, 1:2], bcol, AF.Exp, [Bt], [Bgch], scale=128.0)
            act(mtmp[:, 0, :], tabs[:, 2, :], AF.Exp, [Bt], [Bmtmp], scale=fcol)
            act(mtmp[:, 1, :], tabs[:, 4, :], AF.Exp, [Bt], [Bmtmp], scale=bcol)
            tt("dve", mtmp[:, 0, :], mtmp[:, 0, :], tabs[:, 3, :], ALU.mult, [Bt], [Bmtmp])
            tt("dve", mtmp[:, 1, :], mtmp[:, 1, :], tabs[:, 5, :], ALU.mult, [Bt], [Bmtmp])
            tt("dve", MT[:], mtmp[:, 0, :], mtmp[:, 1, :], ALU.add, [Bmtmp], [BMT])
            for n in range(NKT):
                ip = n % 2
                tr(ptk[ip][:, 0, :], kT[:, n * 128:(n + 1) * 128], ident_b[:], [BkT, B_const], [Bptk[ip]])
                if n == 0:
                    cf, cb = 2, 4
                elif n == 1:
                    cf, cb = 3, 5
                else:
                    cf, cb = 0, 1
                ts("dve", kw[:, n, 0, :], ptk[ip][:, 0, :], wk[:, cf:cf + 1], None, ALU.mult, None, [Bptk[ip], Bwk], [Bkw])
                ts("dve", kw[:, n, 1, :], ptk[ip][:, 0, :], wk[:, cb:cb + 1], None, ALU.mult, None, [Bptk[ip], Bwk], [Bkw])
            for d_ in range(2):
                mm(pu[d_][:], kw[:, 0, d_, :], V[:, 0, :], True, False, [Bkw, BV], [Bpu[d_]])
                mm(pu[d_][:], kw[:, 1, d_, :], V[:, 1, :], False, True, [Bkw, BV], [Bpu[d_]])
                cp("dve", S32[d_][:], pu[d_][:], [Bpu[d_]], [BS32[d_]])
                first = 0 if d_ == 0 else 31
                cp("act", SB[d_][:, first, :], S32[d_][:], [BS32[d_]], [BSB[d_]])
            for k in range(31):
                for d_ in range(2):
                    c = k if d_ == 0 else 31 - k
                    mm(pu[d_][:], kw[:, 2 + c, d_, :], V[:, 2 + c, :], True, True, [Bkw, BV], [Bpu[d_]])
                    stt(S32[d_][:], S32[d_][:], gch[:, d_:d_ + 1], pu[d_][:], ALU.mult, ALU.add, [Bpu[d_], Bgch], [BS32[d_]])
                    nxtc = c + 1 if d_ == 0 else c - 1
                    cp("act", SB[d_][:, nxtc, :], S32[d_][:], [BS32[d_]], [BSB[d_]])
            for c in range(32):
                i3 = c % 3
                t0 = c * 128
                mm(pS[:], kT[:, LC + t0:LC + t0 + 128], qT[:, t0:t0 + 128], True, True, [BkT, BqT], [BpS])
                tt("dve", Sm[i3][:], pS[:], MT[:], ALU.mult, [BpS, BMT], [BSm[i3]])
                tt("pool", qw[i3][:, 0, :], qT[:, t0:t0 + 128], wq[:, 0, :], ALU.mult, [BqT, Bwq], [Bqw[i3]])
                tt("pool", qw[i3][:, 1, :], qT[:, t0:t0 + 128], wq[:, 1, :], ALU.mult, [BqT, Bwq], [Bqw[i3]])
                mm(pO[i3][:], Sm[i3][:], V[:, 2 + c, :], True, False, [BSm[i3], BV], [BpO[i3]])
                mm(pO[i3][:], qw[i3][:, 0, :], SB[0][:, c, :], False, False, [Bqw[i3], BSB[0]], [BpO[i3]])
                mm(pO[i3][:], qw[i3][:, 1, :], SB[1][:, c, :], False, True, [Bqw[i3], BSB[1]], [BpO[i3]])
                cp("act", Oall[:, c, :], pO[i3][:], [BpO[i3]], [BOall[c]])
                kb.op("dve", lambda c=c: nc.vector.bn_stats(out=bst[:, c, :], in_=Oall[:, c, :]), [BOall[c]], [Bbst[c]])
            for c in range(32):
                kb.op("dve", lambda c=c: nc.vector.bn_aggr(out=mv[:, c, :], in_=bst[:, c, :]), [Bbst[c]], [Bmv])
            kb.op("dve", lambda: nc.vector.tensor_scalar(out=rs_[:], in0=mv[:, :, 1], scalar1=EPS, scalar2=None, op0=ALU.add), [Bmv], [Brs])
            kb.op("pool", lambda: nc.gpsimd.tensor_tensor(out=rs_[:], in0=rs_[:], in1=mh32[:], op=ALU.pow), [Bt], [Brs])
            for c in range(32):
                i2 = c % 2
                t0 = c * 128
                ts("dve", on[i2][:], Oall[:, c, :], mv[:, c, 0:1], rs_[:, c:c + 1], ALU.subtract, ALU.mult, [BOall[c], Bmv, Brs], [Bon[i2]])
                io = (c // 4) % 2
                for k in range(2):
                    tr(ptk[i2][:, k, :], on[i2][:, k * 128:(k + 1) * 128], ident_b[:], [Bon[i2], B_const], [Bptk[i2]])
                for k in range(2):
                    stt(orb[io][:, k, (c % 4) * 128:(c % 4 + 1) * 128], ptk[i2][:, k, :], gn_t[:, 2 * h + k:2 * h + k + 1], sg[:, k, t0:t0 + 128],
                        ALU.mult, ALU.mult, [Bptk[i2], Bt, Bsg], [Borb[io]])
                if c % 4 == 3:
                    tb = (c // 4) * 512
                    ld("pool", orT_d[2 * h:2 * h + 2, :, tb:tb + 512].rearrange("c p t -> p c t"), orb[io][:], [], [Borb[io]])
    if stop_after <= 4:
        kb.finish(); return nc

    w_oa_in = din("w_o_att", [1024, D]); w_or_in = din("w_o_ret", [1024, D]); w_out_in = din("w_out", [D, D])
    w_r_in = din("w_router", [D, 16])
    yT_d = dscr("yT_d", [128, NCH, T], BF16)
    x1_d = dscr("x1_d", [T, D], F32)
    h2_d = dscr("h2_d", [T, D], BF16)
    AFF = G.sb("AFF", [128, 32, 16], F32); BAFF = Buf()
    with Phase(kb) as ph:
        Woa = ph.sb("Woa", [128, 8, D], BF16); Wor = ph.sb("Wor", [128, 8, D], BF16); BWo = Buf()
        BWo_b = Buf()
        ld("pool", Woa[:], w_oa_in.rearrange("(c p) n -> p c n", p=128), [BWo])
        ld("pool", Wor[:], w_or_in.rearrange("(c p) n -> p c n", p=128), [BWo_b])
        oa = [ph.sb(f"oa{i}", [128, 8, 512], BF16) for i in range(2)]; Boa = [Buf(), Buf()]
        orr = [ph.sb(f"orr{i}", [128, 8, 512], BF16) for i in range(2)]; Borr = [Buf(), Buf()]
        ga = [ph.sb(f"ga{i}", [128, 16, 512], BF16) for i in range(2)]; Bga = [Buf(), Buf()]
        gr = [ph.sb(f"gr{i}", [128, 16, 512], BF16) for i in range(2)]; Bgr = [Buf(), Buf()]
        pA = [ph.ps(f"pA{i}", [128, 512], F32) for i in range(2)]; BpA = [Buf(), Buf()]
        pB = [ph.ps(f"pB{i}", [128, 512], F32) for i in range(2)]; BpB = [Buf(), Buf()]
        ta = [ph.sb(f"ta{i}", [128, 512], F32) for i in range(2)]; Bta = [Buf(), Buf()]
        tb_ = [ph.sb(f"tb{i}", [128, 512], F32) for i in range(2)]; Btb = [Buf(), Buf()]
        yTb = [ph.sb(f"yTb{i}", [128, NCH, 512], BF16) for i in range(2)]; ByT = [Buf(), Buf()]
        k = 0
        for blk in range(T // 512):
            ib = blk % 2
            t0 = blk * 512
            ld("sp", oa[ib][:], oaT_d[:, :, t0:t0 + 512].rearrange("c p t -> p c t"), [Boa[ib]])
            ld("sp", orr[ib][:], orT_d[:, :, t0:t0 + 512].rearrange("c p t -> p c t"), [Borr[ib]])
            ld("sp", ga[ib][:], gaT_d[:, :, t0:t0 + 512].rearrange("c p t -> p c t"), [Bga[ib]])
            ld("sp", gr[ib][:], grT_d[:, :, t0:t0 + 512].rearrange("c p t -> p c t"), [Bgr[ib]])
            for dm in range(16):
                i2 = k % 2; k += 1
                for f in range(8):
                    mm(pA[i2][:], Woa[:, f, dm * 128:(dm + 1) * 128], oa[ib][:, f, :], f == 0, f == 7, [BWo, Boa[ib]], [BpA[i2]])
                for f in range(8):
                    mm(pB[i2][:], Wor[:, f, dm * 128:(dm + 1) * 128], orr[ib][:, f, :], f == 0, f == 7, [BWo_b, Borr[ib]], [BpB[i2]])
                tt("dve", ta[i2][:], pA[i2][:], ga[ib][:, dm, :], ALU.mult, [BpA[i2], Bga[ib]], [Bta[i2]])
                tt("dve", tb_[i2][:], pB[i2][:], gr[ib][:, dm, :], ALU.mult, [BpB[i2], Bgr[ib]], [Btb[i2]])
                tt("pool", yTb[ib][:, dm, :], ta[i2][:], tb_[i2][:], ALU.add, [Bta[i2], Btb[i2]], [ByT[ib]])
            ld("pool", yT_d[:, :, t0:t0 + 512], yTb[ib][:], [], [ByT[ib]])
    if stop_after <= 5:
        kb.finish(); return nc

    with Phase(kb) as ph:
        Wo = ph.sb("Wo", [128, NCH, D], BF16); BWo2 = Buf()
        ld("pool", Wo[:], w_out_in.rearrange("(c p) n -> p c n", p=128), [BWo2])
        wr = ph.sb("wr", [128, NCH, 16], F32); Bwr = Buf()
        ld("sp", wr[:], w_r_in.rearrange("(c p) e -> p c e", p=128), [Bwr])
        gt1, Bgt1 = bcast_row(ph, "gt1", mod_d[0:1, 2 * D:3 * D])
        po1, Bpo1 = bcast_row(ph, "po1", post1[0:1, :])
        tt("dve", gt1[:], gt1[:], po1[:], ALU.mult, [Bpo1], [Bgt1])
        g2, Bg2 = bcast_row(ph, "g2", mod_d[0:1, 4 * D:5 * D])
        ld("sp", po1[:], pre2[0:1, :].partition_broadcast(128), [Bpo1])
        stt(g2[:], g2[:], 1.0, po1[:], ALU.add, ALU.mult, [Bpo1], [Bg2])
        s2, Bs2 = bcast_row(ph, "s2", mod_d[0:1, 3 * D:4 * D])
        yTt = [ph.sb(f"yTt{i}", [128, NCH, 128], BF16) for i in range(3)]; ByTt = [Buf() for _ in range(3)]
        xt = [ph.sb(f"xt{i}", [128, D], F32) for i in range(3)]; Bxt = [Buf() for _ in range(3)]
        pY = [ph.ps(f"pY{i}", [128, 512], F32) for i in range(4)]; BpY = [Buf() for _ in range(4)]
        ptr = [ph.ps(f"ptr{i}", [128, 4, 128], F32) for i in range(2)]; Bptr = [Buf(), Buf()]
        pL = ph.ps("pL", [128, 16], F32); BpL = Buf()
        Ysb = [ph.sb(f"Ysb{i}", [128, D], F32) for i in range(2)]; BY = [Buf(), Buf()]
        junk = ph.sb("junk", [128, D], BF16); Bjunk = Buf()
        sq = [ph.sb(f"sq5{i}", [128, 4], F32) for i in range(2)]; Bsq5 = [Buf(), Buf()]
        x1t = [ph.sb(f"x1t{i}", [128, D], F32) for i in range(2)]; Bx1 = [Buf(), Buf()]
        h2f = [ph.sb(f"h2f{i}", [128, D], F32) for i in range(2)]; Bh2f = [Buf(), Buf()]
        h2b = [ph.sb(f"h2b{i}", [128, D], BF16) for i in range(2)]; Bh2b = [Buf(), Buf()]
        h2T = ph.sb("h2T", [128, NCH, 128], F32); Bh2T = Buf()
        lgt = [ph.sb(f"lgt{i}", [128, 16], F32) for i in range(2)]; Blgt = [Buf(), Buf()]
        NT5 = T // 128

        def ld5(i):
            if i < NT5:
                t0_ = i * 128
                ld("sp", yTt[i % 3][:], yT_d[:, :, t0_:t0_ + 128], [ByTt[i % 3]])
                ld("sp", xt[i % 3][:], xk[LC + t0_:LC + t0_ + 128, :], [Bxt[i % 3]])
        ld5(0); ld5(1)

        def s5A(i):
            s = i % 2; s3 = i % 3
            t0 = i * 128
            ld5(i + 2)
            for db in range(4):
                for c in range(NCH):
                    mm(pY[db][:], yTt[s3][:, c, :], Wo[:, c, db * 512:(db + 1) * 512], c == 0, c == NCH - 1, [ByTt[s3], BWo2], [BpY[db]])
            for db in range(4):
                cp("act", Ysb[s][:, db * 512:(db + 1) * 512], pY[db][:], [BpY[db]], [BY[s]])
            act(junk[:], Ysb[s][:], AF.Square, [BY[s]], [Bjunk, Bsq5[s]], accum_out=sq[s][:, 0:1])
            rstd_from(sq[s][:, 1:2], sq[s][:, 0:1], D, [], [Bsq5[s]])
            stt(Ysb[s][:], Ysb[s][:], sq[s][:, 1:2], gt1[:], ALU.mult, ALU.mult, [Bsq5[s], Bgt1], [BY[s]])
            tt("dve", x1t[s][:], Ysb[s][:], xt[s3][:], ALU.add, [BY[s], Bxt[s3]], [Bx1[s]])
            ld("pool", x1_d[t0:t0 + 128, :], x1t[s][:], [], [Bx1[s]])

        def s5B(i):
            s = i % 2
            t0 = i * 128
            act(junk[:], x1t[s][:], AF.Square, [Bx1[s]], [Bjunk, Bsq5[s]], accum_out=sq[s][:, 2:3])
            rstd_from(sq[s][:, 3:4], sq[s][:, 2:3], D, [], [Bsq5[s]])
            stt(h2f[s][:], x1t[s][:], sq[s][:, 3:4], g2[:], ALU.mult, ALU.mult, [Bx1[s], Bsq5[s], Bg2], [Bh2f[s]])
            tt("dve", h2f[s][:], h2f[s][:], s2[:], ALU.add, [Bs2], [Bh2f[s]])

        def s5C(i):
            s = i % 2
            t0 = i * 128
            cp("act", h2b[s][:], h2f[s][:], [Bh2f[s]], [Bh2b[s]])
            ld("pool", h2_d[t0:t0 + 128, :], h2b[s][:], [], [Bh2b[s]])
            for q4 in range(4):
                ip = q4 % 2
                for c in range(4):
                    cc = q4 * 4 + c
                    tr(ptr[ip][:, c, :], h2f[s][:, cc * 128:(cc + 1) * 128], ident_f[:], [Bh2f[s], B_const], [Bptr[ip]])
                cp("dve" if q4 % 2 == 0 else "act", h2T[:, q4 * 4:(q4 + 1) * 4, :], ptr[ip][:], [Bptr[ip]], [Bh2T])
            for c in range(NCH):
                mm(pL[:], h2T[:, c, :], wr[:, c, :], c == 0, c == NCH - 1, [Bh2T, Bwr], [BpL])
            cp("dve", AFF[:, i, :], pL[:], [BpL], [BAFF])

        s5A(0)
        for i in range(NT5):
            if i >= 1:
                s5C(i - 1)
            s5B(i)
            if i + 1 < NT5:
                s5A(i + 1)
        s5C(NT5 - 1)
        mx = ph.sb("mx", [128, 32], F32); Bmx = Buf()
        kb.op("dve", lambda: nc.vector.tensor_reduce(out=mx[:], in_=AFF[:], axis=AX.X, op=ALU.max), [BAFF], [Bmx])
        tt("dve", AFF[:], AFF[:], mx[:].unsqueeze(2).to_broadcast([128, 32, 16]), ALU.subtract, [Bmx], [BAFF])
        act(AFF[:], AFF[:], AF.Exp, [], [BAFF])
        kb.op("dve", lambda: nc.vector.tensor_reduce(out=mx[:], in_=AFF[:], axis=AX.X, op=ALU.add), [BAFF], [Bmx])
        kb.op("dve", lambda: nc.vector.reciprocal(out=mx[:], in_=mx[:]), [], [Bmx])
        tt("dve", AFF[:], AFF[:], mx[:].unsqueeze(2).to_broadcast([128, 32, 16]), ALU.mult, [Bmx], [BAFF])
        if "aff_d" in dbg:
            aff_d = dscr("aff_d", [128, 32, 16], F32)
            ld("sp", aff_d[:, :, :], AFF[:], [], [BAFF])
    if stop_after <= 6:
        kb.finish(); return nc

    NE = NEXP
    wg_in = din("w_gate", [NE, D, D]); wu_in = din("w_up", [NE, D, D]); wd_in = din("w_down", [NE, D, D])
    iota_in = din("iota512", [128, 512]); tri_in = din("tri", [128, 128]); tvc_in = din("tvc", [128, 32, 2])
    y2_d = dscr("y2_d", [T, D], F32)
    By2 = Buf()
    pos = G.sb("pos", [128, 32, 16], F32); maskf = G.sb("maskf", [128, 32, 16], F32)
    affh = G.sb("affh", [128, 32, 16], BF16); affl = G.sb("affl", [128, 32, 16], BF16)
    Bsel = Buf()
    with Phase(kb) as ph:
        zt = ph.sb("zt", [128, D], F32); Bz = Buf()
        kb.op("pool", lambda: nc.gpsimd.memset(zt[:], 0.0), [], [Bz])
        for i in range(T // 128):
            kb.dma("sp", lambda i=i: nc.sync.dma_start(out=y2_d[i * 128:(i + 1) * 128, :], in_=zt[:]), [Bz], [])
        lo = ph.sb("lo", [128, 16], F32); hi = ph.sb("hi", [128, 16], F32); mid = ph.sb("mid", [128, 16], F32); Bl = Buf()
        cmpb = ph.sb("cmpb", [128, 32, 16], BF16); Bcmp = Buf()
        partb = ph.sb("partb", [128, 16], BF16); Bpart = Buf()
        selu = ph.sb("selu", [128, 2, 16], U32); Bsu = Buf()
        tri_b = ph.sb("tri_b", [128, 128], BF16); Btri = Buf()
        ld("pool", tri_b[:], tri_in[:, :], [Btri])
        pc = ph.ps("pc", [128, 16], F32); Bpc = Buf()
        pcs = ph.ps("pcs", [128, 512], F32); Bpcs = Buf()
        pw = ph.ps("pw", [128, 512], F32); Bpw = Buf()
        maskb = ph.sb("maskb", [128, 32, 16], BF16); Bmb = Buf()
        tcum = ph.sb("tcum", [128, 32, 16], F32); Btc = Buf()
        kb.op("dve", lambda: nc.vector.memset(lo[:], 0.0), [], [Bl])
        kb.op("dve", lambda: nc.vector.memset(hi[:], 1.0), [], [Bl])
        for it in range(NBISECT):
            tt("dve", mid[:], lo[:], hi[:], ALU.add, [], [Bl])
            ts("dve", mid[:], mid[:], 0.5, None, ALU.mult, None, [], [Bl])
            tt("dve", cmpb[:], AFF[:], mid[:].unsqueeze(1).to_broadcast([128, 32, 16]), ALU.is_ge, [BAFF, Bl], [Bcmp])
            with nc.allow_low_precision(reason="exact small integer counts"):
                kb.op("dve", lambda: nc.vector.tensor_reduce(out=partb[:], in_=cmpb[:].rearrange("p t e -> p e t"), axis=AX.X, op=ALU.add), [Bcmp], [Bpart])
            mm(pc[:], ones_b[:], partb[:], True, True, [Bpart, B_const], [Bpc])
            ts("dve", selu[:, 0, :], pc[:], 511.5, None, ALU.is_ge, None, [Bpc], [Bsu])
            ts("dve", selu[:, 1, :], pc[:], 511.5, None, ALU.is_lt, None, [Bpc], [Bsu])
            kb.op("dve", lambda: nc.vector.copy_predicated(out=lo[:], mask=selu[:, 0, :], data=mid[:]), [Bsu], [Bl])
            kb.op("dve", lambda: nc.vector.copy_predicated(out=hi[:], mask=selu[:, 1, :], data=mid[:]), [Bsu], [Bl])
        tt("dve", maskb[:], AFF[:], lo[:].unsqueeze(1).to_broadcast([128, 32, 16]), ALU.is_ge, [BAFF, Bl], [Bmb])
        cp("dve", maskf[:], maskb[:], [Bmb], [Bsel])
        mbf = maskb[:].rearrange("p t e -> p (t e)")
        mm(pcs[:], ones_b[:], mbf, True, True, [Bmb, B_const], [Bpcs])
        mm(pw[:], tri_b[:], mbf, True, True, [Bmb, Btri], [Bpw])
        kb.op("dve", lambda: nc.vector.memset(tcum[:, 0, :], 0.0), [], [Btc])
        for t in range(1, 32):
            tt("dve", tcum[:, t, :], tcum[:, t - 1, :], pcs[:, (t - 1) * 16:t * 16], ALU.add, [Bpcs], [Btc])
        tt("dve", pos[:].rearrange("p t e -> p (t e)"), pw[:], tcum[:].rearrange("p t e -> p (t e)"), ALU.add, [Bpw, Btc], [Bsel])
        cp("dve", affh[:], AFF[:], [BAFF], [Bsel])
        tt("dve", affl[:], AFF[:], affh[:], ALU.subtract, [BAFF], [Bsel])
        if "sel_d" in dbg:
            sel_d = dscr("sel_d", [2, 128, 32, 16], F32)
            ld("sp", sel_d[0], pos[:], [], [Bsel]); ld("sp", sel_d[1], maskf[:], [], [Bsel])
    if stop_after <= 7:
        kb.finish(); return nc

    with Phase(kb) as ph:
        iota = ph.sb("iota", [128, 512], F32); Bio = Buf()
        ld("sp", iota[:], iota_in[:, :], [Bio])
        tv = [ph.sb(f"tv{i}", [128, 32, 4], BF16) for i in range(2)]; Btv = [Buf(), Buf()]
        for i in range(2):
            ld("pool", tv[i][:, :, 0:2], tvc_in[:, :, :], [Btv[i]])
        Pm = [ph.sb(f"Pm{i}", [128, 32, 128], BF16) for i in range(2)]; BPm = [Buf(), Buf()]
        pidx = ph.ps("pidx", [128, 4], F32); Bpidx = Buf()
        idxf = ph.sb("idxf", [128, 4], F32); Bidxf = Buf()
        idx_i = ph.sb("idx_i", [128, 2, 4], I32); Bidx = [[Buf() for _ in range(4)] for _ in range(2)]
        wsl = ph.sb("wsl", [128, 2, 4], F32)
        xg = ph.sb("xg", [128, 4, D], BF16); Bxg = [Buf() for _ in range(4)]
        ptx = ph.ps("ptx", [128, 8, 128], BF16); Bptx = Buf()
        xgT = [ph.sb(f"xgT{i}", [128, NCH, 512], BF16) for i in range(2)]; BxgT = [Buf(), Buf()]
        NR = 5
        Wr = [ph.sb(f"Wr{i}", [128, NCH, 512], BF16) for i in range(NR)]; BWr = [Buf() for _ in range(NR)]
        pa = [ph.ps(f"pa{i}", [128, 512], F32) for i in range(2)]; Bpa = [Buf(), Buf()]
        pb = [ph.ps(f"pb{i}", [128, 512], F32) for i in range(2)]; Bpb = [Buf(), Buf()]
        pyo = [ph.ps(f"pyo{i}", [128, 512], F32) for i in range(2)]; Bpyo = [Buf(), Buf()]
        sa = [ph.sb(f"sa{i}", [128, 512], F32) for i in range(2)]; Bsa = [Buf(), Buf()]
        hT = ph.sb("hTe", [128, NCH, 512], BF16); BhT = Buf()
        Ysb = ph.sb("Ye", [128, 4, D], BF16); BYe = [Buf() for _ in range(4)]

        def A_tv(e):
            s = e % 2
            cp("pool", tv[s][:, :, 2], affh[:, :, e], [Bsel], [Btv[s]])
            cp("pool", tv[s][:, :, 3], affl[:, :, e], [Bsel], [Btv[s]])

        def A_pm(e, q):
            b = q % 2
            for t in range(32):
                ts("dve", Pm[b][:, t, :], iota[:, q * 128:(q + 1) * 128], pos[:, t, e:e + 1], maskf[:, t, e:e + 1], ALU.is_equal, ALU.mult, [Bio, Bsel], [BPm[b]])

        def A_idx(e, q):
            s = e % 2; b = q % 2
            for t in range(32):
                mm(pidx[:], Pm[b][:, t, :], tv[s][:, t, :], t == 0, t == 31, [BPm[b], Btv[s]], [Bpidx])
            cp("dve", idxf[:], pidx[:], [Bpidx], [Bidxf])
            tt("dve", idx_i[:, s, q:q + 1], idxf[:, 0:1], idxf[:, 1:2], ALU.add, [Bidxf], [Bidx[s][q]])
            tt("dve", wsl[:, s, q:q + 1], idxf[:, 2:3], idxf[:, 3:4], ALU.add, [Bidxf], [Bidx[s][q]])

        def A_gather(e, q):
            s = e % 2
            kb.dma("pool", lambda: nc.gpsimd.indirect_dma_start(
                out=xg[:, q, :], out_offset=None, in_=h2_d[:, :],
                in_offset=bass.IndirectOffsetOnAxis(ap=idx_i[:, s, q:q + 1], axis=0)), [Bidx[s][q]], [Bxg[q]])

        def A_tr(e, q):
            s = e % 2
            for cg in range(2):
                for c in range(8):
                    cc = cg * 8 + c
                    tr(ptx[:, c, :], xg[:, q, cc * 128:(cc + 1) * 128], ident_b[:], [Bxg[q], B_const], [Bptx])
                cp("act" if cg == 0 else "dve", xgT[s][:, cg * 8:(cg + 1) * 8, q * 128:(q + 1) * 128], ptx[:], [Bptx], [BxgT[s]])

        pieces = []
        for e in range(NE):
            for fg in range(4):
                pieces.append((wg_in, e, fg)); pieces.append((wu_in, e, fg))
            for db in range(4):
                pieces.append((wd_in, e, db))
        pstate = dict(loaded=0)

        def load_piece(k):
            wt, e, j = pieces[k]
            r = k % NR
            wvw = wt[e].rearrange("(c p) n -> p c n", p=128)
            ld("pool", Wr[r][:], wvw[:, :, j * 512:(j + 1) * 512], [BWr[r]])

        def need(k):
            while pstate["loaded"] <= min(k + NR - 2, len(pieces) - 1):
                load_piece(pstate["loaded"]); pstate["loaded"] += 1

        def stageB(e, hooks):
            s = e % 2
            kbase = e * 12
            ii = 0
            for fg in range(4):
                kg = kbase + fg * 2; ku = kg + 1
                need(ku)
                rg = kg % NR; ru = ku % NR
                for fc in range(4):
                    i2 = ii % 2; ii += 1
                    for c in range(NCH):
                        mm(pa[i2][:], Wr[rg][:, c, fc * 128:(fc + 1) * 128], xgT[s][:, c, :], c == 0, c == NCH - 1, [BWr[rg], BxgT[s]], [Bpa[i2]])
                    for c in range(NCH):
                        mm(pb[i2][:], Wr[ru][:, c, fc * 128:(fc + 1) * 128], xgT[s][:, c, :], c == 0, c == NCH - 1, [BWr[ru], BxgT[s]], [Bpb[i2]])
                    act(sa[i2][:], pa[i2][:], AF.Silu, [Bpa[i2]], [Bsa[i2]])
                    tt("dve", hT[:, fg * 4 + fc, :], sa[i2][:], pb[i2][:], ALU.mult, [Bsa[i2], Bpb[i2]], [BhT])
                hooks("fg", fg)
            jj = 0
            for db in range(4):
                kd = kbase + 8 + db
                need(kd)
                rd_ = kd % NR
                for sc in range(4):
                    i2 = jj % 2; jj += 1
                    for fcn in range(NCH):
                        mm(pyo[i2][:], hT[:, fcn, sc * 128:(sc + 1) * 128], Wr[rd_][:, fcn, :], fcn == 0, fcn == NCH - 1, [BhT, BWr[rd_]], [Bpyo[i2]])
                    if jj % 2 == 0:
                        act(Ysb[:, sc, db * 512:(db + 1) * 512], pyo[i2][:], AF.Copy, [Bpyo[i2], Bidx[s][sc]], [BYe[sc]], scale=wsl[:, s, sc:sc + 1])
                    else:
                        ts("dve", Ysb[:, sc, db * 512:(db + 1) * 512], pyo[i2][:], wsl[:, s, sc:sc + 1], None, ALU.mult, None, [Bpyo[i2], Bidx[s][sc]], [BYe[sc]])
                hooks("db", db)
            for sc in range(4):
                kb.dma("pool", lambda sc=sc, s=s: nc.gpsimd.indirect_dma_start(
                    out=y2_d[:, :], out_offset=bass.IndirectOffsetOnAxis(ap=idx_i[:, s, sc:sc + 1], axis=0),
                    in_=Ysb[:, sc, :], in_offset=None, compute_op=ALU.add), [BYe[sc], Bidx[s][sc]], [By2])

        kb.barrier()
        A_tv(0)
        for q in range(4):
            A_pm(0, q); A_idx(0, q); A_gather(0, q)
        for q in range(4):
            A_tr(0, q)
        for e in range(NE):
            nx = e + 1

            def hooks(kind, j, nx=nx):
                if nx >= NE:
                    return
                if kind == "fg":
                    if j >= 1:
                        A_gather(nx, j - 1)
                    A_idx(nx, j)
                    if j + 1 < 4:
                        A_pm(nx, j + 1)
                else:
                    if j == 0:
                        A_gather(nx, 3)
                    A_tr(nx, j)
            if nx < NE:
                A_tv(nx)
                A_pm(nx, 0)
            stageB(e, hooks)
    if stop_after <= 8:
        kb.finish(); return nc

    with Phase(kb) as ph:
        gt2, Bgt2 = bcast_row(ph, "gt2", mod_d[0:1, 5 * D:6 * D])
        po2, Bpo2 = bcast_row(ph, "po2", post2[0:1, :])
        tt("dve", gt2[:], gt2[:], po2[:], ALU.mult, [Bpo2], [Bgt2])
        yt = [ph.sb(f"y2t{i}", [128, D], F32) for i in range(3)]; Byt = [Buf() for _ in range(3)]
        x1t = [ph.sb(f"x1u{i}", [128, D], F32) for i in range(3)]; Bx1 = [Buf() for _ in range(3)]
        junk = ph.sb("junk8", [128, D], BF16); Bjunk = Buf()
        sq = [ph.sb(f"sq8{i}", [128, 2], F32) for i in range(2)]; Bsq = [Buf(), Buf()]
        ot = [ph.sb(f"ot{i}", [128, D], F32) for i in range(2)]; Bot = [Buf(), Buf()]
        NT8 = T // 128

        def ld8(i):
            if i < NT8:
                ld("sp", yt[i % 3][:], y2_d[i * 128:(i + 1) * 128, :], [Byt[i % 3]])
                ld("sp", x1t[i % 3][:], x1_d[i * 128:(i + 1) * 128, :], [Bx1[i % 3]])
        ld8(0); ld8(1)
        for i in range(NT8):
            s = i % 2; s3 = i % 3
            t0 = i * 128
            ld8(i + 2)
            act(junk[:], yt[s3][:], AF.Square, [Byt[s3]], [Bjunk, Bsq[s]], accum_out=sq[s][:, 0:1])
            rstd_from(sq[s][:, 1:2], sq[s][:, 0:1], D, [], [Bsq[s]])
            stt(yt[s3][:], yt[s3][:], sq[s][:, 1:2], gt2[:], ALU.mult, ALU.mult, [Bsq[s], Bgt2], [Byt[s3]])
            tt("dve", ot[s][:], yt[s3][:], x1t[s3][:], ALU.add, [Byt[s3], Bx1[s3]], [Bot[s]])
            ld("pool", out[t0:t0 + 128, :], ot[s][:], [], [Bot[s]])

    kb.finish()
    return nc


def make_inputs(inp, b):
    L = 0
    Rm, cosT, sinT = rope_consts()
    m = dict(
        xk=np.ascontiguousarray(np.concatenate([inp["ctx"][b], inp["x"][b]], axis=0)),
        c2=np.ascontiguousarray(np.stack([inp["c"][b], inp["c_ctx"]], axis=0)),
        w_mod=inp["w_mod"][L], b_mod=inp["b_mod"][L][None, :],
        pre1=inp["pre_norm1"][L][None, :], post1=inp["post_norm1"][L][None, :],
        pre2=inp["pre_norm2"][L][None, :], post2=inp["post_norm2"][L][None, :],
        w_in=inp["w_in"][L],
        ident=np.eye(128, dtype=np.float32),
        q_norm=inp["q_norm"][L][:, None], k_norm=inp["k_norm"][L][:, None],
        rotm=Rm, cosT=cosT, sinT=sinT,
        w_o_att=inp["w_o_att"][L], w_o_ret=inp["w_o_ret"][L], w_out=inp["w_out"][L], w_router=inp["w_router"][L],
        w_gate=inp["w_gate"][L][:NEXP], w_up=inp["w_up"][L][:NEXP], w_down=inp["w_down"][L][:NEXP],
        iota512=np.ascontiguousarray(np.broadcast_to(np.arange(512, dtype=np.float32), (128, 512))),
        tri=np.triu(np.ones((128, 128), np.float32), 1),
        tvc=np.ascontiguousarray(np.stack([np.broadcast_to(np.arange(128, dtype=np.float32)[:, None], (128, 32)),
                                           np.broadcast_to(128.0 * np.arange(32, dtype=np.float32)[None, :], (128, 32))], axis=-1)),
        ret_decay=inp["ret_decay"][L].reshape(1, 8), ret_gn=np.ascontiguousarray(inp["ret_gn"][L].reshape(8, 128).T),
    )
    ii = np.arange(128, dtype=np.float32)
    jj = ii[:, None]; i2 = ii[None, :]
    tabs = np.stack([np.broadcast_to(i2 + 1, (128, 128)), np.broadcast_to(128 - i2, (128, 128)),
                     np.maximum(i2 - jj, 0), (i2 >= jj).astype(np.float32),
                     np.maximum(jj - i2, 0), (jj >= i2).astype(np.float32)]).astype(np.float32)
    m["ret_tabs"] = np.ascontiguousarray(tabs)
    p = ii
    m["ret_pcols"] = np.ascontiguousarray(np.stack([127 - p, p, 255 - p, 127 - p, p, 128 + p], axis=1).astype(np.float32))
    return m


_NC_CACHE = {}


def kernel(**inputs):
    inp = {k: np.asarray(v) for k, v in inputs.items()}
    if "nc" not in _NC_CACHE:
        _NC_CACHE["nc"] = build()
    nc = _NC_CACHE["nc"]
    B = inp["x"].shape[0]
    maps = [make_inputs(inp, b) for b in range(B)]
    in_maps = [maps[i % B] for i in range(8)]
    res = run_bass_kernel_spmd(nc, in_maps, core_ids=list(range(8)))
    out = np.stack([np.asarray(res.results[b]["out"]) for b in range(B)], axis=0)
    return out.astype(np.float32)
```

```python
import numpy as np
import contextlib
import concourse.bass as bass
import concourse.mybir as mybir
from concourse.bass_utils import run_bass_kernel_spmd

F32 = mybir.dt.float32
BF16 = mybir.dt.bfloat16
I32 = mybir.dt.int32
U32 = mybir.dt.uint32
AF = mybir.ActivationFunctionType
ALU = mybir.AluOpType
AX = mybir.AxisListType

D = 2048
T = 4096
LC = 256
TK = T + LC
NCH = 16
QW = 6656
INW = 8704
EPS = 1e-6


class Buf:
    __slots__ = ("w", "r", "name")

    def __init__(self, name=""):
        self.w = None
        self.r = {}
        self.name = name


class KB:
    def __init__(self, nc):
        self.nc = nc
        self.es = contextlib.ExitStack()
        self.E = dict(pe=nc.tensor, act=nc.scalar, dve=nc.vector, pool=nc.gpsimd, sp=nc.sync)
        self.clk = {}
        self.seen = {e: {} for e in self.E}
        self.nsem = 0
        for e in ("pe", "act", "dve", "pool"):
            self._new_clk(e)
        self.dpool = {}
        for q, n in (("sp", 20), ("pool", 12), ("act", 4)):
            self.dpool[q] = [[self._sem(f"d{q}{i}"), 0] for i in range(n)]
        self.dnext = {q: 0 for q in self.dpool}

    def _sem(self, name):
        self.nsem += 1
        sm = self.es.enter_context(self.nc.semaphore(name + f"_{self.nsem}"))
        if not hasattr(self, "_keep"):
            self._keep = []
        self._keep.append(sm)
        return sm

    def _new_clk(self, e):
        self.clk[e] = [self._sem("clk" + e), 0]

    def wait(self, eng, tok):
        sem, val = tok
        k = id(sem)
        if eng == "pe" and sem is self.clk["pe"][0]:
            return
        if self.seen[eng].get(k, 0) >= val:
            return
        self.E[eng].wait_ge(sem, val)
        self.seen[eng][k] = val

    def _deps(self, eng, reads, writes):
        for b in reads:
            if b.w is not None:
                self.wait(eng, b.w)
        for b in writes:
            if b.w is not None:
                self.wait(eng, b.w)
            for k, (sem, val) in b.r.items():
                self.wait(eng, (sem, val))

    def _commit(self, tok, reads, writes):
        sem, val = tok
        k = id(sem)
        for b in reads:
            if k not in b.r or b.r[k][1] < val:
                b.r[k] = (sem, val)
        for b in writes:
            b.w = tok
            b.r = {}

    def op(self, eng, fn, reads=(), writes=()):
        self._deps(eng, reads, writes)
        ins = fn()
        c = self.clk[eng]
        c[1] += 1
        ins.then_inc(c[0], 1)
        tok = (c[0], c[1])
        self._commit(tok, reads, writes)
        return tok

    def dma(self, q, fn, reads=(), writes=()):
        self._deps(q, reads, writes)
        pool = self.dpool[q]
        i = self.dnext[q]
        self.dnext[q] = (i + 1) % len(pool)
        ent = pool[i]
        if ent[1] > 0:
            self.wait(q, (ent[0], ent[1]))
        ins = fn()
        ent[1] += 16
        ins.then_inc(ent[0], 16)
        tok = (ent[0], ent[1])
        self._commit(tok, reads, writes)
        return tok

    def _alltoks(self):
        toks = []
        for e, c in self.clk.items():
            if c[1] > 0:
                toks.append((c[0], c[1]))
        for q, pool in self.dpool.items():
            for ent in pool:
                if ent[1] > 0:
                    toks.append((ent[0], ent[1]))
        return toks

    def barrier(self):
        toks = self._alltoks()
        for e in self.E:
            for t in toks:
                self.wait(e, t)

    def new_phase(self):
        self.barrier()
        for e in ("pe", "act", "dve", "pool"):
            if self.clk[e][1] > 0:
                self._new_clk(e)

    def finish(self, eng="sp"):
        for t in self._alltoks():
            self.wait(eng, t)


class Phase:
    def __init__(self, kb):
        self.kb = kb
        self.es = contextlib.ExitStack()

    def __enter__(self):
        return self

    def __exit__(self, *a):
        self.kb.new_phase()
        self.es.close()
        return False

    _n = [0]

    def sb(self, name, shape, dt):
        Phase._n[0] += 1
        t = self.es.enter_context(self.kb.nc.sbuf_tensor(f"{name}_s{Phase._n[0]}", list(shape), dt))
        return t

    def ps(self, name, shape, dt):
        Phase._n[0] += 1
        return self.es.enter_context(self.kb.nc.psum_tensor(f"{name}_p{Phase._n[0]}", list(shape), dt))


def rope_consts():
    Rm = np.zeros((128, 128), np.float32)
    for a in range(2):
        for j in range(32):
            Rm[a * 64 + 32 + j, a * 64 + j] = -1.0
            Rm[a * 64 + j, a * 64 + 32 + j] = 1.0
    t = np.arange(T)
    r = (t // 64).astype(np.float32)
    cl = (t % 64).astype(np.float32)
    inv = (10000.0 ** (-np.arange(32, dtype=np.float32) / 32)).astype(np.float32)
    ang_r = r[:, None] * inv
    ang_c = cl[:, None] * inv
    ang = np.concatenate([ang_r, ang_r, ang_c, ang_c], axis=-1)
    cosT = np.ascontiguousarray(np.cos(ang).T.astype(np.float32))
    sinT = np.ascontiguousarray(np.sin(ang).T.astype(np.float32))
    return Rm, cosT, sinT


P4STOP = 0
P4VAR = 2
SKIP3 = 0
NEXP = 16
NBISECT = 23


def build(dbg=(), stop_after=99):
    nc = bass.Bass("TRN2", target_bir_lowering=False)
    kb = KB(nc)
    dbg = set(dbg)

    def din(name, shape, dt=F32):
        return nc.dram_tensor(name, list(shape), dt, kind="ExternalInput").ap()

    def dscr(name, shape, dt):
        kind = "ExternalOutput" if name in dbg else "Internal"
        return nc.dram_tensor(name, list(shape), dt, kind=kind).ap()

    xk = din("xk", [TK, D])
    c2 = din("c2", [2, D])
    w_mod = din("w_mod", [D, 6 * D])
    b_mod = din("b_mod", [1, 6 * D])
    pre1 = din("pre1", [1, D]); post1 = din("post1", [1, D]); pre2 = din("pre2", [1, D]); post2 = din("post2", [1, D])
    w_in = din("w_in", [D, INW])
    ident_in = din("ident", [128, 128])
    out = nc.dram_tensor("out", [T, D], F32, kind="ExternalOutput").ap()

    mod_d = dscr("mod_d", [2, 6 * D], F32)
    hT_d = dscr("hT_d", [128, NCH, TK], BF16)

    G = Phase(kb)
    ident_f = G.sb("ident_f", [128, 128], F32)
    ident_b = G.sb("ident_b", [128, 128], BF16)
    eps_t = G.sb("eps_t", [128, 1], F32)
    B_const = Buf("const")
    kb.dma("sp", lambda: nc.sync.dma_start(out=ident_f[:], in_=ident_in[:, :]), writes=[B_const])
    kb.dma("pool", lambda: nc.gpsimd.dma_start(out=ident_b[:], in_=ident_in[:, :]), writes=[B_const])
    kb.op("dve", lambda: nc.vector.memset(eps_t[:], EPS), writes=[B_const])
    mhalf = G.sb("mhalf", [128, 1], F32)
    kb.op("dve", lambda: nc.vector.memset(mhalf[:], -0.5), writes=[B_const])

    def rstd_from(out_ap, ssq_ap, n, R, W):
        kb.op("dve", lambda: nc.vector.tensor_scalar(out=out_ap, in0=ssq_ap, scalar1=1.0 / n, scalar2=EPS, op0=ALU.mult, op1=ALU.add), R, W)
        kb.op("pool", lambda: nc.gpsimd.tensor_tensor(out=out_ap, in0=out_ap, in1=mhalf[:], op=ALU.pow), [B_const], W)
    kb.barrier()

    with Phase(kb) as ph:
        c2T = ph.sb("c2T", [128, 2, 16], F32)
        s2T = ph.sb("s2T", [128, 2, 16], BF16)
        bm2 = ph.sb("bm2", [2, 6 * D], F32)
        B_c2 = Buf(); B_s2 = Buf(); B_bm = Buf()
        for r in range(2):
            kb.dma("sp", lambda r=r: nc.sync.dma_start(out=c2T[:, r, :], in_=c2[r, :].rearrange("(p c) -> p c", c=16)), writes=[B_c2])
            kb.dma("sp", lambda r=r: nc.sync.dma_start(out=bm2[r:r + 1, :], in_=b_mod[:, :]), writes=[B_bm])
        kb.op("act", lambda: nc.scalar.activation(out=s2T[:], in_=c2T[:], func=AF.Silu), reads=[B_c2], writes=[B_s2])
        wmv = w_mod.rearrange("(p c) n -> p c n", c=16)
        NB = 24
        NWB = 3
        wb = [ph.sb(f"wmb{i}", [128, 16, 512], BF16) for i in range(NWB)]
        Bwb = [Buf() for _ in range(NWB)]
        pm = [ph.ps(f"pm{i}", [2, 512], F32) for i in range(2)]
        Bpm = [Buf(), Buf()]
        mrow = [ph.sb(f"mrow{i}", [2, 512], F32) for i in range(2)]
        Bmr = [Buf(), Buf()]

        def ldw(j):
            s_ = j % NWB
            kb.dma("pool", lambda j=j, s_=s_: nc.gpsimd.dma_start(out=wb[s_][:], in_=wmv[:, :, j * 512:(j + 1) * 512]), writes=[Bwb[s_]])
        ldw(0); ldw(1)
        for j in range(NB):
            s = j % 2; sw = j % NWB
            if j + 2 < NB:
                ldw(j + 2)
            for c in range(16):
                kb.op("pe", lambda c=c, s=s, sw=sw: nc.tensor.matmul(pm[s][:], lhsT=s2T[:, :, c], rhs=wb[sw][:, c, :], start=(c == 0), stop=(c == 15)),
                      reads=[B_s2, Bwb[sw]], writes=[Bpm[s]])
            kb.op("dve", lambda j=j, s=s: nc.vector.tensor_tensor(out=mrow[s][:], in0=pm[s][:], in1=bm2[:, j * 512:(j + 1) * 512], op=ALU.add),
                  reads=[Bpm[s], B_bm], writes=[Bmr[s]])
            kb.dma("sp", lambda j=j, s=s: nc.sync.dma_start(out=mod_d[:, j * 512:(j + 1) * 512], in_=mrow[s][:]), reads=[Bmr[s]])
    if stop_after <= 0:
        kb.finish(); return nc

    def bcast_row(ph, name, src_ap):
        t = ph.sb(name, [128, D], F32)
        b = Buf(name)
        kb.dma("sp", lambda: nc.sync.dma_start(out=t[:], in_=src_ap.partition_broadcast(128)), writes=[b])
        return t, b

    with Phase(kb) as ph:
        g1, Bg1 = bcast_row(ph, "g1", pre1[0:1, :])
        Gm = []
        for r in range(2):
            sc, Bsc = bcast_row(ph, f"sc{r}", mod_d[r:r + 1, D:2 * D])
            sh, Bsh = bcast_row(ph, f"sh{r}", mod_d[r:r + 1, 0:D])
            kb.op("dve", lambda sc=sc: nc.vector.scalar_tensor_tensor(out=sc[:], in0=sc[:], scalar=1.0, in1=g1[:], op0=ALU.add, op1=ALU.mult),
                  reads=[Bg1], writes=[Bsc])
            Gm.append((sc, Bsc, sh, Bsh))
        NT = TK // 128
        xt = [ph.sb(f"xt{i}", [128, D], F32) for i in range(3)]; Bxt = [Buf() for _ in range(3)]
        junk = ph.sb("junk", [128, D], BF16); Bjunk = Buf()
        ssq = [ph.sb(f"ssq{i}", [128, 1], F32) for i in range(2)]; Bssq = [Buf(), Buf()]
        rstd = [ph.sb(f"rstd{i}", [128, 1], F32) for i in range(2)]; Brstd = [Buf(), Buf()]
        h1 = [ph.sb(f"h1{i}", [128, D], F32) for i in range(2)]; Bh1 = [Buf(), Buf()]
        hb = [ph.sb(f"hb{i}", [128, D], BF16) for i in range(2)]; Bhb = [Buf(), Buf()]
        pt = [ph.ps(f"pt{i}", [128, 8, 128], BF16) for i in range(4)]; Bpt = [Buf() for _ in range(4)]
        hT = [ph.sb(f"hTt{i}", [128, NCH, 512], BF16) for i in range(2)]; BhT = [Buf(), Buf()]

        def ldx(i):
            if i < NT:
                kb.dma("sp", lambda: nc.sync.dma_start(out=xt[i % 3][:], in_=xk[i * 128:(i + 1) * 128, :]), writes=[Bxt[i % 3]])
        ldx(0); ldx(1)

        def stA(i):
            s3 = i % 3; s = i % 2
            ldx(i + 2)
            r = 1 if i < LC // 128 else 0
            sc, Bsc, sh, Bsh = Gm[r]
            kb.op("act", lambda: nc.scalar.activation(out=junk[:], in_=xt[s3][:], func=AF.Square, accum_out=ssq[s][:]),
                  reads=[Bxt[s3]], writes=[Bjunk, Bssq[s]])
            rstd_from(rstd[s][:], ssq[s][:], D, [Bssq[s]], [Brstd[s]])
            kb.op("dve", lambda: nc.vector.scalar_tensor_tensor(out=h1[s][:], in0=xt[s3][:], scalar=rstd[s][:], in1=sc[:], op0=ALU.mult, op1=ALU.mult),
                  reads=[Bxt[s3], Brstd[s], Bsc], writes=[Bh1[s]])
            kb.op("pool", lambda: nc.gpsimd.tensor_tensor(out=hb[s][:], in0=h1[s][:], in1=sh[:], op=ALU.add),
                  reads=[Bh1[s], Bsh], writes=[Bhb[s]])

        def stB(i):
            s = i % 2
            if i < 2:
                grp, gi, gn_ = 0, i, 2
            else:
                grp, gi, gn_ = 1 + (i - 2) // 4, (i - 2) % 4, 4
            sg_ = grp % 2
            for hf in range(2):
                pi = (2 * i + hf) % 4
                for c in range(8):
                    cc = hf * 8 + c
                    kb.op("pe", lambda c=c, cc=cc: nc.tensor.transpose(out=pt[pi][:, c, :], in_=hb[s][:, cc * 128:(cc + 1) * 128], identity=ident_b[:]),
                          reads=[Bhb[s]], writes=[Bpt[pi]])
                dst = hT[sg_][:, hf * 8:(hf + 1) * 8, gi * 128:(gi + 1) * 128]
                if hf == 0:
                    kb.op("act", lambda: nc.scalar.copy(out=dst, in_=pt[pi][:]), reads=[Bpt[pi]], writes=[BhT[sg_]])
                else:
                    kb.op("dve", lambda: nc.vector.tensor_copy(out=dst, in_=pt[pi][:]), reads=[Bpt[pi]], writes=[BhT[sg_]])
            if gi == gn_ - 1:
                tk0 = 0 if grp == 0 else LC + (grp - 1) * 512
                nn = gn_ * 128
                for hh in range(2):
                    kb.dma("sp", lambda hh=hh: nc.sync.dma_start(out=hT_d[:, hh * 8:(hh + 1) * 8, tk0:tk0 + nn], in_=hT[sg_][:, hh * 8:(hh + 1) * 8, :nn]), reads=[BhT[sg_]])

        for i in range(NT + 1):
            if i < NT:
                stA(i)
            if i >= 1:
                stB(i - 1)
    if stop_after <= 1:
        kb.finish(); return nc

    def mm(out_, lhsT, rhs, st, sp, R, W):
        return kb.op("pe", lambda: nc.tensor.matmul(out_, lhsT=lhsT, rhs=rhs, start=st, stop=sp), R, W)

    def tr(out_, in_, idn, R, W):
        return kb.op("pe", lambda: nc.tensor.transpose(out=out_, in_=in_, identity=idn), R, W)

    def act(out_, in_, func, R, W, **kw):
        return kb.op("act", lambda: nc.scalar.activation(out=out_, in_=in_, func=func, **kw), R, W)

    def tt(eng, out_, in0, in1, op, R, W):
        e = nc.vector if eng == "dve" else nc.gpsimd
        return kb.op(eng, lambda: e.tensor_tensor(out=out_, in0=in0, in1=in1, op=op), R, W)

    def stt(out_, in0, scalar, in1, op0, op1, R, W):
        return kb.op("dve", lambda: nc.vector.scalar_tensor_tensor(out=out_, in0=in0, scalar=scalar, in1=in1, op0=op0, op1=op1), R, W)

    def ts(eng, out_, in0, s1, s2, op0, op1, R, W):
        e = nc.vector if eng == "dve" else nc.gpsimd
        if op1 is None:
            return kb.op(eng, lambda: e.tensor_scalar(out=out_, in0=in0, scalar1=s1, scalar2=None, op0=op0), R, W)
        return kb.op(eng, lambda: e.tensor_scalar(out=out_, in0=in0, scalar1=s1, scalar2=s2, op0=op0, op1=op1), R, W)

    def cp(eng, out_, in_, R, W):
        if eng == "act":
            return kb.op("act", lambda: nc.scalar.copy(out=out_, in_=in_), R, W)
        e = nc.vector if eng == "dve" else nc.gpsimd
        return kb.op(eng, lambda: e.tensor_copy(out=out_, in_=in_), R, W)

    def ld(q, out_, in_, W, R=()):
        e = nc.sync if q == "sp" else nc.gpsimd
        return kb.dma(q, lambda: e.dma_start(out=out_, in_=in_), R, W)

    qn_in = din("q_norm", [128, 1]); kn_in = din("k_norm", [128, 1])
    rm_in = din("rotm", [128, 128]); cos_in = din("cosT", [128, T]); sin_in = din("sinT", [128, T])
    qaT_d = dscr("qaT_d", [8, 128, T], BF16); qrT_d = dscr("qrT_d", [4, 128, T], BF16)
    sgT_d = dscr("sgT_d", [8, 128, T], BF16); gaT_d = dscr("gaT_d", [16, 128, T], BF16); grT_d = dscr("grT_d", [16, 128, T], BF16)
    kaT_d = dscr("kaT_d", [2, 128, TK], BF16); krT_d = dscr("krT_d", [4, 128, TK], BF16)
    va_d = dscr("va_d", [TK, 256], BF16); vr_d = dscr("vr_d", [TK, 1024], BF16)
    ones_b = G.sb("ones_b", [128, 128], BF16)
    kb.op("dve", lambda: nc.vector.memset(ones_b[:], 1.0), writes=[B_const])
    kb.barrier()

    with Phase(kb) as ph:
        qn_t = ph.sb("qn_t", [128, 1], F32); kn_t = ph.sb("kn_t", [128, 1], F32)
        rm_b = ph.sb("rm_b", [128, 128], BF16)
        cos_t = ph.sb("cos_t", [128, T], F32); sin_t = ph.sb("sin_t", [128, T], F32)
        Bc = Buf()
        ld("sp", qn_t[:], qn_in[:, :], [Bc]); ld("sp", kn_t[:], kn_in[:, :], [Bc])
        ld("pool", rm_b[:], rm_in[:, :], [Bc])
        ld("sp", cos_t[:], cos_in[:, :], [Bc]); ld("sp", sin_t[:], sin_in[:, :], [Bc])
        wv = w_in.rearrange("(c p) n -> p c n", p=128)
        Wt = [ph.sb(f"Wt{i}", [128, NCH, 768], BF16) for i in range(2)]; BW = [Buf(), Buf()]
        hB = [ph.sb(f"hB{i}", [128, NCH, 512], BF16) for i in range(2)]; BhB = [Buf(), Buf()]
        pz = [ph.ps(f"pz{i}", [128, 512], F32) for i in range(4)]; Bpz = [Buf() for _ in range(4)]
        pq = [ph.ps(f"pq{i}", [128, 512], F32) for i in range(2)]; Bpq = [Buf() for _ in range(2)]
        pr = [ph.ps(f"pr{i}", [128, 512], F32) for i in range(2)]; Bpr = [Buf() for _ in range(2)]
        sqb = [ph.sb(f"sqb{i}", [128, 512], BF16) for i in range(2)]; Bsq = [Buf() for _ in range(2)]
        sd = [ph.sb(f"sd{i}", [128, 512], F32) for i in range(2)]; Bsd = [Buf() for _ in range(2)]
        qnb = [ph.sb(f"qnb{i}", [128, 512], BF16) for i in range(3)]; Bqn = [Buf() for _ in range(3)]
        t1 = [ph.sb(f"t1{i}", [128, 512], F32) for i in range(2)]; Bt1 = [Buf() for _ in range(2)]
        t2 = [ph.sb(f"t2{i}", [128, 512], F32) for i in range(2)]; Bt2 = [Buf() for _ in range(2)]
        ob = [ph.sb(f"ob{i}", [128, 512], BF16) for i in range(4)]; Bob = [Buf() for _ in range(4)]
        cnt = dict(z=0, q=0, r=0, sq=0, sd=0, qn=0, t=0, ob=0, w=0, h=0)
        deferred = []

        def nxt(k, n):
            v = cnt[k] % n
            cnt[k] += 1
            return v

        def run_deferred():
            todo = list(deferred)
            deferred.clear()
            for f in todo:
                f()

        def rope_and_store(iq, N, t0, dst):
            ir = nxt("r", 2)
            mm(pr[ir][:, :N], rm_b[:], qnb[iq][:, :N], True, True, [Bqn[iq], Bc], [Bpr[ir]])

            def fin():
                it = nxt("t", 2); io = nxt("ob", 4)
                tt("dve", t1[it][:, :N], qnb[iq][:, :N], cos_t[:, t0:t0 + N], ALU.mult, [Bqn[iq], Bc], [Bt1[it]])
                tt("dve", t2[it][:, :N], pr[ir][:, :N], sin_t[:, t0:t0 + N], ALU.mult, [Bpr[ir], Bc], [Bt2[it]])
                tt("pool", ob[io][:, :N], t1[it][:, :N], t2[it][:, :N], ALU.add, [Bt1[it], Bt2[it]], [Bob[io]])
                ld("pool", dst, ob[io][:, :N], [], [Bob[io]])
            deferred.append(fin)

        def evac(kind, iz, N, t0, dst, rope):
            if kind in ("silu", "sig"):
                io = nxt("ob", 4)
                act(ob[io][:, :N], pz[iz][:, :N], AF.Silu if kind == "silu" else AF.Sigmoid, [Bpz[iz]], [Bob[io]])
                ld("pool", dst, ob[io][:, :N], [], [Bob[io]])
                return
            if kind[0] == "r":
                iq = nxt("qn", 3)
                sc = (128.0 ** -0.5) if kind == "rk" else 1.0
                act(qnb[iq][:, :N], pz[iz][:, :N], AF.Copy, [Bpz[iz]], [Bqn[iq]], scale=sc)
                if rope:
                    deferred.append(lambda: rope_and_store(iq, N, t0, dst))
                else:
                    ld("pool", dst, qnb[iq][:, :N], [], [Bqn[iq]])
                return
            gt = qn_t if kind == "nq" else kn_t
            isq = nxt("sq", 2)
            act(sqb[isq][:, :N], pz[iz][:, :N], AF.Square, [Bpz[iz]], [Bsq[isq]])

            def st2():
                ip = nxt("q", 2)
                mm(pq[ip][:, :N], ones_b[:], sqb[isq][:, :N], True, True, [Bsq[isq], B_const], [Bpq[ip]])
                isd = nxt("sd", 2)
                act(sd[isd][:, :N], pq[ip][:, :N], AF.Sqrt, [Bpq[ip], B_const], [Bsd[isd]], bias=eps_t[:], scale=1.0 / 128)
                kb.op("dve", lambda: nc.vector.reciprocal(out=sd[isd][:, :N], in_=sd[isd][:, :N]), [], [Bsd[isd]])
                iq = nxt("qn", 3)
                stt(qnb[iq][:, :N], pz[iz][:, :N], gt[:], sd[isd][:, :N], ALU.mult, ALU.mult, [Bpz[iz], Bsd[isd], Bc], [Bqn[iq]])
                if rope:
                    deferred.append(lambda: rope_and_store(iq, N, t0, dst))
                else:
                    ld("pool", dst, qnb[iq][:, :N], [], [Bqn[iq]])
            deferred.append(st2)

        groups = []
        for g in range(2):
            groups.append((g * 512, 512, "nq", qaT_d, g * 4, False))
        groups.append((1024, 512, "rq", qrT_d, 0, False))
        for g in range(2):
            groups.append((1536 + g * 512, 512, "silu", sgT_d, g * 4, False))
        for g in range(4):
            groups.append((2560 + g * 512, 512, "sig", gaT_d, g * 4, False))
        for g in range(4):
            groups.append((4608 + g * 512, 512, "sig", grT_d, g * 4, False))
        groups.append((-1, 768, "k", None, 0, True))
        vgroups = [(6912, 256, va_d, 0), (7680, 512, vr_d, 0), (8192, 512, vr_d, 512)]

        def load_w(gidx):
            iw_ = gidx % 2
            if gidx < len(groups):
                (c0_, ncol_, kind_, _, _, _) = groups[gidx]
                if kind_ == "k":
                    ld("pool", Wt[iw_][:, :, 0:256], wv[:, :, 6656:6912], [BW[iw_]])
                    ld("pool", Wt[iw_][:, :, 256:768], wv[:, :, 7168:7680], [BW[iw_]])
                else:
                    ld("pool", Wt[iw_][:, :, 0:512], wv[:, :, c0_:c0_ + 512], [BW[iw_]])
            elif gidx - len(groups) < len(vgroups):
                (c0_, ncol_, _, _) = vgroups[gidx - len(groups)]
                ld("pool", Wt[iw_][:, :, 0:ncol_], wv[:, :, c0_:c0_ + ncol_], [BW[iw_]])
        load_w(0)
        for gidx, (c0, ncol, kind, dstT, h0, kv) in enumerate(groups):
            iw = gidx % 2
            load_w(gidx + 1)
            blks = range(0, 9) if kv else range(1, 9)
            for blk in blks:
                ih = nxt("h", 2)
                tk0 = 0 if blk == 0 else LC + (blk - 1) * 512
                N = LC if blk == 0 else 512
                ld("sp", hB[ih][:, :, :N], hT_d[:, :, tk0:tk0 + N], [BhB[ih]])
                for ch in range(ncol // 128):
                    iz = nxt("z", 4)
                    for c in range(NCH):
                        mm(pz[iz][:, :N], Wt[iw][:, c, ch * 128:(ch + 1) * 128], hB[ih][:, c, :N], c == 0, c == NCH - 1, [BW[iw], BhB[ih]], [Bpz[iz]])
                    run_deferred()
                    t0 = tk0 - LC
                    if kind == "k":
                        if ch < 2:
                            evac("nk", iz, N, t0, kaT_d[ch, :, tk0:tk0 + N], rope=(blk > 0))
                        else:
                            evac("rk", iz, N, t0, krT_d[ch - 2, :, tk0:tk0 + N], rope=(blk > 0))
                    else:
                        evac(kind, iz, N, t0, dstT[h0 + ch, :, t0:t0 + N], rope=True)
        run_deferred(); run_deferred(); run_deferred()
        pv = pz
        vo = [ph.sb(f"vo{i}", [128, 512], BF16) for i in range(2)]; Bvo = [Buf(), Buf()]
        for vi, (c0, ncol, dstT, dc0) in enumerate(vgroups):
            gidx = len(groups) + vi
            iw = gidx % 2
            load_w(gidx + 1)
            for blk in range(0, 9):
                ih = nxt("h", 2)
                tk0 = 0 if blk == 0 else LC + (blk - 1) * 512
                N = LC if blk == 0 else 512
                ld("sp", hB[ih][:, :, :N], hT_d[:, :, tk0:tk0 + N], [BhB[ih]])
                for tl in range(N // 128):
                    iz = nxt("z", 4)
                    for c in range(NCH):
                        mm(pv[iz][:, :ncol], hB[ih][:, c, tl * 128:(tl + 1) * 128], Wt[iw][:, c, :ncol], c == 0, c == NCH - 1, [BW[iw], BhB[ih]], [Bpz[iz]])
                    io = nxt("ob", 2)
                    if tl % 2 == 0:
                        cp("act", vo[io][:, :ncol], pv[iz][:, :ncol], [Bpz[iz]], [Bvo[io]])
                    else:
                        cp("dve", vo[io][:, :ncol], pv[iz][:, :ncol], [Bpz[iz]], [Bvo[io]])
                    r0 = tk0 + tl * 128
                    ld("pool", dstT[r0:r0 + 128, dc0:dc0 + ncol], vo[io][:, :ncol], [], [Bvo[io]])
    if stop_after <= 2:
        kb.finish(); return nc

    oaT_d = dscr("oaT_d", [8, 128, T], BF16)
    orT_d = dscr("orT_d", [8, 128, T], BF16)

    ones_f = G.sb("ones_f", [128, 128], F32)
    kb.op("dve", lambda: nc.vector.memset(ones_f[:], 1.0), writes=[B_const])
    kb.barrier()
    with Phase(kb) as ph:
        KT = ph.sb("KT", [128, TK], BF16); BKT = Buf()
        Vt = ph.sb("Vt", [128, TK // 128, 128], BF16); BVt = Buf()
        QT = [ph.sb(f"QT{i}", [128, 512], BF16) for i in range(2)]; BQT = [Buf(), Buf()]
        NS = 2
        ps_s = [ph.ps(f"ps_s{i}", [128, 1024], F32) for i in range(NS)]; Bps = [Buf() for _ in range(NS)]
        po = [ph.ps(f"po{i}", [128, 512], F32) for i in range(2)]; Bpo = [Buf(), Buf()]
        pd = [ph.ps(f"pd{i}", [128, 512], F32) for i in range(2)]; Bpd = [Buf(), Buf()]
        NPT = 3
        pT = [ph.sb(f"pT{i}", [128, 1024], BF16) for i in range(NPT)]; BpT = [Buf() for _ in range(NPT)]
        accO = [ph.sb(f"accO{i}", [128, 512], F32) for i in range(2)]; BaO = [Buf(), Buf()]
        rd = [ph.sb(f"rd{i}", [128, 512], F32) for i in range(2)]; Brd = [Buf(), Buf()]
        oo = [ph.sb(f"oo{i}", [128, 512], BF16) for i in range(2)]; Boo = [Buf(), Buf()]
        NKT = TK // 128
        NPR = NKT // 2
        it = 0
        pend = []
        sidx = 0
        pidx_ = 0
        for g in range(0 if not SKIP3 else 2, 2):
            ld("sp", KT[:], kaT_d[g, :, :], [BKT])
            ld("sp", Vt[:], va_d[:, g * 128:(g + 1) * 128].rearrange("(n p) d -> p n d", p=128), [BVt])
            for j in range(4):
                h = g * 4 + j
                for qb in range(T // 512):
                    iq = it % 2; it += 1
                    t0 = qb * 512
                    ld("sp", QT[iq][:], qaT_d[h, :, t0:t0 + 512], [BQT[iq]])
                    s_of = {}

                    def issue_s(pr):
                        nonlocal sidx
                        s_of[pr] = sidx % NS; sidx += 1
                        sn = s_of[pr]
                        for u in range(2):
                            kt = 2 * pr + u
                            mm(ps_s[sn][:, u * 512:(u + 1) * 512], KT[:, kt * 128:(kt + 1) * 128], QT[iq][:], True, True, [BKT, BQT[iq]], [Bps[sn]])
                    issue_s(0)
                    for pr in range(NPR):
                        if pr + 1 < NPR:
                            issue_s(pr + 1)
                        if pr == 2 and pend:
                            pend.pop(0)()
                        sc_ = s_of[pr]
                        ip = pidx_ % NPT; pidx_ += 1
                        act(pT[ip][:], ps_s[sc_][:], AF.Exp, [Bps[sc_]], [BpT[ip]], scale=128.0 ** -0.5)
                        for u in range(2):
                            kt = 2 * pr + u
                            mm(po[iq][:], Vt[:, kt, :], pT[ip][:, u * 512:(u + 1) * 512], kt == 0, kt == NKT - 1, [BVt, BpT[ip]], [Bpo[iq]])
                        mm(pd[iq][:], ones_b[:], pT[ip][:, 0:512], pr == 0, False, [B_const, BpT[ip]], [Bpd[iq]])
                        if pr == 0:
                            cp("dve", accO[iq][:], pT[ip][:, 512:1024], [BpT[ip]], [BaO[iq]])
                        else:
                            tt("dve", accO[iq][:], accO[iq][:], pT[ip][:, 512:1024], ALU.add, [BpT[ip]], [BaO[iq]])

                    def epilogue(iq=iq, h=h, t0=t0):
                        mm(pd[iq][:], ones_f[:], accO[iq][:], False, True, [B_const, BaO[iq]], [Bpd[iq]])
                        kb.op("dve", lambda: nc.vector.reciprocal(out=rd[iq][:], in_=pd[iq][:]), [Bpd[iq]], [Brd[iq]])
                        tt("dve", oo[iq][:], po[iq][:], rd[iq][:], ALU.mult, [Bpo[iq], Brd[iq]], [Boo[iq]])
                        ld("pool", oaT_d[h, :, t0:t0 + 512], oo[iq][:], [], [Boo[iq]])
                    pend.append(epilogue)
        while pend:
            pend.pop(0)()
    if stop_after <= 3:
        kb.finish(); return nc

    rdec_in = din("ret_decay", [1, 8])
    gn_in = din("ret_gn", [128, 8])
    tabs_in = din("ret_tabs", [6, 128, 128])
    pcols_in = din("ret_pcols", [128, 6])
    with Phase(kb) as ph:
        Bt = Buf()
        lg = ph.sb("lg", [128, 8], F32)
        tabs = ph.sb("tabs", [128, 6, 128], F32)
        pcols = ph.sb("pcols", [128, 6], F32)
        gn_t = ph.sb("gn_t", [128, 8], F32)
        ld("sp", lg[:], rdec_in[0:1, :].partition_broadcast(128), [Bt])
        ld("sp", tabs[:], tabs_in.rearrange("k p i -> p k i"), [Bt])
        ld("sp", pcols[:], pcols_in[:, :], [Bt])
        ld("sp", gn_t[:], gn_in[:, :], [Bt])
        act(lg[:], lg[:], AF.Exp, [], [Bt])
        act(lg[:], lg[:], AF.Ln, [], [Bt], bias=1.0)
        ts("dve", lg[:], lg[:], -1.0, None, ALU.mult, None, [], [Bt])
        kT = ph.sb("kT", [128, TK], BF16); BkT = Buf()
        qT = ph.sb("qT", [128, T], BF16); BqT = Buf()
        V = ph.sb("V", [128, TK // 128, 256], BF16); BV = Buf()
        sg = ph.sb("sg", [128, 2, T], BF16); Bsg = Buf()
        wq = ph.sb("wq", [128, 2, 128], F32); Bwq = Buf()
        wk = ph.sb("wk", [128, 6], F32); Bwk = Buf()
        gch = ph.sb("gch", [128, 2], F32); Bgch = Buf()
        MT = ph.sb("MT", [128, 128], F32); BMT = Buf()
        mtmp = ph.sb("mtmp", [128, 2, 128], F32); Bmtmp = Buf()
        kw = ph.sb("kw", [128, TK // 128, 2, 128], BF16); Bkwn = [Buf() for _ in range(TK // 128)]
        S32 = [ph.sb(f"S32{i}", [128, 256], F32) for i in range(2)]; BS32 = [Buf(), Buf()]
        SB = [ph.sb(f"SB{i}", [128, 32, 256], BF16) for i in range(2)]; BSB = [Buf(), Buf()]
        ptk = [ph.ps(f"ptk{i}", [128, 2, 128], BF16) for i in range(2)]; Bptk = [Buf(), Buf()]
        pu = [ph.ps(f"pu{i}", [128, 256], F32) for i in range(2)]; Bpu = [Buf(), Buf()]
        pS = ph.ps("pS", [128, 128], F32); BpS = Buf()
        pO = [ph.ps(f"pO{i}", [128, 256], F32) for i in range(3)]; BpO = [Buf() for _ in range(3)]
        Sm = [ph.sb(f"Sm{i}", [128, 128], BF16) for i in range(3)]; BSm = [Buf() for _ in range(3)]
        qwa = ph.sb("qwa", [128, 2, T], BF16); Bqwa = Buf()
        gsg = ph.sb("gsg", [128, 2, T], BF16); Bgsg = Buf()
        nmr = ph.sb("nmr", [128, 32], F32)
        Oall = ph.sb("Oall", [128, 32, 256], F32); BOall = [Buf() for _ in range(32)]
        bst = ph.sb("bst", [128, 32, 6], F32); Bbst = [Buf() for _ in range(32)]
        mv = ph.sb("mv", [128, 32, 2], F32); Bmv = Buf()
        rs_ = ph.sb("rs", [128, 32], F32); Brs = Buf()
        mh32 = ph.sb("mh32", [128, 32], F32)
        kb.op("dve", lambda: nc.vector.memset(mh32[:], -0.5), [], [Bt])
        on = [ph.sb(f"on{i}", [128, 256], BF16) for i in range(2)]; Bon = [Buf(), Buf()]
        orb = [ph.sb(f"orb{i}", [128, 2, 512], BF16) for i in range(2)]; Borb = [Buf(), Buf()]
        NKT = TK // 128
        for h in range(4):
            fcol = lg[:, h:h + 1]; bcol = lg[:, 4 + h:5 + h]
            ld("sp", kT[:], krT_d[h, :, :], [BkT])
            ld("sp", qT[:], qrT_d[h, :, :], [BqT])
            ld("sp", V[:], vr_d[:, h * 256:(h + 1) * 256].rearrange("(n p) d -> p n d", p=128), [BV])
            ld("sp", sg[:], sgT_d[2 * h:2 * h + 2, :, :].rearrange("c p t -> p c t"), [Bsg])
            act(wq[:, 0, :], tabs[:, 0, :], AF.Exp, [Bt], [Bwq], scale=fcol)
            act(wq[:, 1, :], tabs[:, 1, :], AF.Exp, [Bt], [Bwq], scale=bcol)
            for k in range(6):
                act(wk[:, k:k + 1], pcols[:, k:k + 1], AF.Exp, [Bt], [Bwk], scale=(fcol if k in (0, 2, 3) else bcol))
            act(gch[:, 0:1], fcol, AF.Exp, [Bt], [Bgch], scale=128.0)
            act(gch[:, 1:2], bcol, AF.Exp, [Bt], [Bgch], scale=128.0)
            act(mtmp[:, 0, :], tabs[:, 2, :], AF.Exp, [Bt], [Bmtmp], scale=fcol)
            act(mtmp[:, 1, :], tabs[:, 4, :], AF.Exp, [Bt], [Bmtmp], scale=bcol)
            tt("dve", mtmp[:, 0, :], mtmp[:, 0, :], tabs[:, 3, :], ALU.mult, [Bt], [Bmtmp])
            tt("dve", mtmp[:, 1, :], mtmp[:, 1, :], tabs[:, 5, :], ALU.mult, [Bt], [Bmtmp])
            tt("dve", MT[:], mtmp[:, 0, :], mtmp[:, 1, :], ALU.add, [Bmtmp], [BMT])
            for k in range(2):
                tt("pool", qwa[:, k, :].rearrange("p (c i) -> p c i", i=128), qT[:].rearrange("p (c i) -> p c i", i=128),
                   wq[:, k, :].unsqueeze(1).to_broadcast([128, 32, 128]), ALU.mult, [BqT, Bwq], [Bqwa])
                act(gsg[:, k, :], sg[:, k, :], AF.Copy, [Bsg, Bt], [Bgsg], scale=gn_t[:, 2 * h + k:2 * h + k + 1])
            for n in range(NKT):
                ip = n % 2
                tr(ptk[ip][:, 0, :], kT[:, n * 128:(n + 1) * 128], ident_b[:], [BkT, B_const], [Bptk[ip]])
                if n == 0:
                    cf, cb = 2, 4
                elif n == 1:
                    cf, cb = 3, 5
                else:
                    cf, cb = 0, 1
                if ip == 0:
                    ts("dve", kw[:, n, 0, :], ptk[ip][:, 0, :], wk[:, cf:cf + 1], None, ALU.mult, None, [Bptk[ip], Bwk], [Bkwn[n]])
                    ts("dve", kw[:, n, 1, :], ptk[ip][:, 0, :], wk[:, cb:cb + 1], None, ALU.mult, None, [Bptk[ip], Bwk], [Bkwn[n]])
                else:
                    act(kw[:, n, 0, :], ptk[ip][:, 0, :], AF.Copy, [Bptk[ip], Bwk], [Bkwn[n]], scale=wk[:, cf:cf + 1])
                    act(kw[:, n, 1, :], ptk[ip][:, 0, :], AF.Copy, [Bptk[ip], Bwk], [Bkwn[n]], scale=wk[:, cb:cb + 1])
            for d_ in range(2):
                mm(pu[d_][:], kw[:, 0, d_, :], V[:, 0, :], True, False, [Bkwn[0], BV], [Bpu[d_]])
                mm(pu[d_][:], kw[:, 1, d_, :], V[:, 1, :], False, True, [Bkwn[1], BV], [Bpu[d_]])
                cp("dve", S32[d_][:], pu[d_][:], [Bpu[d_]], [BS32[d_]])
                first = 0 if d_ == 0 else 31
                cp("act", SB[d_][:, first, :], S32[d_][:], [BS32[d_]], [BSB[d_]])
            for k in range(31):
                for d_ in range(2):
                    c = k if d_ == 0 else 31 - k
                    mm(pu[d_][:], kw[:, 2 + c, d_, :], V[:, 2 + c, :], True, True, [Bkwn[2 + c], BV], [Bpu[d_]])
                    stt(S32[d_][:], S32[d_][:], gch[:, d_:d_ + 1], pu[d_][:], ALU.mult, ALU.add, [Bpu[d_], Bgch], [BS32[d_]])
                    nxtc = c + 1 if d_ == 0 else c - 1
                    cp("act", SB[d_][:, nxtc, :], S32[d_][:], [BS32[d_]], [BSB[d_]])
            for c in range(32):
                i3 = c % 3
                t0 = c * 128
                mm(pS[:], kT[:, LC + t0:LC + t0 + 128], qT[:, t0:t0 + 128], True, True, [BkT, BqT], [BpS])
                tt("dve", Sm[i3][:], pS[:], MT[:], ALU.mult, [BpS, BMT], [BSm[i3]])
                mm(pO[i3][:], Sm[i3][:], V[:, 2 + c, :], True, False, [BSm[i3], BV], [BpO[i3]])
                mm(pO[i3][:], qwa[:, 0, t0:t0 + 128], SB[0][:, c, :], False, False, [Bqwa, BSB[0]], [BpO[i3]])
                mm(pO[i3][:], qwa[:, 1, t0:t0 + 128], SB[1][:, c, :], False, True, [Bqwa, BSB[1]], [BpO[i3]])
                cp("act", Oall[:, c, :], pO[i3][:], [BpO[i3]], [BOall[c]])
                kb.op("dve", lambda c=c: nc.vector.bn_stats(out=bst[:, c, :], in_=Oall[:, c, :]), [BOall[c]], [Bbst[c]])
            for c in range(32):
                kb.op("dve", lambda c=c: nc.vector.bn_aggr(out=mv[:, c, :], in_=bst[:, c, :]), [Bbst[c]], [Bmv])
            kb.op("dve", lambda: nc.vector.tensor_scalar(out=rs_[:], in0=mv[:, :, 1], scalar1=EPS, scalar2=None, op0=ALU.add), [Bmv], [Brs])
            kb.op("pool", lambda: nc.gpsimd.tensor_tensor(out=rs_[:], in0=rs_[:], in1=mh32[:], op=ALU.pow), [Bt], [Brs])
            stt(nmr[:], mv[:, :, 0], -1.0, rs_[:], ALU.mult, ALU.mult, [Bmv], [Brs])
            for c in range(32):
                i2 = c % 2
                t0 = c * 128
                act(on[i2][:], Oall[:, c, :], AF.Identity, [BOall[c], Bmv, Brs], [Bon[i2]], scale=rs_[:, c:c + 1], bias=nmr[:, c:c + 1])
                io = (c // 4) % 2
                for k in range(2):
                    tr(ptk[i2][:, k, :], on[i2][:, k * 128:(k + 1) * 128], ident_b[:], [Bon[i2], B_const], [Bptk[i2]])
                tt("dve", orb[io][:, :, (c % 4) * 128:(c % 4 + 1) * 128], ptk[i2][:], gsg[:, :, t0:t0 + 128], ALU.mult, [Bptk[i2], Bgsg], [Borb[io]])
                if c % 4 == 3:
                    tb = (c // 4) * 512
                    ld("pool", orT_d[2 * h:2 * h + 2, :, tb:tb + 512].rearrange("c p t -> p c t"), orb[io][:], [], [Borb[io]])
    if stop_after <= 4:
        kb.finish(); return nc

    w_oa_in = din("w_o_att", [1024, D]); w_or_in = din("w_o_ret", [1024, D]); w_out_in = din("w_out", [D, D])
    w_r_in = din("w_router", [D, 16])
    yT_d = dscr("yT_d", [128, NCH, T], BF16)
    x1_d = dscr("x1_d", [T, D], F32)
    h2_d = dscr("h2_d", [T, D], BF16)
    AFF = G.sb("AFF", [128, 32, 16], F32); BAFF = Buf()
    with Phase(kb) as ph:
        Woa = ph.sb("Woa", [128, 8, D], BF16); Wor = ph.sb("Wor", [128, 8, D], BF16); BWo = Buf()
        BWo_b = Buf()
        ld("pool", Woa[:], w_oa_in.rearrange("(c p) n -> p c n", p=128), [BWo])
        ld("pool", Wor[:], w_or_in.rearrange("(c p) n -> p c n", p=128), [BWo_b])
        oa = [ph.sb(f"oa{i}", [128, 8, 512], BF16) for i in range(2)]; Boa = [Buf(), Buf()]
        orr = [ph.sb(f"orr{i}", [128, 8, 512], BF16) for i in range(2)]; Borr = [Buf(), Buf()]
        ga = [ph.sb(f"ga{i}", [128, 16, 512], BF16) for i in range(2)]; Bga = [Buf(), Buf()]
        gr = [ph.sb(f"gr{i}", [128, 16, 512], BF16) for i in range(2)]; Bgr = [Buf(), Buf()]
        pA = [ph.ps(f"pA{i}", [128, 512], F32) for i in range(2)]; BpA = [Buf(), Buf()]
        pB = [ph.ps(f"pB{i}", [128, 512], F32) for i in range(2)]; BpB = [Buf(), Buf()]
        ta = [ph.sb(f"ta{i}", [128, 512], F32) for i in range(2)]; Bta = [Buf(), Buf()]
        tb_ = [ph.sb(f"tb{i}", [128, 512], F32) for i in range(2)]; Btb = [Buf(), Buf()]
        yTb = [ph.sb(f"yTb{i}", [128, NCH, 512], BF16) for i in range(2)]; ByT = [Buf(), Buf()]
        k = 0
        for blk in range(T // 512):
            ib = blk % 2
            t0 = blk * 512
            ld("sp", oa[ib][:], oaT_d[:, :, t0:t0 + 512].rearrange("c p t -> p c t"), [Boa[ib]])
            ld("sp", orr[ib][:], orT_d[:, :, t0:t0 + 512].rearrange("c p t -> p c t"), [Borr[ib]])
            ld("sp", ga[ib][:], gaT_d[:, :, t0:t0 + 512].rearrange("c p t -> p c t"), [Bga[ib]])
            ld("sp", gr[ib][:], grT_d[:, :, t0:t0 + 512].rearrange("c p t -> p c t"), [Bgr[ib]])
            for dm in range(16):
                i2 = k % 2; k += 1
                for f in range(8):
                    mm(pA[i2][:], Woa[:, f, dm * 128:(dm + 1) * 128], oa[ib][:, f, :], f == 0, f == 7, [BWo, Boa[ib]], [BpA[i2]])
                for f in range(8):
                    mm(pB[i2][:], Wor[:, f, dm * 128:(dm + 1) * 128], orr[ib][:, f, :], f == 0, f == 7, [BWo_b, Borr[ib]], [BpB[i2]])
                tt("dve", ta[i2][:], pA[i2][:], ga[ib][:, dm, :], ALU.mult, [BpA[i2], Bga[ib]], [Bta[i2]])
                tt("dve", tb_[i2][:], pB[i2][:], gr[ib][:, dm, :], ALU.mult, [BpB[i2], Bgr[ib]], [Btb[i2]])
                tt("pool", yTb[ib][:, dm, :], ta[i2][:], tb_[i2][:], ALU.add, [Bta[i2], Btb[i2]], [ByT[ib]])
            ld("pool", yT_d[:, :, t0:t0 + 512], yTb[ib][:], [], [ByT[ib]])
    if stop_after <= 5:
        kb.finish(); return nc

    with Phase(kb) as ph:
        Wo = ph.sb("Wo", [128, NCH, D], BF16); BWo2 = Buf()
        ld("pool", Wo[:], w_out_in.rearrange("(c p) n -> p c n", p=128), [BWo2])
        wr = ph.sb("wr", [128, NCH, 16], F32); Bwr = Buf()
        ld("sp", wr[:], w_r_in.rearrange("(c p) e -> p c e", p=128), [Bwr])
        gt1, Bgt1 = bcast_row(ph, "gt1", mod_d[0:1, 2 * D:3 * D])
        po1, Bpo1 = bcast_row(ph, "po1", post1[0:1, :])
        tt("dve", gt1[:], gt1[:], po1[:], ALU.mult, [Bpo1], [Bgt1])
        g2, Bg2 = bcast_row(ph, "g2", mod_d[0:1, 4 * D:5 * D])
        ld("sp", po1[:], pre2[0:1, :].partition_broadcast(128), [Bpo1])
        stt(g2[:], g2[:], 1.0, po1[:], ALU.add, ALU.mult, [Bpo1], [Bg2])
        s2, Bs2 = bcast_row(ph, "s2", mod_d[0:1, 3 * D:4 * D])
        yTt = [ph.sb(f"yTt{i}", [128, NCH, 128], BF16) for i in range(3)]; ByTt = [Buf() for _ in range(3)]
        xt = [ph.sb(f"xt{i}", [128, D], F32) for i in range(3)]; Bxt = [Buf() for _ in range(3)]
        pY = [ph.ps(f"pY{i}", [128, 512], F32) for i in range(4)]; BpY = [Buf() for _ in range(4)]
        ptr = [ph.ps(f"ptr{i}", [128, 4, 128], F32) for i in range(2)]; Bptr = [Buf(), Buf()]
        pL = ph.ps("pL", [128, 16], F32); BpL = Buf()
        Ysb = [ph.sb(f"Ysb{i}", [128, D], F32) for i in range(2)]; BY = [Buf(), Buf()]
        junk = ph.sb("junk", [128, D], BF16); Bjunk = Buf()
        sq = [ph.sb(f"sq5{i}", [128, 4], F32) for i in range(2)]; Bsq5 = [Buf(), Buf()]
        x1t = [ph.sb(f"x1t{i}", [128, D], F32) for i in range(2)]; Bx1 = [Buf(), Buf()]
        h2f = [ph.sb(f"h2f{i}", [128, D], F32) for i in range(2)]; Bh2f = [Buf(), Buf()]
        h2b = [ph.sb(f"h2b{i}", [128, D], BF16) for i in range(2)]; Bh2b = [Buf(), Buf()]
        h2T = ph.sb("h2T", [128, NCH, 128], F32); Bh2T = Buf()
        lgt = [ph.sb(f"lgt{i}", [128, 16], F32) for i in range(2)]; Blgt = [Buf(), Buf()]
        NT5 = T // 128

        def ld5(i):
            if i < NT5:
                t0_ = i * 128
                ld("sp", yTt[i % 3][:], yT_d[:, :, t0_:t0_ + 128], [ByTt[i % 3]])
                ld("sp", xt[i % 3][:], xk[LC + t0_:LC + t0_ + 128, :], [Bxt[i % 3]])
        ld5(0); ld5(1)

        def s5A(i):
            s = i % 2; s3 = i % 3
            t0 = i * 128
            ld5(i + 2)
            for db in range(4):
                for c in range(NCH):
                    mm(pY[db][:], yTt[s3][:, c, :], Wo[:, c, db * 512:(db + 1) * 512], c == 0, c == NCH - 1, [ByTt[s3], BWo2], [BpY[db]])
            for db in range(4):
                cp("act", Ysb[s][:, db * 512:(db + 1) * 512], pY[db][:], [BpY[db]], [BY[s]])
            act(junk[:], Ysb[s][:], AF.Square, [BY[s]], [Bjunk, Bsq5[s]], accum_out=sq[s][:, 0:1])
            rstd_from(sq[s][:, 1:2], sq[s][:, 0:1], D, [], [Bsq5[s]])
            stt(Ysb[s][:], Ysb[s][:], sq[s][:, 1:2], gt1[:], ALU.mult, ALU.mult, [Bsq5[s], Bgt1], [BY[s]])
            tt("dve", x1t[s][:], Ysb[s][:], xt[s3][:], ALU.add, [BY[s], Bxt[s3]], [Bx1[s]])
            ld("pool", x1_d[t0:t0 + 128, :], x1t[s][:], [], [Bx1[s]])

        def s5B(i):
            s = i % 2
            t0 = i * 128
            act(junk[:], x1t[s][:], AF.Square, [Bx1[s]], [Bjunk, Bsq5[s]], accum_out=sq[s][:, 2:3])
            rstd_from(sq[s][:, 3:4], sq[s][:, 2:3], D, [], [Bsq5[s]])
            stt(h2f[s][:], x1t[s][:], sq[s][:, 3:4], g2[:], ALU.mult, ALU.mult, [Bx1[s], Bsq5[s], Bg2], [Bh2f[s]])
            tt("dve", h2f[s][:], h2f[s][:], s2[:], ALU.add, [Bs2], [Bh2f[s]])

        def s5C(i):
            s = i % 2
            t0 = i * 128
            cp("act", h2b[s][:], h2f[s][:], [Bh2f[s]], [Bh2b[s]])
            ld("pool", h2_d[t0:t0 + 128, :], h2b[s][:], [], [Bh2b[s]])
            for q4 in range(4):
                ip = q4 % 2
                for c in range(4):
                    cc = q4 * 4 + c
                    tr(ptr[ip][:, c, :], h2f[s][:, cc * 128:(cc + 1) * 128], ident_f[:], [Bh2f[s], B_const], [Bptr[ip]])
                cp("dve" if q4 % 2 == 0 else "act", h2T[:, q4 * 4:(q4 + 1) * 4, :], ptr[ip][:], [Bptr[ip]], [Bh2T])
            for c in range(NCH):
                mm(pL[:], h2T[:, c, :], wr[:, c, :], c == 0, c == NCH - 1, [Bh2T, Bwr], [BpL])
            cp("dve", AFF[:, i, :], pL[:], [BpL], [BAFF])

        s5A(0)
        for i in range(NT5):
            if i >= 1:
                s5C(i - 1)
            s5B(i)
            if i + 1 < NT5:
                s5A(i + 1)
        s5C(NT5 - 1)
        mx = ph.sb("mx", [128, 32], F32); Bmx = Buf()
        kb.op("dve", lambda: nc.vector.tensor_reduce(out=mx[:], in_=AFF[:], axis=AX.X, op=ALU.max), [BAFF], [Bmx])
        tt("dve", AFF[:], AFF[:], mx[:].unsqueeze(2).to_broadcast([128, 32, 16]), ALU.subtract, [Bmx], [BAFF])
        act(AFF[:], AFF[:], AF.Exp, [], [BAFF])
        kb.op("dve", lambda: nc.vector.tensor_reduce(out=mx[:], in_=AFF[:], axis=AX.X, op=ALU.add), [BAFF], [Bmx])
        kb.op("dve", lambda: nc.vector.reciprocal(out=mx[:], in_=mx[:]), [], [Bmx])
        tt("dve", AFF[:], AFF[:], mx[:].unsqueeze(2).to_broadcast([128, 32, 16]), ALU.mult, [Bmx], [BAFF])
        if "aff_d" in dbg:
            aff_d = dscr("aff_d", [128, 32, 16], F32)
            ld("sp", aff_d[:, :, :], AFF[:], [], [BAFF])
    if stop_after <= 6:
        kb.finish(); return nc

    NE = NEXP
    wg_in = din("w_gate", [NE, D, D]); wu_in = din("w_up", [NE, D, D]); wd_in = din("w_down", [NE, D, D])
    iota_in = din("iota512", [128, 512]); tri_in = din("tri", [128, 128]); tvc_in = din("tvc", [128, 32, 2])
    y2_d = dscr("y2_d", [T, D], F32)
    By2 = Buf()
    pos = G.sb("pos", [128, 32, 16], F32); maskf = G.sb("maskf", [128, 32, 16], F32)
    affh = G.sb("affh", [128, 32, 16], BF16); affl = G.sb("affl", [128, 32, 16], BF16)
    Bsel = Buf()
    with Phase(kb) as ph:
        zt = ph.sb("zt", [128, D], F32); Bz = Buf()
        kb.op("pool", lambda: nc.gpsimd.memset(zt[:], 0.0), [], [Bz])
        for i in range(T // 128):
            kb.dma("sp", lambda i=i: nc.sync.dma_start(out=y2_d[i * 128:(i + 1) * 128, :], in_=zt[:]), [Bz], [])
        lo = ph.sb("lo", [128, 16], F32); hi = ph.sb("hi", [128, 16], F32); mid = ph.sb("mid", [128, 16], F32); Bl = Buf()
        cmpb = ph.sb("cmpb", [128, 32, 16], BF16); Bcmp = Buf()
        partb = ph.sb("partb", [128, 16], BF16); Bpart = Buf()
        selu = ph.sb("selu", [128, 2, 16], U32); Bsu = Buf()
        tri_b = ph.sb("tri_b", [128, 128], BF16); Btri = Buf()
        ld("pool", tri_b[:], tri_in[:, :], [Btri])
        pc = ph.ps("pc", [128, 16], F32); Bpc = Buf()
        pcs = ph.ps("pcs", [128, 512], F32); Bpcs = Buf()
        pw = ph.ps("pw", [128, 512], F32); Bpw = Buf()
        maskb = ph.sb("maskb", [128, 32, 16], BF16); Bmb = Buf()
        tcum = ph.sb("tcum", [128, 32, 16], F32); Btc = Buf()
        kb.op("dve", lambda: nc.vector.memset(lo[:], 0.0), [], [Bl])
        kb.op("dve", lambda: nc.vector.memset(hi[:], 1.0), [], [Bl])
        for it in range(NBISECT):
            tt("dve", mid[:], lo[:], hi[:], ALU.add, [], [Bl])
            ts("dve", mid[:], mid[:], 0.5, None, ALU.mult, None, [], [Bl])
            tt("dve", cmpb[:], AFF[:], mid[:].unsqueeze(1).to_broadcast([128, 32, 16]), ALU.is_ge, [BAFF, Bl], [Bcmp])
            with nc.allow_low_precision(reason="exact small integer counts"):
                kb.op("dve", lambda: nc.vector.tensor_reduce(out=partb[:], in_=cmpb[:].rearrange("p t e -> p e t"), axis=AX.X, op=ALU.add), [Bcmp], [Bpart])
            mm(pc[:], ones_b[:], partb[:], True, True, [Bpart, B_const], [Bpc])
            ts("dve", selu[:, 0, :], pc[:], 511.5, None, ALU.is_ge, None, [Bpc], [Bsu])
            ts("dve", selu[:, 1, :], pc[:], 511.5, None, ALU.is_lt, None, [Bpc], [Bsu])
            kb.op("dve", lambda: nc.vector.copy_predicated(out=lo[:], mask=selu[:, 0, :], data=mid[:]), [Bsu], [Bl])
            kb.op("dve", lambda: nc.vector.copy_predicated(out=hi[:], mask=selu[:, 1, :], data=mid[:]), [Bsu], [Bl])
        tt("dve", maskb[:], AFF[:], lo[:].unsqueeze(1).to_broadcast([128, 32, 16]), ALU.is_ge, [BAFF, Bl], [Bmb])
        cp("dve", maskf[:], maskb[:], [Bmb], [Bsel])
        mbf = maskb[:].rearrange("p t e -> p (t e)")
        mm(pcs[:], ones_b[:], mbf, True, True, [Bmb, B_const], [Bpcs])
        mm(pw[:], tri_b[:], mbf, True, True, [Bmb, Btri], [Bpw])
        kb.op("dve", lambda: nc.vector.memset(tcum[:, 0, :], 0.0), [], [Btc])
        for t in range(1, 32):
            tt("dve", tcum[:, t, :], tcum[:, t - 1, :], pcs[:, (t - 1) * 16:t * 16], ALU.add, [Bpcs], [Btc])
        tt("dve", pos[:].rearrange("p t e -> p (t e)"), pw[:], tcum[:].rearrange("p t e -> p (t e)"), ALU.add, [Bpw, Btc], [Bsel])
        cp("dve", affh[:], AFF[:], [BAFF], [Bsel])
        tt("dve", affl[:], AFF[:], affh[:], ALU.subtract, [BAFF], [Bsel])
        if "sel_d" in dbg:
            sel_d = dscr("sel_d", [2, 128, 32, 16], F32)
            ld("sp", sel_d[0], pos[:], [], [Bsel]); ld("sp", sel_d[1], maskf[:], [], [Bsel])
    if stop_after <= 7:
        kb.finish(); return nc

    with Phase(kb) as ph:
        iota = ph.sb("iota", [128, 512], F32); Bio = Buf()
        ld("sp", iota[:], iota_in[:, :], [Bio])
        tv = [ph.sb(f"tv{i}", [128, 32, 4], BF16) for i in range(2)]; Btv = [Buf(), Buf()]
        for i in range(2):
            ld("pool", tv[i][:, :, 0:2], tvc_in[:, :, :], [Btv[i]])
        Pm = [ph.sb(f"Pm{i}", [128, 32, 128], BF16) for i in range(2)]; BPm = [Buf(), Buf()]
        pidx = ph.ps("pidx", [128, 4], F32); Bpidx = Buf()
        idxf = ph.sb("idxf", [128, 4], F32); Bidxf = Buf()
        idx_i = ph.sb("idx_i", [128, 2, 4], I32); Bidx = [[Buf() for _ in range(4)] for _ in range(2)]
        wsl = ph.sb("wsl", [128, 2, 4], F32)
        xg = ph.sb("xg", [128, 4, D], BF16); Bxg = [Buf() for _ in range(4)]
        ptx = ph.ps("ptx", [128, 8, 128], BF16); Bptx = Buf()
        xgT = [ph.sb(f"xgT{i}", [128, NCH, 512], BF16) for i in range(2)]; BxgT = [Buf(), Buf()]
        NR = 5
        Wr = [ph.sb(f"Wr{i}", [128, NCH, 512], BF16) for i in range(NR)]; BWr = [Buf() for _ in range(NR)]
        pa = [ph.ps(f"pa{i}", [128, 512], F32) for i in range(2)]; Bpa = [Buf(), Buf()]
        pb = [ph.ps(f"pb{i}", [128, 512], F32) for i in range(2)]; Bpb = [Buf(), Buf()]
        pyo = [ph.ps(f"pyo{i}", [128, 512], F32) for i in range(2)]; Bpyo = [Buf(), Buf()]
        sa = [ph.sb(f"sa{i}", [128, 512], F32) for i in range(2)]; Bsa = [Buf(), Buf()]
        hT = ph.sb("hTe", [128, NCH, 512], BF16); BhT = Buf()
        Ysb = ph.sb("Ye", [128, 4, D], BF16); BYe = [Buf() for _ in range(4)]

        def A_tv(e):
            s = e % 2
            cp("pool", tv[s][:, :, 2], affh[:, :, e], [Bsel], [Btv[s]])
            cp("pool", tv[s][:, :, 3], affl[:, :, e], [Bsel], [Btv[s]])

        def A_pm(e, q):
            b = q % 2
            for t in range(32):
                ts("dve", Pm[b][:, t, :], iota[:, q * 128:(q + 1) * 128], pos[:, t, e:e + 1], maskf[:, t, e:e + 1], ALU.is_equal, ALU.mult, [Bio, Bsel], [BPm[b]])

        def A_idx(e, q):
            s = e % 2; b = q % 2
            for t in range(32):
                mm(pidx[:], Pm[b][:, t, :], tv[s][:, t, :], t == 0, t == 31, [BPm[b], Btv[s]], [Bpidx])
            cp("dve", idxf[:], pidx[:], [Bpidx], [Bidxf])
            tt("dve", idx_i[:, s, q:q + 1], idxf[:, 0:1], idxf[:, 1:2], ALU.add, [Bidxf], [Bidx[s][q]])
            tt("dve", wsl[:, s, q:q + 1], idxf[:, 2:3], idxf[:, 3:4], ALU.add, [Bidxf], [Bidx[s][q]])

        def A_gather(e, q):
            s = e % 2
            kb.dma("pool", lambda: nc.gpsimd.indirect_dma_start(
                out=xg[:, q, :], out_offset=None, in_=h2_d[:, :],
                in_offset=bass.IndirectOffsetOnAxis(ap=idx_i[:, s, q:q + 1], axis=0)), [Bidx[s][q]], [Bxg[q]])

        def A_tr(e, q):
            s = e % 2
            for cg in range(2):
                for c in range(8):
                    cc = cg * 8 + c
                    tr(ptx[:, c, :], xg[:, q, cc * 128:(cc + 1) * 128], ident_b[:], [Bxg[q], B_const], [Bptx])
                cp("act" if cg == 0 else "dve", xgT[s][:, cg * 8:(cg + 1) * 8, q * 128:(q + 1) * 128], ptx[:], [Bptx], [BxgT[s]])

        pieces = []
        for e in range(NE):
            for fg in range(4):
                pieces.append((wg_in, e, fg)); pieces.append((wu_in, e, fg))
            for db in range(4):
                pieces.append((wd_in, e, db))
        pstate = dict(loaded=0)

        def load_piece(k):
            wt, e, j = pieces[k]
            r = k % NR
            wvw = wt[e].rearrange("(c p) n -> p c n", p=128)
            ld("pool", Wr[r][:], wvw[:, :, j * 512:(j + 1) * 512], [BWr[r]])

        def need(k):
            while pstate["loaded"] <= min(k + NR - 2, len(pieces) - 1):
                load_piece(pstate["loaded"]); pstate["loaded"] += 1

        def stageB(e, hooks):
            s = e % 2
            kbase = e * 12
            ii = 0
            for fg in range(4):
                kg = kbase + fg * 2; ku = kg + 1
                need(ku)
                rg = kg % NR; ru = ku % NR
                for fc in range(4):
                    i2 = ii % 2; ii += 1
                    for c in range(NCH):
                        mm(pa[i2][:], Wr[rg][:, c, fc * 128:(fc + 1) * 128], xgT[s][:, c, :], c == 0, c == NCH - 1, [BWr[rg], BxgT[s]], [Bpa[i2]])
                    for c in range(NCH):
                        mm(pb[i2][:], Wr[ru][:, c, fc * 128:(fc + 1) * 128], xgT[s][:, c, :], c == 0, c == NCH - 1, [BWr[ru], BxgT[s]], [Bpb[i2]])
                    act(sa[i2][:], pa[i2][:], AF.Silu, [Bpa[i2]], [Bsa[i2]])
                    tt("dve", hT[:, fg * 4 + fc, :], sa[i2][:], pb[i2][:], ALU.mult, [Bsa[i2], Bpb[i2]], [BhT])
                hooks("fg", fg)
            jj = 0
            for db in range(4):
                kd = kbase + 8 + db
                need(kd)
                rd_ = kd % NR
                for sc in range(4):
                    i2 = jj % 2; jj += 1
                    for fcn in range(NCH):
                        mm(pyo[i2][:], hT[:, fcn, sc * 128:(sc + 1) * 128], Wr[rd_][:, fcn, :], fcn == 0, fcn == NCH - 1, [BhT, BWr[rd_]], [Bpyo[i2]])
                    if jj % 2 == 0:
                        act(Ysb[:, sc, db * 512:(db + 1) * 512], pyo[i2][:], AF.Copy, [Bpyo[i2], Bidx[s][sc]], [BYe[sc]], scale=wsl[:, s, sc:sc + 1])
                    else:
                        ts("dve", Ysb[:, sc, db * 512:(db + 1) * 512], pyo[i2][:], wsl[:, s, sc:sc + 1], None, ALU.mult, None, [Bpyo[i2], Bidx[s][sc]], [BYe[sc]])
                hooks("db", db)
            for sc in range(4):
                kb.dma("pool", lambda sc=sc, s=s: nc.gpsimd.indirect_dma_start(
                    out=y2_d[:, :], out_offset=bass.IndirectOffsetOnAxis(ap=idx_i[:, s, sc:sc + 1], axis=0),
                    in_=Ysb[:, sc, :], in_offset=None, compute_op=ALU.add), [BYe[sc], Bidx[s][sc]], [By2])

        kb.barrier()
        A_tv(0)
        for q in range(4):
            A_pm(0, q); A_idx(0, q); A_gather(0, q)
        for q in range(4):
            A_tr(0, q)
        for e in range(NE):
            nx = e + 1

            def hooks(kind, j, nx=nx):
                if nx >= NE:
                    return
                if kind == "fg":
                    if j >= 1:
                        A_gather(nx, j - 1)
                    A_idx(nx, j)
                    if j + 1 < 4:
                        A_pm(nx, j + 1)
                else:
                    if j == 0:
                        A_gather(nx, 3)
                    A_tr(nx, j)
            if nx < NE:
                A_tv(nx)
                A_pm(nx, 0)
            stageB(e, hooks)
    if stop_after <= 8:
        kb.finish(); return nc

    with Phase(kb) as ph:
        gt2, Bgt2 = bcast_row(ph, "gt2", mod_d[0:1, 5 * D:6 * D])
        po2, Bpo2 = bcast_row(ph, "po2", post2[0:1, :])
        tt("dve", gt2[:], gt2[:], po2[:], ALU.mult, [Bpo2], [Bgt2])
        yt = [ph.sb(f"y2t{i}", [128, D], F32) for i in range(3)]; Byt = [Buf() for _ in range(3)]
        x1t = [ph.sb(f"x1u{i}", [128, D], F32) for i in range(3)]; Bx1 = [Buf() for _ in range(3)]
        junk = ph.sb("junk8", [128, D], BF16); Bjunk = Buf()
        sq = [ph.sb(f"sq8{i}", [128, 2], F32) for i in range(2)]; Bsq = [Buf(), Buf()]
        ot = [ph.sb(f"ot{i}", [128, D], F32) for i in range(2)]; Bot = [Buf(), Buf()]
        NT8 = T // 128

        def ld8(i):
            if i < NT8:
                ld("sp", yt[i % 3][:], y2_d[i * 128:(i + 1) * 128, :], [Byt[i % 3]])
                ld("sp", x1t[i % 3][:], x1_d[i * 128:(i + 1) * 128, :], [Bx1[i % 3]])
        ld8(0); ld8(1)
        for i in range(NT8):
            s = i % 2; s3 = i % 3
            t0 = i * 128
            ld8(i + 2)
            act(junk[:], yt[s3][:], AF.Square, [Byt[s3]], [Bjunk, Bsq[s]], accum_out=sq[s][:, 0:1])
            rstd_from(sq[s][:, 1:2], sq[s][:, 0:1], D, [], [Bsq[s]])
            stt(yt[s3][:], yt[s3][:], sq[s][:, 1:2], gt2[:], ALU.mult, ALU.mult, [Bsq[s], Bgt2], [Byt[s3]])
            tt("dve", ot[s][:], yt[s3][:], x1t[s3][:], ALU.add, [Byt[s3], Bx1[s3]], [Bot[s]])
            ld("pool", out[t0:t0 + 128, :], ot[s][:], [], [Bot[s]])

    kb.finish()
    return nc


def make_inputs(inp, b):
    L = 0
    Rm, cosT, sinT = rope_consts()
    m = dict(
        xk=np.ascontiguousarray(np.concatenate([inp["ctx"][b], inp["x"][b]], axis=0)),
        c2=np.ascontiguousarray(np.stack([inp["c"][b], inp["c_ctx"]], axis=0)),
        w_mod=inp["w_mod"][L], b_mod=inp["b_mod"][L][None, :],
        pre1=inp["pre_norm1"][L][None, :], post1=inp["post_norm1"][L][None, :],
        pre2=inp["pre_norm2"][L][None, :], post2=inp["post_norm2"][L][None, :],
        w_in=inp["w_in"][L],
        ident=np.eye(128, dtype=np.float32),
        q_norm=inp["q_norm"][L][:, None], k_norm=inp["k_norm"][L][:, None],
        rotm=Rm, cosT=cosT, sinT=sinT,
        w_o_att=inp["w_o_att"][L], w_o_ret=inp["w_o_ret"][L], w_out=inp["w_out"][L], w_router=inp["w_router"][L],
        w_gate=inp["w_gate"][L][:NEXP], w_up=inp["w_up"][L][:NEXP], w_down=inp["w_down"][L][:NEXP],
        iota512=np.ascontiguousarray(np.broadcast_to(np.arange(512, dtype=np.float32), (128, 512))),
        tri=np.triu(np.ones((128, 128), np.float32), 1),
        tvc=np.ascontiguousarray(np.stack([np.broadcast_to(np.arange(128, dtype=np.float32)[:, None], (128, 32)),
                                           np.broadcast_to(128.0 * np.arange(32, dtype=np.float32)[None, :], (128, 32))], axis=-1)),
        ret_decay=inp["ret_decay"][L].reshape(1, 8), ret_gn=np.ascontiguousarray(inp["ret_gn"][L].reshape(8, 128).T),
    )
    ii = np.arange(128, dtype=np.float32)
    jj = ii[:, None]; i2 = ii[None, :]
    tabs = np.stack([np.broadcast_to(i2 + 1, (128, 128)), np.broadcast_to(128 - i2, (128, 128)),
                     np.maximum(i2 - jj, 0), (i2 >= jj).astype(np.float32),
                     np.maximum(jj - i2, 0), (jj >= i2).astype(np.float32)]).astype(np.float32)
    m["ret_tabs"] = np.ascontiguousarray(tabs)
    p = ii
    m["ret_pcols"] = np.ascontiguousarray(np.stack([127 - p, p, 255 - p, 127 - p, p, 128 + p], axis=1).astype(np.float32))
    return m


_NC_CACHE = {}


def kernel(**inputs):
    inp = {k: np.asarray(v) for k, v in inputs.items()}
    if "nc" not in _NC_CACHE:
        _NC_CACHE["nc"] = build()
    nc = _NC_CACHE["nc"]
    B = inp["x"].shape[0]
    maps = [make_inputs(inp, b) for b in range(B)]
    in_maps = [maps[i % B] for i in range(8)]
    res = run_bass_kernel_spmd(nc, in_maps, core_ids=list(range(8)))
    out = np.stack([np.asarray(res.results[b]["out"]) for b in range(B)], axis=0)
    return out.astype(np.float32)
```
